# Optimizing a Trainium2 kernel written in Bass

```python
import jax, jax.numpy as jnp
from jax import lax
import numpy as np

D_MODEL = 1024
BATCH = 8
SEQ = 2048
DEPTH = 1

CONV_GROUPS = 8
CONV_GROUP_DIM = 64
D_CONV = CONV_GROUPS * CONV_GROUP_DIM
CONV_WIDTH = 3
RWKV_HEADS = 8
RWKV_HEAD = 64
D_RWKV = RWKV_HEADS * RWKV_HEAD
D_MIX = D_CONV + D_RWKV
DECAY_LORA = 64
AAA_LORA = 64
GATE_LORA = 128
D_RWKV_PROJ = 3 * D_RWKV + DECAY_LORA + AAA_LORA + GATE_LORA
D_IN = 3 * D_CONV + D_RWKV_PROJ
N_GROUPS = 4
EXPERTS_PER_GROUP = 8
N_EXPERTS = N_GROUPS * EXPERTS_PER_GROUP
TOP_K_IN_GROUP = 2
D_EXPERT = D_MODEL // 4
RMS_EPS = 1e-6
LN_X_EPS = 64e-5
L2_EPS = 1e-12

kernel_name = "hybrid_conv_rwkv7_hmoe_block"


def rms_norm(x, g):
    xf = x.astype(jnp.float32)
    y = xf * lax.rsqrt(jnp.mean(xf * xf, axis=-1, keepdims=True) + RMS_EPS)
    return y.astype(x.dtype) * g


def token_shift(z):
    return jnp.pad(z, ((0, 0), (1, 0), (0, 0)))[:, :-1]


def causal_depthwise_conv(u, w):
    return lax.conv_general_dilated(
        u, w[:, None, :].astype(u.dtype), window_strides=(1,), padding=[(CONV_WIDTH - 1, 0)],
        dimension_numbers=("NWC", "WIO", "NWC"), feature_group_count=u.shape[-1])


def short_conv_mixer(z, conv_w):
    b_gate, c_gate, h = jnp.split(z, 3, axis=-1)
    return b_gate * causal_depthwise_conv(c_gate * h, conv_w)


def rwkv7_recurrence(r, decay, k, v, kk, a):
    bsz = r.shape[0]
    to_time = lambda t: jnp.moveaxis(t.astype(jnp.float32), 1, 0)

    def step(S, inp):
        r_t, w_t, k_t, v_t, kk_t, a_t = inp
        s_kk = jnp.einsum("bhvk,bhk->bhv", S, -kk_t)
        S = (S * w_t[:, :, None, :]
             + s_kk[..., None] * (kk_t * a_t)[:, :, None, :]
             + v_t[..., None] * k_t[:, :, None, :])
        o_t = jnp.einsum("bhvk,bhk->bhv", S, r_t)
        return S, o_t

    S0 = jnp.zeros((bsz, RWKV_HEADS, RWKV_HEAD, RWKV_HEAD), jnp.float32)
    _, o = lax.scan(step, S0, (to_time(r), to_time(decay), to_time(k),
                               to_time(v), to_time(kk), to_time(a)))
    return jnp.moveaxis(o, 0, 1)


def rwkv7_mixer(z, mu, decay_up, decay_base, aaa_up, aaa_base, gate_up,
                k_k, k_a, r_k, ln_w, ln_b):
    bsz, seq, _ = z.shape
    zs = z + (token_shift(z) - z) * mu
    cuts = [D_RWKV, 2 * D_RWKV, 3 * D_RWKV, 3 * D_RWKV + DECAY_LORA,
            3 * D_RWKV + DECAY_LORA + AAA_LORA]
    r, k, v, w_lo, a_lo, g_lo = jnp.split(zs, cuts, axis=-1)
    w = -jax.nn.softplus(-(decay_base + jnp.tanh(w_lo) @ decay_up)) - 0.5
    decay = jnp.exp(-jnp.exp(w.astype(jnp.float32)))
    a = jax.nn.sigmoid(aaa_base + a_lo @ aaa_up)
    g = jax.nn.sigmoid(g_lo) @ gate_up
    heads = lambda t: t.reshape(bsz, seq, RWKV_HEADS, RWKV_HEAD)
    kk = heads(k * k_k).astype(jnp.float32)
    kk = kk / jnp.maximum(jnp.linalg.norm(kk, axis=-1, keepdims=True), L2_EPS)
    k = k * (1.0 + (a - 1.0) * k_a)
    rh, kh, vh, ah = heads(r), heads(k), heads(v), heads(a)
    o = rwkv7_recurrence(rh, heads(decay), kh, vh, kk, ah)
    mean = jnp.mean(o, axis=-1, keepdims=True)
    var = jnp.mean(jnp.square(o - mean), axis=-1, keepdims=True)
    o = ((o - mean) * lax.rsqrt(var + LN_X_EPS)).astype(z.dtype)
    o = o * ln_w.reshape(RWKV_HEADS, RWKV_HEAD) + ln_b.reshape(RWKV_HEADS, RWKV_HEAD)
    bonus = jnp.sum(rh * kh * r_k, axis=-1, keepdims=True) * vh
    return (o + bonus).reshape(bsz, seq, D_RWKV) * g


def hierarchical_moe(h, group_w, group_b, expert_w, expert_b, w_gate, w_up, w_down):
    shape = h.shape
    t = h.reshape(-1, shape[-1])
    n_tok = t.shape[0]
    group_prob = jax.nn.softmax((t @ group_w + group_b).astype(jnp.float32), axis=-1)
    g_prob, g_idx = lax.top_k(group_prob, 1)
    e_logits = (t @ expert_w + expert_b).astype(jnp.float32)
    e_logits = e_logits.reshape(n_tok, N_GROUPS, EXPERTS_PER_GROUP)
    in_group = e_logits[jnp.arange(n_tok), g_idx[:, 0]]
    top_logit, top_idx = lax.top_k(in_group, TOP_K_IN_GROUP)
    gates = g_prob * jax.nn.softmax(top_logit, axis=-1)
    expert_idx = g_idx * EXPERTS_PER_GROUP + top_idx
    combine = jnp.sum(jax.nn.one_hot(expert_idx, N_EXPERTS, dtype=jnp.float32)
                      * gates[..., None], axis=1).astype(h.dtype)
    out = jnp.zeros_like(t)
    for e in range(N_EXPERTS):
        hid = jax.nn.silu(t @ w_gate[e]) * (t @ w_up[e])
        out = out + combine[:, e:e + 1] * (hid @ w_down[e])
    return out.reshape(shape)


def setup_inputs(seed: int = 0) -> dict:
    key = jax.random.key(seed)
    ks = jax.random.split(key, 26)
    nrm = lambda k, s, sc: jax.random.normal(k, s, jnp.float32) * sc
    L = DEPTH
    return {
        "x": nrm(ks[0], (BATCH, SEQ, D_MODEL), 1.0),
        "norm_mix_g": 1.0 + nrm(ks[1], (L, D_MODEL), 0.02),
        "w_in": nrm(ks[2], (L, D_MODEL, D_IN), D_MODEL ** -0.5),
        "rwkv_mu": jax.random.uniform(ks[3], (L, D_RWKV_PROJ), jnp.float32),
        "conv_w": nrm(ks[4], (L, CONV_WIDTH, D_CONV), CONV_WIDTH ** -0.5),
        "decay_up": nrm(ks[5], (L, DECAY_LORA, D_RWKV), 0.5 * DECAY_LORA ** -0.5),
        "decay_base": jax.random.uniform(ks[6], (L, D_RWKV), jnp.float32, -6.0, 1.0),
        "aaa_up": nrm(ks[7], (L, AAA_LORA, D_RWKV), 0.5 * AAA_LORA ** -0.5),
        "aaa_base": nrm(ks[8], (L, D_RWKV), 0.1),
        "gate_up": nrm(ks[9], (L, GATE_LORA, D_RWKV), GATE_LORA ** -0.5),
        "k_k": 0.85 + nrm(ks[10], (L, D_RWKV), 0.05),
        "k_a": 1.0 + nrm(ks[11], (L, D_RWKV), 0.05),
        "r_k": nrm(ks[12], (L, RWKV_HEADS, RWKV_HEAD), 0.1),
        "ln_x_w": 1.0 + nrm(ks[13], (L, D_RWKV), 0.02),
        "ln_x_b": nrm(ks[14], (L, D_RWKV), 0.02),
        "w_out": nrm(ks[15], (L, D_MIX, D_MODEL), D_MIX ** -0.5),
        "norm_ffn_g": 1.0 + nrm(ks[16], (L, D_MODEL), 0.02),
        "router_group_w": nrm(ks[17], (L, D_MODEL, N_GROUPS), D_MODEL ** -0.5),
        "router_group_b": nrm(ks[18], (L, N_GROUPS), 0.01),
        "router_expert_w": nrm(ks[19], (L, D_MODEL, N_EXPERTS), D_MODEL ** -0.5),
        "router_expert_b": nrm(ks[20], (L, N_EXPERTS), 0.01),
        "expert_w_gate": nrm(ks[21], (L, N_EXPERTS, D_MODEL, D_EXPERT), D_MODEL ** -0.5),
        "expert_w_up": nrm(ks[22], (L, N_EXPERTS, D_MODEL, D_EXPERT), D_MODEL ** -0.5),
        "expert_w_down": nrm(ks[23], (L, N_EXPERTS, D_EXPERT, D_MODEL), D_EXPERT ** -0.5),
        "norm_final_g": 1.0 + nrm(ks[24], (D_MODEL,), 0.02),
    }


def reference(x, norm_mix_g, w_in, rwkv_mu, conv_w, decay_up, decay_base, aaa_up,
              aaa_base, gate_up, k_k, k_a, r_k, ln_x_w, ln_x_b, w_out, norm_ffn_g,
              router_group_w, router_group_b, router_expert_w, router_expert_b,
              expert_w_gate, expert_w_up, expert_w_down, norm_final_g):
    h = x
    for l in range(DEPTH):
        u = rms_norm(h, norm_mix_g[l])
        z = u @ w_in[l]
        y_conv = short_conv_mixer(z[..., :3 * D_CONV], conv_w[l])
        y_rwkv = rwkv7_mixer(z[..., 3 * D_CONV:], rwkv_mu[l], decay_up[l], decay_base[l],
                             aaa_up[l], aaa_base[l], gate_up[l], k_k[l], k_a[l], r_k[l],
                             ln_x_w[l], ln_x_b[l])
        y = jnp.concatenate([y_conv, y_rwkv], axis=-1)
        h = h + y @ w_out[l]
        u = rms_norm(h, norm_ffn_g[l])
        h = h + hierarchical_moe(u, router_group_w[l], router_group_b[l],
                                 router_expert_w[l], router_expert_b[l],
                                 expert_w_gate[l], expert_w_up[l], expert_w_down[l])
    return rms_norm(h, norm_final_g)
```

```python
import numpy as np
from contextlib import ExitStack
import concourse.bass as bass
import concourse.mybir as mybir
from concourse.bass_utils import run_bass_kernel_spmd

F32 = mybir.dt.float32
BF16 = mybir.dt.bfloat16
AF = mybir.ActivationFunctionType
ALU = mybir.AluOpType
AX = mybir.AxisListType

T = 2048
D = 1024
NTILE = 16
NE = 32
C0 = float(np.exp(-0.5))
ORDER = [24, 25, 12, 16, 20, 13, 17, 21, 14, 18, 22, 15, 19, 23,
         0, 4, 8, 1, 5, 9, 2, 6, 10, 3, 7, 11]
PC_MU = 0
PC_CW = 14
PC_DB = 26
PC_AB = 30
PC_KK = 34
PC_KA = 38
PC_RK = 42
PC_LW = 46
PC_LB = 50
NPAR = 54


class Prog:
    ENG = ('pe', 'act', 'dve', 'pool', 'sp')

    def __init__(self, nc, es, n_dma=24):
        self.nc = nc
        self.q = {e: [] for e in self.ENG}
        self.sem = {e: es.enter_context(nc.semaphore("s_" + e)) for e in ('pe', 'act', 'dve', 'pool')}
        self.dsem = [es.enter_context(nc.semaphore("d%d" % i)) for i in range(n_dma)]
        self.cnt = {e: 0 for e in ('pe', 'act', 'dve', 'pool')}
        self.dval = [0] * n_dma
        self.drr = 0
        self.waited = {}
        self.lastw = {}
        self.readers = {}
        self.ninst = 0
        self.cap = None

    def _semh(self, key):
        return self.sem[key] if isinstance(key, str) else self.dsem[key]

    def _need(self, eng, deps):
        for key, val in deps.items():
            if key == eng and eng == 'pe':
                continue
            if self.waited.get((eng, key), 0) >= val:
                continue
            self.waited[(eng, key)] = val
            h = self._semh(key)
            self.q[eng].append(lambda e, h=h, val=val: e.wait_ge(h, val))

    def _collect(self, eng, reads, writes):
        deps = {}
        for t in list(reads) + list(writes):
            w = self.lastw.get(t)
            if w and deps.get(w[0], 0) < w[1]:
                deps[w[0]] = w[1]
        for t in writes:
            for k, v in self.readers.get(t, {}).items():
                if k == eng and isinstance(k, str):
                    continue
                if deps.get(k, 0) < v:
                    deps[k] = v
        return deps

    def _record(self, key, val, reads, writes):
        for t in reads:
            r = self.readers.setdefault(t, {})
            if r.get(key, 0) < val:
                r[key] = val
        for t in writes:
            self.lastw[t] = (key, val)
            self.readers[t] = {}

    def op(self, eng, fn, reads=(), writes=(), inc=True):
        if self.cap is not None:
            self.cap.append(('op', eng, fn, list(reads), list(writes), inc))
            return
        self.ninst += 1
        deps = self._collect(eng, reads, writes)
        self._need(eng, deps)
        if inc or eng != 'pe':
            self.cnt[eng] += 1
            val = self.cnt[eng]
            h = self.sem[eng]
            self.q[eng].append(lambda e, fn=fn, h=h: fn(e).then_inc(h, 1))
        else:
            val = self.cnt[eng] + 1
            self.q[eng].append(lambda e, fn=fn: fn(e))
        self._record(eng, val, reads, writes)

    def dma(self, out, in_, reads=(), writes=()):
        if self.cap is not None:
            self.cap.append(('dma', out, in_, list(reads), list(writes)))
            return
        self.ninst += 1
        eng = 'sp'
        j = self.drr
        self.drr = (self.drr + 1) % len(self.dsem)
        deps = self._collect(eng, reads, writes)
        if self.dval[j] > 0:
            deps[j] = max(deps.get(j, 0), self.dval[j])
        self._need(eng, deps)
        self.dval[j] += 16
        val = self.dval[j]
        h = self.dsem[j]
        self.q[eng].append(lambda e, h=h, out=out, in_=in_: e.dma_start(out=out, in_=in_).then_inc(h, 16))
        self._record(j, val, reads, writes)

    def capture(self, f):
        assert self.cap is None
        self.cap = []
        f()
        ops, self.cap = self.cap, None
        return ops

    def replay(self, *lists):
        lists = [l for l in lists if l]
        idx = [0] * len(lists)
        tot = max(len(l) for l in lists) if lists else 0
        for step in range(tot):
            for j, l in enumerate(lists):
                hi = (step + 1) * len(l) // tot
                while idx[j] < hi:
                    o = l[idx[j]]
                    idx[j] += 1
                    if o[0] == 'op':
                        self.op(o[1], o[2], o[3], o[4], o[5])
                    else:
                        self.dma(o[1], o[2], o[3], o[4])

    def barrier(self):
        deps = {k: v for k, v in self.cnt.items() if v > 0}
        for j, v in enumerate(self.dval):
            if v > 0:
                deps[j] = v
        for eng in self.ENG:
            d = {k: v for k, v in deps.items() if k != eng}
            self._need(eng, d)

    def finish(self):
        deps = {j: v for j, v in enumerate(self.dval) if v > 0}
        self._need('sp', deps)
        with self.nc.Block() as block:
            @block.tensor
            def _(e):
                for f in self.q['pe']:
                    f(e)

            @block.scalar
            def _(e):
                for f in self.q['act']:
                    f(e)

            @block.vector
            def _(e):
                for f in self.q['dve']:
                    f(e)

            @block.gpsimd
            def _(e):
                for f in self.q['pool']:
                    f(e)

            @block.sync
            def _(e):
                for f in self.q['sp']:
                    f(e)


class Arena:
    def __init__(self, big, nwords):
        self.big = big
        self.n = nwords
        self.top = 0

    def f32(self, n, at=None):
        if at is None:
            at = self.top
            self.top += n
        assert at + n <= self.n, (at, n, self.n)
        return self.big[:, at:at + n]

    def bf(self, n, at=None):
        w = (n + 1) // 2
        if at is None:
            at = self.top
            self.top += w
        assert at + w <= self.n, (at, w, self.n)
        return self.big[:, at:at + w].bitcast(BF16)


def build(stop=None, dbg=()):
    nc = bass.Bass("TRN2", target_bir_lowering=False)
    dram = {}

    def din(name, shape, dt=F32):
        dram[name] = nc.dram_tensor(name, list(shape), dt, kind="ExternalInput").ap()
        return dram[name]

    x_d = din("x", [T, D])
    win_d = din("w_in_t", [26, 128, 8 * 128])
    par_d = din("params", [128, NPAR])
    lup_d = din("lora_up", [128, 512])
    gup_d = din("gate_up", [128, 512])
    g1_d = din("g_mix", [D])
    g2_d = din("g_ffn", [D])
    g3_d = din("g_fin", [D])
    wout_d = din("w_out_t", [128, 8 * 1024])
    wr_d = din("w_r_t", [128, 8 * 36])
    br_d = din("b_r", [36])
    wg_d = din("wg_t", [NE, 128, 8 * 256])
    wu_d = din("wu_t", [NE, 128, 8 * 256])
    wd_d = din("wd_t", [NE, 128, 2 * 1024])
    cst_d = din("consts", [128, 1024])
    out_d = nc.dram_tensor("out", [T, D], F32, kind="ExternalOutput").ap()
    dbg_out = {}

    es = ExitStack()
    with es:
        NW = 51200
        big = es.enter_context(nc.sbuf_tensor("big", [128, NW], F32))
        banks = [es.enter_context(nc.psum_tensor("ps%d" % i, [128, 512], F32)) for i in range(8)]
        P = Prog(nc, es)
        A = Arena(big, NW)

        def dump(name, ap, shape, dt=F32, reads=()):
            if name not in dbg:
                return
            t = nc.dram_tensor("dbg_" + name, list(shape), dt, kind="ExternalOutput").ap()
            dbg_out[name] = t
            P.dma(t, ap, reads=reads)

        cst = A.f32(1024)
        par = A.f32(64)
        gbc = A.f32(1024)
        P.dma(cst, cst_d[:, :], writes=['cst'])
        P.dma(par[:, 0:NPAR], par_d[:, :], writes=['par'])
        identF = cst[:, 0:128]
        bonesF = cst[:, 128:256]
        cstb = cst[:, 256:1024].bitcast(BF16)
        identB = cstb[:, 0:128]
        bonesB = cstb[:, 128:256]
        omka = A.f32(4)
        hpar = A.f32(8)
        omm_ = A.f32(16)
        P.op('dve', lambda e: e.tensor_scalar(out=omka, in0=par[:, PC_KA:PC_KA + 4], scalar1=-1.0, scalar2=1.0,
                                               op0=ALU.mult, op1=ALU.add), reads=['par'], writes=['omka'])

        base0 = A.top
        uT = A.bf(8 * T).rearrange("p (k t) -> p k t", t=T)
        tha = A.bf(T)
        sg = A.bf(T)
        pers0 = A.top
        AR = A.bf(4 * 2 * T).rearrange("p (h x t) -> p h x t", h=4, x=2)
        BK = A.bf(4 * 2 * T).rearrange("p (h x t) -> p h x t", h=4, x=2)
        VB = A.bf(4 * 2 * T).rearrange("p (h x t) -> p h x t", h=4, x=2)
        WC = A.f32(128).rearrange("p (h c) -> p h c", h=4)
        pers1 = A.top
        wst = [A.f32(1024) for _ in range(2)]
        wbf = [A.bf(1024).rearrange("p (k c) -> p k c", c=128) for _ in range(4)]
        lupb = A.bf(512)
        gupb = A.bf(512)
        work0 = A.top

        xall = A.f32(16 * D, at=pers0).rearrange("p (i d) -> p i d", d=D)
        xn = [A.f32(D, at=work0 + i * D) for i in range(2)]
        junk = A.bf(D, at=work0 + 2 * D)
        ss = A.f32(16, at=work0 + 2 * D + 512)
        rstd = A.f32(16, at=work0 + 2 * D + 528)
        lst = A.f32(512, at=work0 + 2 * D + 1024)
        gst = A.f32(512, at=work0 + 2 * D + 1536)

        P.dma(gbc, g1_d.partition_broadcast(128), writes=['gbc'])
        P.dma(lst, lup_d[:, :], writes=['lst'])
        P.dma(gst, gup_d[:, :], writes=['gst'])
        P.op('pool', lambda e: e.tensor_copy(out=lupb, in_=lst), reads=['lst'], writes=['lupb'])
        P.op('pool', lambda e: e.tensor_copy(out=gupb, in_=gst), reads=['gst'], writes=['gupb'])

        def rms_stats(src, tag):
            for i in range(NTILE):
                P.op('act', lambda e, i=i: e.activation(out=junk, in_=src(i), func=AF.Square,
                                                        accum_out=ss[:, i:i + 1]),
                     reads=[tag + str(i)], writes=['junk', 'ss'])
            P.op('dve', lambda e: e.tensor_scalar(out=rstd, in0=ss, scalar1=1.0 / D, scalar2=1e-6,
                                                   op0=ALU.mult, op1=ALU.add), reads=['ss'], writes=['rstd'])
            P.op('act', lambda e: e.activation(out=rstd, in_=rstd, func=AF.Ln), reads=['rstd'], writes=['rstd'])
            P.op('act', lambda e: e.activation(out=rstd, in_=rstd, func=AF.Exp, scale=-0.5), reads=['rstd'], writes=['rstd'])

        for i in range(NTILE):
            P.dma(xall[:, i, :], x_d[i * 128:(i + 1) * 128, :], writes=['xa%d' % i])
        rms_stats(lambda i: xall[:, i, :], 'xa')

        def norm_transpose(src, tag, dstT, dst_tag):
            for i in range(NTILE):
                xb = xn[i % 2]
                P.op('dve', lambda e, i=i, xb=xb: e.scalar_tensor_tensor(
                    out=xb, in0=src(i), scalar=rstd[:, i:i + 1], in1=gbc, op0=ALU.mult, op1=ALU.mult),
                    reads=[tag + str(i), 'rstd', 'gbc'], writes=['xn%d' % (i % 2)])
                for half in range(2):
                    bk = banks[(2 * i + half) % 4]
                    btag = 'bank%d' % ((2 * i + half) % 4)
                    for kk in range(4):
                        k = half * 4 + kk
                        P.op('pe', lambda e, bk=bk, kk=kk, k=k, xb=xb: e.transpose(
                            out=bk[:, kk * 128:(kk + 1) * 128], in_=xb[:, k * 128:(k + 1) * 128], identity=identF),
                            reads=['xn%d' % (i % 2), 'cst'], writes=[btag], inc=(kk == 3))
                    P.op('act', lambda e, bk=bk, half=half, i=i: e.activation(
                        out=dstT[:, half * 4:half * 4 + 4, i * 128:(i + 1) * 128],
                        in_=bk[:, :].rearrange("p (k t) -> p k t", t=128), func=AF.Copy),
                        reads=[btag], writes=[dst_tag])

        norm_transpose(lambda i: xall[:, i, :], 'xa', uT, 'uT')
        dump('uT', uT[:, 0, :], [128, T], BF16, reads=['uT'])
        if stop == 'A':
            P.finish()
            return nc, dbg_out
        P.barrier()

        def ACT(out, in_, func, reads, writes, **kw):
            P.op('act', lambda e: e.activation(out=out, in_=in_, func=func, **kw), reads, writes)

        def TT(eng, out, a, b, op, reads, writes):
            P.op(eng, lambda e: e.tensor_tensor(out=out, in0=a, in1=b, op=op), reads, writes)

        def TS(eng, out, a, s1, s2, op0, op1, reads, writes):
            P.op(eng, lambda e: e.tensor_scalar(out=out, in0=a, scalar1=s1, scalar2=s2, op0=op0, op1=op1), reads, writes)

        def STT(out, a, s, b, op0, op1, reads, writes):
            P.op('dve', lambda e: e.scalar_tensor_tensor(out=out, in0=a, scalar=s, in1=b, op0=op0, op1=op1), reads, writes)

        def MM(out, lhsT, rhs, reads, writes, start=True, stop=True, inc=True):
            P.op('pe', lambda e: e.matmul(out, lhsT=lhsT, rhs=rhs, start=start, stop=stop), reads, writes, inc=inc)

        def BT(b):
            return 'bank%d' % b

        wk = {}
        wtop = [work0]

        def walloc(name, n=512, dt=F32):
            if dt == F32:
                wk[name] = A.f32(n, at=wtop[0])
                wtop[0] += n
            else:
                wk[name] = A.bf(n, at=wtop[0])
                wtop[0] += (n + 1) // 2
            return wk[name]

        for nm in ('r', 'k', 't1'):
            walloc(nm)
        chb = walloc('chb', 514)
        conv_end = wtop[0]
        zc = {}
        for nm in ('r', 'k', 'v'):
            zc[nm] = walloc('zc_' + nm, 514)
        dtile = walloc('d')
        smask = walloc('smask')
        for nm in ('v', 'sig', 'a', 'cs', 'winc', 'winv'):
            walloc(nm)
        sqb = walloc('sqb', 512, BF16)
        rkb = walloc('rkb', 512, BF16)
        assert wtop[0] <= NW, wtop[0]
        W = wk
        ycT = A.bf(4 * T, at=work0 + 3968).rearrange("p (j t) -> p j t", t=T)
        assert conv_end <= work0 + 3968

        TS('dve', hpar, par[:, PC_DB:PC_DB + 8], 0.5, None, ALU.mult, ALU.bypass, ['par'], ['hpar'])
        TS('dve', omm_[:, 0:14], par[:, PC_MU:PC_MU + 14], -1.0, 1.0, ALU.mult, ALU.add, ['par'], ['omm'])

        wctr = [0]

        def load_w(ci):
            b = wctr[0] % 4
            sb = wctr[0] % 2
            wctr[0] += 1
            P.dma(wst[sb], win_d[ci, :, :], writes=['wst%d' % sb])
            P.op('pool', lambda e: e.tensor_copy(out=wbf[b][:, :, :].rearrange("p k c -> p (k c)"), in_=wst[sb]),
                 reads=['wst%d' % sb], writes=['wbf%d' % b])
            return wbf[b], 'wbf%d' % b

        def proj(wap, wtag, tb, b):
            for k in range(8):
                MM(banks[b][:, :], wap[:, k, :], uT[:, k, tb * 512:(tb + 1) * 512], [wtag, 'uT'], [BT(b)],
                   start=(k == 0), stop=(k == 7), inc=(k == 7))

        def shift_mix(nm, b, tb, mucol, out_ap, out_tag):
            z = zc[nm]
            zt = 'zc_' + nm
            if tb > 0:
                ACT(z[:, 0:1], z[:, 512:513], AF.Copy, [zt], [zt])
            else:
                P.op('pool', lambda e: e.memset(z[:, 0:1], 0.0), [], [zt])
            ACT(z[:, 1:513], banks[b][:, :], AF.Copy, [BT(b)], [zt])
            ACT(out_ap, banks[b][:, :], AF.Copy, [BT(b), 'omm'], [out_tag], scale=omm_[:, mucol - PC_MU:mucol - PC_MU + 1])
            STT(out_ap, z[:, 0:512], par[:, mucol:mucol + 1], out_ap, ALU.mult, ALU.add, [zt, 'par', out_tag], [out_tag])

        w24, t24 = load_w(0)
        w25, t25 = load_w(1)
        for tb in range(4):
            sl = slice(tb * 512, (tb + 1) * 512)
            proj(w24, t24, tb, 0)
            shift_mix('r', 0, tb, PC_MU + 12, W['t1'], 't1')
            ACT(tha[0:64, sl], W['t1'][0:64, :], AF.Tanh, ['t1'], ['tha'])
            ACT(tha[64:128, sl], W['t1'][64:128, :], AF.Copy, ['t1'], ['tha'])
            proj(w25, t25, tb, 1)
            shift_mix('k', 1, tb, PC_MU + 13, W['t1'], 't1')
            ACT(W['t1'], W['t1'], AF.Tanh, ['t1'], ['t1'], scale=0.5)
            TS('pool', sg[:, sl], W['t1'], 0.5, 0.5, ALU.mult, ALU.add, ['t1'], ['sg'])
        dump('tha', tha, [128, T], BF16, reads=['tha'])
        dump('sg', sg, [128, T], BF16, reads=['sg'])

        P.op('pool', lambda e: e.memset(smask, 1.0), [], ['smask'])
        P.op('pool', lambda e: e.memset(smask.rearrange("p (c t) -> p c t", t=64)[:, :, 0:1], 0.0), [], ['smask'])

        WBs = {}
        _ar3, _bk3, _vb3 = pers0 + 6144, pers0 + 8192 + 6144, pers0 + 16384 + 6144
        for j_, nm in enumerate(('r', 'k', 'v', 'sig')):
            WBs[nm] = A.f32(512, at=_ar3 + j_ * 512)
        for j_, nm in enumerate(('a', 'cs', 'winc', 'winv')):
            WBs[nm] = A.f32(512, at=_bk3 + j_ * 512)
        WBs['t1'] = A.f32(512, at=_vb3)
        WBs['sqb'] = A.bf(512, at=_vb3 + 512)
        WBs['rkb'] = A.bf(512, at=_vb3 + 768)
        WAs = dict(W)
        WAs['sqb'] = sqb
        WAs['rkb'] = rkb

        def wset(hp, tb):
            if hp < 3 and (hp * 4 + tb) % 2 == 1:
                return WBs, 'B'
            return WAs, 'A'

        def prep(hp, tb, part='AB'):
            Wx, sx = wset(hp, tb)
            T_ = lambda n: n + sx
            u_ = '%d%d' % (hp, tb)
            sl = slice(tb * 512, (tb + 1) * 512)
            hc = slice(hp * 128, (hp + 1) * 128)
            r_, k_, v_, sig, a_, cs, winc, winv, t1 = (Wx[n] for n in ('r', 'k', 'v', 'sig', 'a', 'cs', 'winc', 'winv', 't1'))
            sqb_, rkb_ = Wx['sqb'], Wx['rkb']
            if 'A' in part:
                prepA_(hp, tb, Wx, T_, u_, sl, hc)
            if 'B' in part:
                prepB_(hp, tb, Wx, T_, u_, sl, hc)

        def prepA_(hp, tb, Wx, T_, u_, sl, hc):
            r_, k_, v_, sig, a_, cs, winc, winv, t1 = (Wx[n] for n in ('r', 'k', 'v', 'sig', 'a', 'cs', 'winc', 'winv', 't1'))
            sqb_, rkb_ = Wx['sqb'], Wx['rkb']
            MM(banks[4][:, :], lupb[0:64, hc], tha[0:64, sl], ['lupb', 'tha'], [BT(4)])
            MM(banks[5][:, :], lupb[64:128, hc], tha[64:128, sl], ['lupb', 'tha'], [BT(5)])
            ACT(sig, banks[4][:, :], AF.Tanh, [BT(4), 'hpar'], [T_('sig')], scale=0.5, bias=hpar[:, hp:hp + 1])
            ACT(a_, banks[5][:, :], AF.Tanh, [BT(5), 'hpar'], [T_('a')], scale=0.5, bias=hpar[:, 4 + hp:5 + hp])
            TS('dve', sig, sig, 0.5, 0.5, ALU.mult, ALU.add, [T_('sig')], [T_('sig')])
            TS('dve', a_, a_, 0.5, 0.5, ALU.mult, ALU.add, [T_('a')], [T_('a')])
            P.op('dve', lambda e: e.tensor_tensor_scan(out=cs, data0=smask, data1=sig, initial=0.0,
                                                        op0=ALU.mult, op1=ALU.add), ['smask', T_('sig')], [T_('cs')])
            ACT(winc, cs, AF.Exp, [T_('cs')], [T_('winc')], scale=-C0)
            ACT(winv, cs, AF.Exp, [T_('cs')], [T_('winv')], scale=C0)
            TT('dve', sig, cs, sig, ALU.subtract, [T_('cs'), T_('sig')], [T_('sig')])
            ACT(sig, sig, AF.Exp, [T_('sig')], [T_('sig')], scale=-C0)
            ACT(WC[:, hp, tb * 8:(tb + 1) * 8], winc.rearrange("p (c t) -> p c t", t=64)[:, :, 63], AF.Copy,
                [T_('winc')], ['WC' + u_])
            TT('pool', AR[:, hp, 1, sl], r_, winc, ALU.mult, [T_('r'), T_('winc')], ['ARr' + u_])
            ACT(winc, k_, AF.Copy, [T_('k'), 'par'], [T_('winc')], scale=par[:, PC_KK + hp:PC_KK + hp + 1])
            TT('dve', sqb_, winc, winc, ALU.mult, [T_('winc')], [T_('sqb')])
            MM(banks[6][:, :], bonesB, sqb_, ['cst', T_('sqb')], [BT(6)])

        def prepB_(hp, tb, Wx, T_, u_, sl, hc):
            r_, k_, v_, sig, a_, cs, winc, winv, t1 = (Wx[n] for n in ('r', 'k', 'v', 'sig', 'a', 'cs', 'winc', 'winv', 't1'))
            sqb_, rkb_ = Wx['sqb'], Wx['rkb']
            TS('dve', t1, banks[6][:, :], 1e-24, None, ALU.max, ALU.bypass, [BT(6)], [T_('t1')])
            ACT(t1, t1, AF.Ln, [T_('t1')], [T_('t1')])
            ACT(t1, t1, AF.Exp, [T_('t1')], [T_('t1')], scale=-0.5)
            TT('dve', winc, winc, t1, ALU.mult, [T_('winc'), T_('t1')], [T_('winc')])
            STT(AR[:, hp, 0, sl], winc, -1.0, sig, ALU.mult, ALU.mult, [T_('winc'), T_('sig')], ['ARa' + u_])
            TS('dve', t1, a_, par[:, PC_KA + hp:PC_KA + hp + 1], omka[:, hp:hp + 1], ALU.mult, ALU.add,
               [T_('a'), 'par', 'omka'], [T_('t1')])
            TT('pool', k_, k_, t1, ALU.mult, [T_('k'), T_('t1')], [T_('k')])
            TT('dve', t1, a_, winc, ALU.mult, [T_('a'), T_('winc')], [T_('t1')])
            TT('pool', BK[:, hp, 0, sl], t1, winv, ALU.mult, [T_('t1'), T_('winv')], ['BKb' + u_])
            TT('pool', BK[:, hp, 1, sl], k_, winv, ALU.mult, [T_('k'), T_('winv')], ['BKk' + u_])
            STT(rkb_, r_, par[:, PC_RK + hp:PC_RK + hp + 1], k_, ALU.mult, ALU.mult, [T_('r'), T_('k'), 'par'], [T_('rkb')])
            MM(banks[7][:, :], bonesB, rkb_, ['cst', T_('rkb')], [BT(7)])
            TT('dve', VB[:, hp, 1, sl], banks[7][:, :], v_, ALU.mult, [BT(7), T_('v')], ['VBb' + u_])
            ACT(VB[:, hp, 0, sl], v_, AF.Copy, [T_('v')], ['VBv' + u_])

        wsets = {}

        def part1(hp, tb):
            if tb == 0:
                wsets[hp] = [load_w(2 + hp * 3 + ti) for ti in range(3)]
            ws = wsets[hp]
            Wx, sx = wset(hp, tb)
            for ti, nm in enumerate(('r', 'k', 'v')):
                proj(ws[ti][0], ws[ti][1], tb, ti)
                shift_mix(nm, ti, tb, PC_MU + ti * 4 + hp, Wx[nm], nm + sx)

        its = [(hp, tb) for hp in range(3) for tb in range(4)]
        part1(*its[0])
        prep(its[0][0], its[0][1], 'A')
        for n, (hp, tb) in enumerate(its):
            la = P.capture(lambda: prep(hp, tb, 'B'))
            lb = []
            if n + 1 < len(its):
                hp2, tb2 = its[n + 1]
                lb = P.capture(lambda: (part1(hp2, tb2), prep(hp2, tb2, 'A')))
            P.replay(la, lb)
        P.barrier()
        for tb in range(4):
            part1(3, tb)
            prep(3, tb)
        for hp in range(4):
            if hp in (0, 3):
                dump('AR%d' % hp, AR[:, hp, :, :].rearrange("p x t -> p (x t)"), [128, 2 * T], BF16, reads=['AR'])
                dump('BK%d' % hp, BK[:, hp, :, :].rearrange("p x t -> p (x t)"), [128, 2 * T], BF16, reads=['BK'])
                dump('VB%d' % hp, VB[:, hp, :, :].rearrange("p x t -> p (x t)"), [128, 2 * T], BF16, reads=['VB'])
        for nm_ in ('sig', 'a', 'cs', 'winc', 'winv', 't1'):
            dump('w_' + nm_, W[nm_], [128, 512], F32, reads=[nm_])
        dump('hpar', hpar, [128, 8], F32, reads=['hpar'])
        dump('par', par[:, 0:NPAR], [128, NPAR], F32, reads=['par'])
        dump('WC', WC[:, :, :].rearrange("p h c -> p (h c)"), [128, 128], F32, reads=['WC'])
        if stop == 'B':
            P.finish()
            return nc, dbg_out
        P.barrier()

        cs0, cs1 = work0, work0 + 2050
        csets = []
        for base_ in (cs0, cs1):
            csets.append(dict(b=A.bf(512, at=base_), c=A.bf(512, at=base_ + 256), t1=A.f32(512, at=base_ + 512),
                              chb=A.f32(514, at=base_ + 1024)))
        assert cs1 + 1538 <= work0 + 3968
        cits = [(j, tb) for j in range(4) for tb in range(4)]
        cws = {}

        def conv_it(n):
            j, tb = cits[n]
            if tb == 0:
                cws[j] = [load_w(14 + j * 3 + ti) for ti in range(3)]
            ws = cws[j]
            S_ = csets[n % 2]
            Sp = csets[(n + 1) % 2]
            sx = 'c%d' % (n % 2)
            px = 'c%d' % ((n + 1) % 2)
            sl = slice(tb * 512, (tb + 1) * 512)
            bo = 4 * (n % 2)
            for ti in range(3):
                proj(ws[ti][0], ws[ti][1], tb, bo + ti)
            ACT(S_['b'], banks[bo][:, :], AF.Copy, [BT(bo)], ['b' + sx])
            ACT(S_['c'], banks[bo + 1][:, :], AF.Copy, [BT(bo + 1)], ['c' + sx])
            chb_ = S_['chb']
            if tb == 0:
                P.op('pool', lambda e: e.memset(chb_[:, 0:2], 0.0), [], ['cy' + sx])
            TT('dve', chb_[:, 2:514], banks[bo + 2][:, :], S_['c'], ALU.mult, [BT(bo + 2), 'c' + sx], ['chb' + sx])
            def carry():
                if tb < 3:
                    ACT(Sp['chb'][:, 0:2], chb_[:, 512:514], AF.Copy, ['chb' + sx], ['cy' + px])
            if n % 2 == 0:
                carry()
            cw = lambda tap: par[:, PC_CW + tap * 4 + j:PC_CW + tap * 4 + j + 1]
            t1_ = S_['t1']
            ACT(t1_, chb_[:, 2:514], AF.Copy, ['chb' + sx, 'par'], ['t1' + sx], scale=cw(2))
            STT(t1_, chb_[:, 1:513], cw(1), t1_, ALU.mult, ALU.add, ['chb' + sx, 'cy' + sx, 't1' + sx, 'par'], ['t1' + sx])
            STT(t1_, chb_[:, 0:512], cw(0), t1_, ALU.mult, ALU.add, ['chb' + sx, 'cy' + sx, 't1' + sx, 'par'], ['t1' + sx])
            TT('pool', ycT[:, j, sl], t1_, S_['b'], ALU.mult, ['t1' + sx, 'b' + sx], ['ycT%d%d' % (j, tb)])
            if n % 2 == 1:
                carry()

        for n in range(0, len(cits), 2):
            la = P.capture(lambda: conv_it(n))
            lb = P.capture(lambda: conv_it(n + 1))
            P.replay(la, lb)
        P.barrier()
        dump('ycT', ycT[:, 0, :], [128, T], BF16, reads=['ycT'])
        if stop == 'C':
            P.finish()
            return nc, dbg_out
        P.barrier()

        maskS = cstb[:, 256:384]
        maskL = cstb[:, 384:448]
        ident2 = cstb[:, 448:512]
        r1, r2, r3 = [base0], [pers1], [work0]

        def cbf(reg, n):
            ap = A.bf(n, at=reg[0])
            reg[0] += (n + 1) // 2
            return ap

        def cf(reg, n):
            ap = A.f32(n, at=reg[0])
            reg[0] += n
            return ap

        h128 = lambda ap: ap.rearrange("p (h x) -> p h x", x=128)
        h64 = lambda ap: ap.rearrange("p (h x) -> p h x", x=64)
        hcx = lambda ap: ap.rearrange("p (h c x) -> p h c x", h=4, c=2)
        yrT = cbf(r1, 4 * T).rearrange("p (j t) -> p j t", t=T)
        AL = [h128(cbf(r1, 1024)) for _ in range(2)]
        RB = [h128(cbf(r1, 1024)) for _ in range(2)]
        Ktm = [h64(cbf(r1, 512)) for _ in range(2)]
        Vtm = [h64(cbf(r1, 512)) for _ in range(2)]
        S2m = [h128(cbf(r1, 1024)) for _ in range(2)]
        AUs = [h128(cbf(r1, 1024)) for _ in range(2)]
        assert r1[0] <= base0 + 9216, r1[0]
        NY = [h128(cbf(r2, 1024)) for _ in range(2)]
        NTs = [h64(cbf(r2, 512)) for _ in range(2)]
        Yf = h64(cbf(r2, 512))
        RhT = [hcx(cbf(r2, 512)) for _ in range(2)]
        GT = [hcx(cbf(r2, 512)) for _ in range(2)]
        O0s = [hcx(cf(r2, 512)) for _ in range(2)]
        Hb = [h64(cbf(r2, 256)) for _ in range(2)]
        assert r2[0] <= pers1 + 4096 + 256, r2[0]
        Qs = [hcx(cf(r3, 512)) for _ in range(2)]
        ot = cf(r3, 512)
        osq = cf(r3, 512)
        dd = cf(r3, 512)
        rs = cf(r3, 512)
        htmp = cf(r3, 256)
        ob = cbf(r3, 512)
        osqb = cbf(r3, 512)
        assert r3[0] <= work0 + 3968, r3[0]
        WC4 = A.f32(128, at=pers1 - 128).rearrange("p (h c o) -> p h c o", h=4, o=1)
        lnw4 = par[:, PC_LW:PC_LW + 4].rearrange("p (h o) -> p h o", o=1)
        lnb4 = par[:, PC_LB:PC_LB + 4].rearrange("p (h o) -> p h o", o=1)

        def pbf(b):
            return banks[b][:, :].bitcast(BF16)

        hv = lambda ap, hd: ap.rearrange("p (a b) x -> p a b x", b=2)[:, :, hd, :]
        crs = (slice(0, 64), slice(64, 128))
        hrs = crs
        NTB = (4, 7)
        QB = (4, 7)
        OB = (5, 6)
        G4 = [(cp, hd) for cp in range(2) for hd in range(2)]
        gt_ = lambda name, cp, hd: '%s_%d%d' % (name, cp, hd)
        both = lambda name, hd: [gt_(name, 0, hd), gt_(name, 1, hd)]
        allg = lambda name: [gt_(name, cp, hd) for cp, hd in G4]
        bP = lambda cp, hd: 'bP%d' % (cp * 2 + hd)
        bQ = lambda cp, hd: 'bQ%d_%d' % (cp, hd)
        bO = lambda cp, hd: 'bO%d_%d' % (cp, hd)
        m64 = lambda m: m.rearrange("p (o x) -> p o x", o=1).to_broadcast([128, 4, 64])
        mS1, mS2, mL4, id4 = m64(maskS[:, 0:64]), m64(maskS[:, 64:128]), m64(maskL), m64(ident2)
        mSf = maskS.rearrange("p (o x) -> p o x", o=1).to_broadcast([128, 4, 128])
        P.op('pool', lambda e: e.memset(Hb[0][:, :, :], 0.0), [], ['Hb0_0', 'Hb0_1'])

        S1B, S2B, STB = (0, 3), (1, 2), (0, 3)
        PBK = lambda cp, hd: cp * 2 + hd
        QBK = lambda cp, hd: 4 + cp * 2 + hd
        bk = lambda n: ['bk%d' % n]

        def st_T(i):
            par = i % 2
            tk = slice(i * 128, (i + 1) * 128)
            wb5, wb6 = bk(5), bk(6)
            for hp in range(4):
                P.op('pe', lambda e, hp=hp: e.transpose(out=pbf(5)[:, hp * 128:(hp + 1) * 128], in_=AR[:, hp, 0, tk],
                                                        identity=identB), ['AR', 'cst'], wb5, inc=False)
                P.op('pe', lambda e, hp=hp: e.transpose(out=pbf(5)[:, 512 + hp * 128:512 + (hp + 1) * 128],
                                                        in_=BK[:, hp, 0, tk], identity=identB), ['BK', 'cst'], wb5, inc=(hp == 3))
            for hp in range(4):
                P.op('pe', lambda e, hp=hp: e.transpose(out=pbf(6)[:, hp * 128:(hp + 1) * 128], in_=BK[:, hp, 1, tk],
                                                        identity=identB), ['BK', 'cst'], wb6, inc=False)
                P.op('pe', lambda e, hp=hp: e.transpose(out=pbf(6)[:, 512 + hp * 128:512 + (hp + 1) * 128],
                                                        in_=VB[:, hp, 0, tk], identity=identB), ['VB', 'cst'], wb6, inc=(hp == 3))
            ACT(AL[par][:, :, 0:64], h64(pbf(5)[:, 0:512]), AF.Copy, wb5, allg('ALa%d' % par))
            ACT(RB[par][:, :, 64:128], h64(pbf(5)[:, 512:1024]), AF.Copy, wb5, allg('RBb%d' % par))
            ACT(Ktm[par][:, :, :], h64(pbf(6)[:, 0:512]), AF.Copy, wb6, allg('Ktm%d' % par))
            ACT(Vtm[par][:, :, :], h64(pbf(6)[:, 512:1024]), AF.Copy, wb6, allg('Vtm%d' % par))

        def st_S(i, hd):
            par = i % 2
            hr = hrs[hd]
            b1, b2, b3 = S1B[hd], S2B[hd], NTB[hd]
            t1, t2, t3 = bk(b1), bk(b2), bk(b3)
            for hp in range(4):
                for cp in range(2):
                    cr = crs[cp]
                    t64 = slice(i * 128 + cp * 64, i * 128 + cp * 64 + 64)
                    last = (hp == 3 and cp == 1)
                    cs_ = slice(hp * 128, hp * 128 + 128)
                    MM(banks[b1][cr, cs_], BK[hr, hp, 0, t64], AR[hr, hp, :, t64], ['BK', 'AR'], t1, inc=last)
                    MM(banks[b2][cr, cs_], BK[hr, hp, 1, t64], AR[hr, hp, :, t64], ['BK', 'AR'], t2, inc=last)
                    MM(banks[b3][cr, hp * 64:hp * 64 + 64], AR[hr, hp, 0, t64], BK[hr, hp, 0, t64], ['BK', 'AR'], t3, inc=last)
            TT('dve', hv(NY[0], hd)[:, :, 0:64], h128(banks[b1][:, :])[:, :, 0:64], mS1, ALU.mult, t1 + ['cst'], both('NY0n', hd))
            TT('dve', hv(NTs[0], hd), h64(banks[b3][:, 0:256]), mL4, ALU.mult, t3 + ['cst'], both('NT0', hd))
            TT('pool', hv(NY[1], hd)[:, :, 64:128], hv(NY[0], hd)[:, :, 0:64], id4, ALU.add, both('NY0n', hd) + ['cst'], both('NY1y', hd))
            TT('dve', hv(RB[par], hd)[:, :, 0:64], h128(banks[b1][:, :])[:, :, 64:128], mS2, ALU.mult, t1 + ['cst'], both('RBr%d' % par, hd))
            TT('dve', hv(S2m[par], hd), h128(banks[b2][:, :]), mSf, ALU.mult, t2 + ['cst'], both('S2m%d' % par, hd))

        def st_N(i, lv, cp, hd):
            cr = crs[cp]
            pb, qb = PBK(cp, hd), QBK(cp, hd)
            tP, tQ = bk(pb), bk(qb)
            qc = lambda hp: slice(hp * 64, hp * 64 + 64)
            if lv == 0:
                rn, rt = gt_('NY0n', cp, hd), gt_('NT0', cp, hd)
                for hp in range(4):
                    h = 2 * hp + hd
                    MM(banks[pb][cr, hp * 128:hp * 128 + 64], NTs[0][cr, h, :], NY[0][cr, h, 0:64], [rn, rt], tP, inc=(hp == 3))
                    MM(banks[qb][cr, qc(hp)], NY[0][cr, h, 0:64], NTs[0][cr, h, :], [rn, rt], tQ, inc=(hp == 3))
                ACT(hv(NY[1], hd)[cr, :, 0:64], h128(banks[pb][cr, :])[:, :, 0:64], AF.Copy, tP, [gt_('NY1n', cp, hd)])
                ACT(hv(NTs[1], hd)[cr, :, :], h64(banks[qb][cr, 0:256]), AF.Copy, tQ, [gt_('NT1', cp, hd)])
                return
            a_, b_ = lv % 2, (lv + 1) % 2
            NYa, NYb, NTa, NTb = NY[a_], NY[b_], NTs[a_], NTs[b_]
            rn, ry, rt = gt_('NY%dn' % a_, cp, hd), gt_('NY%dy' % a_, cp, hd), gt_('NT%d' % a_, cp, hd)
            wn, wy, wt = gt_('NY%dn' % b_, cp, hd), gt_('NY%dy' % b_, cp, hd), gt_('NT%d' % b_, cp, hd)
            for hp in range(4):
                h = 2 * hp + hd
                if lv < 5:
                    MM(banks[pb][cr, hp * 128:hp * 128 + 128], NTa[cr, h, :], NYa[cr, h, :], [rn, ry, rt], tP, inc=(hp == 3))
                    MM(banks[qb][cr, qc(hp)], NYa[cr, h, 0:64], NTa[cr, h, :], [rn, rt], tQ, inc=(hp == 3))
                else:
                    MM(banks[pb][cr, hp * 128 + 64:hp * 128 + 128], NTa[cr, h, :], NYa[cr, h, 64:128], [ry, rt], tP, inc=(hp == 3))
            bv = h128(banks[pb][cr, :])
            if lv < 5:
                ACT(hv(NYb, hd)[cr, :, 0:64], bv[:, :, 0:64], AF.Copy, tP, [wn])
                TT('dve', hv(NYb, hd)[cr, :, 64:128], bv[:, :, 64:128], hv(NYa, hd)[cr, :, 64:128], ALU.add, tP + [ry], [wy])
                ACT(hv(NTb, hd)[cr, :, :], h64(banks[qb][cr, 0:256]), AF.Copy, tQ, [wt])
            else:
                TT('dve', hv(Yf, hd)[cr, :, :], bv[:, :, 64:128], hv(NYa, hd)[cr, :, 64:128], ALU.add, tP + [ry], [gt_('Yf', cp, hd)])

        def st_L(i, cp, hd):
            par = i % 2
            cr = crs[cp]
            qb = QBK(cp, hd)
            tQ = bk(qb)
            for hp in range(4):
                h = 2 * hp + hd
                MM(banks[qb][cr, hp * 64:hp * 64 + 64], S2m[par][cr, h, 0:64], Vtm[par][cr, h, :],
                   [gt_('S2m%d' % par, cp, hd), gt_('Vtm%d' % par, cp, hd)], tQ, inc=(hp == 3))
            ACT(hv(AL[par], hd)[cr, :, 64:128], h64(banks[qb][cr, 0:256]), AF.Copy, tQ, [gt_('ALl%d' % par, cp, hd)])

        def st_AU(i, cp, hd):
            par = i % 2
            cr = crs[cp]
            pb = PBK(cp, hd)
            tP = bk(pb)
            for hp in range(4):
                h = 2 * hp + hd
                MM(banks[pb][cr, hp * 128:hp * 128 + 128], Yf[cr, h, :], AL[par][cr, h, :],
                   [gt_('Yf', cp, hd), gt_('ALa%d' % par, cp, hd), gt_('ALl%d' % par, cp, hd)], tP, inc=(hp == 3))
            ACT(hv(AUs[par], hd)[cr, :, :], h128(banks[pb][cr, :]), AF.Copy, tP, [gt_('AUs%d' % par, cp, hd)])

        def st_E(i, cp, hd):
            par = i % 2
            cr, hr = crs[cp], hrs[hd]
            pb = PBK(cp, hd)
            tP = bk(pb)
            t64 = slice(i * 128 + cp * 64, i * 128 + cp * 64 + 64)
            for hp in range(4):
                h = 2 * hp + hd
                MM(banks[pb][hr, hp * 128:hp * 128 + 128], AUs[par][cr, h, 0:64], RB[par][cr, h, :],
                   [gt_('AUs%d' % par, cp, hd), gt_('RBr%d' % par, cp, hd), gt_('RBb%d' % par, cp, hd)], tP, inc=(hp == 3))
            ev = h128(banks[pb][hr, :])
            TT('dve', RhT[par][hr, :, cp, :], ev[:, :, 0:64], AR[hr, :, 1, t64], ALU.add, tP + ['AR'], [gt_('RhT%d' % par, cp, hd)])
            idb = ident2[hr, :].rearrange("p (o x) -> p o x", o=1).to_broadcast([64, 4, 64])
            TT('dve', GT[par][hr, :, cp, :], ev[:, :, 64:128], idb, ALU.add, tP + ['cst'], [gt_('GT%d' % par, cp, hd)])

        def st_OQ(i, cp, hd):
            par = i % 2
            cr, hr = crs[cp], hrs[hd]
            c = 2 * i + cp
            ob_, qb = PBK(cp, hd), QBK(cp, hd)
            tO, tQ = bk(ob_), bk(qb)
            rA = [gt_('AUs%d' % par, cp, hd), gt_('RBr%d' % par, cp, hd), gt_('RBb%d' % par, cp, hd)]
            rV = [gt_('Vtm%d' % par, cp, hd), gt_('S2m%d' % par, cp, hd), gt_('Ktm%d' % par, cp, hd)]
            for hp in range(4):
                h = 2 * hp + hd
                col = slice(hp * 64, hp * 64 + 64)
                MM(banks[ob_][hr, col], AUs[par][cr, h, 64:128], RB[par][cr, h, 0:64], rA, tO, start=True, stop=False, inc=False)
                MM(banks[ob_][hr, col], Vtm[par][cr, h, :], S2m[par][cr, h, 64:128], rV, tO, start=False, stop=True, inc=(hp == 3))
                MM(banks[qb][hr, col], RB[par][cr, h, 64:128], AUs[par][cr, h, 64:128], rA, tQ, start=True, stop=False, inc=False)
                MM(banks[qb][hr, col], Ktm[par][cr, h, :], Vtm[par][cr, h, :], rV, tQ, start=False, stop=True, inc=(hp == 3))
            ACT(O0s[par][hr, :, cp, :], h64(banks[ob_][hr, 0:256]), AF.Copy, tO, [gt_('O0s%d' % par, cp, hd)])
            wc1 = WC4[hr, :, c, :].to_broadcast([64, 4, 64])
            TT('dve', Qs[par][hr, :, cp, :], h64(banks[qb][hr, 0:256]), wc1, ALU.mult, tQ + ['WC'], [gt_('Qs%d' % par, cp, hd)])

        o4 = hcx(ot)
        h3 = h64(htmp)

        def st_ST(i, cp, hd):
            par = i % 2
            hr = hrs[hd]
            c = 2 * i + cp
            Hc, Hn = Hb[c % 2], Hb[(c + 1) % 2]
            tc_, tn = 'Hb%d_%d' % (c % 2, hd), 'Hb%d_%d' % ((c + 1) % 2, hd)
            sb = STB[hd]
            tB = bk(sb)
            for hp in range(4):
                MM(banks[sb][hr, hp * 64:hp * 64 + 64], Hc[hr, hp, :], RhT[par][hr, hp, cp, :],
                   [tc_, gt_('RhT%d' % par, cp, hd)], tB, inc=False)
                MM(banks[sb][hr, 256 + hp * 64:256 + hp * 64 + 64], GT[par][hr, hp, cp, :], Hc[hr, hp, :],
                   [tc_, gt_('GT%d' % par, cp, hd)], tB, inc=(hp == 3))
            b7 = banks[sb][hr, :].rearrange("p (z h x) -> p z h x", z=2, h=4)
            TT('dve', o4[hr, :, cp, :], b7[:, 0, :, :], O0s[par][hr, :, cp, :], ALU.add, tB + [gt_('O0s%d' % par, cp, hd)], ['ot_%d' % hd])
            wc1 = WC4[hr, :, c, :].to_broadcast([64, 4, 64])
            TT('dve', h3[hr, :, :], b7[:, 1, :, :], wc1, ALU.mult, tB + ['WC'], ['htmp_%d' % hd])
            TT('pool', Hn[hr, :, :], h3[hr, :, :], Qs[par][hr, :, cp, :], ALU.add, ['htmp_%d' % hd, gt_('Qs%d' % par, cp, hd)], [tn])

        def st_GN(i):
            tk = slice(i * 128, (i + 1) * 128)
            rot = ['ot_0', 'ot_1']
            ACT(ob, ot, AF.Copy, rot, ['ob'])
            TT('pool', osqb, ot, ot, ALU.mult, rot, ['osqb'])
            MM(banks[1][:, :], bonesB, ob, ['cst', 'ob'], bk(1))
            MM(banks[2][:, :], bonesB, osqb, ['cst', 'osqb'], bk(2))
            TS('dve', rs, banks[1][:, :], 1.0 / 64, None, ALU.mult, ALU.bypass, bk(1), ['rs'])
            TT('dve', dd, ot, rs, ALU.subtract, rot + ['rs'], ['dd'])
            TT('pool', rs, rs, rs, ALU.mult, ['rs'], ['rs'])
            TS('dve', osq, banks[2][:, :], 1.0 / 64, 64e-5, ALU.mult, ALU.add, bk(2), ['osq'])
            TT('pool', rs, osq, rs, ALU.subtract, ['osq', 'rs'], ['rs'])
            ACT(rs, rs, AF.Ln, ['rs'], ['rs'])
            ACT(rs, rs, AF.Exp, ['rs'], ['rs'], scale=-0.5)
            for hp in range(4):
                MM(banks[1][:, hp * 128:(hp + 1) * 128], gupb[:, hp * 128:(hp + 1) * 128], sg[:, tk], ['gupb', 'sg'], bk(1),
                   inc=(hp == 3))
            TT('pool', dd, dd, rs, ALU.mult, ['dd', 'rs'], ['dd'])
            d3 = dd.rearrange("p (h x) -> p h x", x=128)
            TT('pool', d3, d3, lnw4.to_broadcast([128, 4, 128]), ALU.mult, ['dd', 'par'], ['dd'])
            TT('pool', d3, d3, lnb4.to_broadcast([128, 4, 128]), ALU.add, ['dd', 'par'], ['dd'])
            TT('pool', d3, d3, VB[:, :, 1, tk], ALU.add, ['dd', 'VB'], ['dd'])
            TT('dve', yrT[:, :, tk], d3, h128(banks[1][:, :]), ALU.mult, ['dd'] + bk(1), ['yrT'])

        def P_stages(i):
            st = [lambda: st_T(i), lambda: (st_S(i, 0), st_S(i, 1))]
            for lv in range(6):
                st.append(lambda lv=lv: [st_N(i, lv, cp, hd) for cp, hd in G4])
            st.append(lambda: [st_L(i, cp, hd) for cp, hd in G4])
            st.append(lambda: [st_AU(i, cp, hd) for cp, hd in G4])
            st.append(lambda: [st_E(i, cp, hd) for cp, hd in G4])
            st.append(lambda: [st_OQ(i, cp, hd) for cp, hd in G4])
            return st

        def Q_stages(i):
            return [lambda: (st_ST(i, 0, 0), st_ST(i, 0, 1)), lambda: (st_ST(i, 1, 0), st_ST(i, 1, 1)), lambda: st_GN(i)]

        import os as _os
        if _os.environ.get('NOPIPE'):
            _ds = int(_os.environ.get('DSTOP', '999'))
            for i in range(NTILE):
                for k_, f in enumerate(P_stages(i) + Q_stages(i)):
                    f()
                    if i == 0 and k_ == _ds:
                        P.finish()
                        return nc, dbg_out
        else:
            for f in P_stages(0):
                f()
            for i in range(NTILE):
                ps = P_stages(i + 1) if i + 1 < NTILE else []
                qs = Q_stages(i)
                _k = int(_os.environ.get('PIPEK', '1'))
                slots = None
                for k, f in enumerate(ps):
                    f()
                    if slots is None:
                        if k == _k:
                            for q_ in qs:
                                q_()
                    elif k in slots:
                        qs[slots[k]]()
                if not ps:
                    for f in qs:
                        f()
        dump('yrT', yrT[:, 0, :], [128, T], BF16, reads=['yrT'])
        if stop == 'D':
            P.finish()
            return nc, dbg_out
        P.barrier()
        h1 = A.f32(16 * D, at=pers0).rearrange("p (i d) -> p i d", d=D)
        o1 = pers0 + 16 * D
        wob = A.bf(8 * D, at=o1).rearrange("p (k c) -> p k c", c=D)
        wos = [A.f32(D, at=o1 + 4096 + s * D) for s in range(2)]
        xs = [A.f32(D, at=o1 + 4096 + 2 * D + s * D) for s in range(2)]
        assert o1 + 4096 + 4 * D <= pers0 + 24576
        for k in range(8):
            P.dma(wos[k % 2], wout_d[:, k * D:(k + 1) * D], writes=['wos%d' % (k % 2)])
            P.op('pool', lambda e, k=k: e.tensor_copy(out=wob[:, k, :], in_=wos[k % 2]), ['wos%d' % (k % 2)], ['wob'])
        for i in range(NTILE):
            tk = slice(i * 128, (i + 1) * 128)
            P.dma(xs[i % 2], x_d[tk, :], writes=['xs%d' % (i % 2)])
            for half in range(2):
                b = (2 * i + half) % 4
                for k in range(8):
                    lhs = ycT[:, k, tk] if k < 4 else yrT[:, k - 4, tk]
                    MM(banks[b][:, :], lhs, wob[:, k, half * 512:(half + 1) * 512], ['ycT', 'yrT', 'wob'], [BT(b)],
                       start=(k == 0), stop=(k == 7), inc=(k == 7))
                TT('dve', h1[:, i, half * 512:(half + 1) * 512], banks[b][:, :], xs[i % 2][:, half * 512:(half + 1) * 512],
                   ALU.add, [BT(b), 'xs%d' % (i % 2)], ['h1_%d' % i])
        dump('h1', h1[:, 0, :], [128, D], F32, reads=['h1_0'])
        if stop == 'E':
            P.finish()
            return nc, dbg_out
        P.barrier()

        stg = [A.f32(2048, at=o1 + s * 2048) for s in range(3)]
        wb = [[A.bf(2048, at=o1 + 6144 + (s2 * 3 + s) * 1024) for s in range(3)] for s2 in range(2)]
        assert o1 + 6144 + 6144 <= work0 - 512
        srcs = (wg_d, wu_d, wd_d)

        def moe_dma(ex):
            for s in range(3):
                P.dma(stg[s], srcs[s][ex, :, :], writes=['stg%d' % s])

        def moe_cvt(ex):
            s2 = ex % 2
            for s in range(3):
                if s != 1:
                    P.op('pool', lambda e, s=s, s2=s2: e.tensor_copy(out=wb[s2][s], in_=stg[s]), ['stg%d' % s], ['wb%d_%d' % (s2, s)])
                else:
                    ACT(wb[s2][s], stg[s], AF.Copy, ['stg%d' % s], ['wb%d_%d' % (s2, s)])

        moe_dma(0)
        moe_cvt(0)
        u2T = A.bf(8 * T, at=base0).rearrange("p (k t) -> p k t", t=T)
        P.dma(gbc, g2_d.partition_broadcast(128), writes=['gbc'])
        rms_stats(lambda i: h1[:, i, :], 'h1_')
        norm_transpose(lambda i: h1[:, i, :], 'h1_', u2T, 'u2T')
        rtop = [work0 + 2700]

        def ralloc(n):
            ap = A.f32(n, at=rtop[0])
            rtop[0] += n
            return ap

        wrs = ralloc(288)
        wrb = A.bf(288, at=rtop[0]).rearrange("p (k c) -> p k c", c=36)
        rtop[0] += 144
        brbc = ralloc(36)
        lg = ralloc(16 * 36).rearrange("p (i c) -> p i c", c=36)
        gmax = ralloc(16)
        gsh = ralloc(64).rearrange("p (i c) -> p i c", c=4)
        gsum = ralloc(16)
        gp = ralloc(16)
        oh = ralloc(64).rearrange("p (i c) -> p i c", c=4)
        elm = ralloc(512).rearrange("p (i c) -> p i c", c=32)
        elm2 = ralloc(512).rearrange("p (i c) -> p i c", c=32)
        e1 = ralloc(512).rearrange("p (i c) -> p i c", c=32)
        e2 = ralloc(512).rearrange("p (i c) -> p i c", c=32)
        comb = ralloc(512).rearrange("p (i c) -> p i c", c=32)
        m1 = ralloc(16)
        m2 = ralloc(16)
        dm = ralloc(16)
        p1 = ralloc(16)
        p2 = ralloc(16)
        assert rtop[0] <= NW
        P.dma(wrs, wr_d[:, :], writes=['wrs'])
        P.dma(brbc, br_d.partition_broadcast(128), writes=['brbc'])
        P.op('pool', lambda e: e.tensor_copy(out=wrb[:, :, :].rearrange("p k c -> p (k c)"), in_=wrs), ['wrs'], ['wrb'])
        for i in range(NTILE):
            tk = slice(i * 128, (i + 1) * 128)
            b = 4 + i // 8
            for k in range(8):
                MM(banks[b][:, (i % 8) * 36:(i % 8) * 36 + 36], u2T[:, k, tk], wrb[:, k, :], ['u2T', 'wrb'], [BT(b)],
                   start=(k == 0), stop=(k == 7), inc=(k == 7))
        bb3 = brbc.rearrange("p (o c) -> p o c", o=1).to_broadcast([128, 8, 36])
        for hb in range(2):
            TT('dve', lg[:, hb * 8:(hb + 1) * 8, :], banks[4 + hb][:, 0:288].rearrange("p (i c) -> p i c", c=36), bb3,
               ALU.add, [BT(4 + hb), 'brbc'], ['lg'])
        dump('lg', lg[:, :, :].rearrange("p i c -> p (i c)"), [128, 576], F32, reads=['lg'])
        gl = lg[:, :, 0:4]
        el = lg[:, :, 4:36]
        b3 = lambda ap, n: ap.rearrange("p (i o) -> p i o", o=1).to_broadcast([128, 16, n])
        P.op('dve', lambda e: e.tensor_reduce(out=gmax, in_=gl, axis=AX.X, op=ALU.max), ['lg'], ['gmax'])
        TT('dve', gsh, gl, b3(gmax, 4), ALU.subtract, ['lg', 'gmax'], ['gsh'])
        TT('dve', oh, gl, b3(gmax, 4), ALU.is_equal, ['lg', 'gmax'], ['oh'])
        ACT(gsh, gsh, AF.Exp, ['gsh'], ['gsh'])
        P.op('dve', lambda e: e.tensor_reduce(out=gsum, in_=gsh, axis=AX.X, op=ALU.add), ['gsh'], ['gsum'])
        P.op('dve', lambda e: e.reciprocal(out=gp, in_=gsum), ['gsum'], ['gp'])
        TS('dve', oh, oh, 1e30, -1e30, ALU.mult, ALU.add, ['oh'], ['oh'])
        TT('dve', elm.rearrange("p i (g x) -> p i g x", g=4), el.rearrange("p i (g x) -> p i g x", g=4),
           oh.rearrange("p i (g o) -> p i g o", o=1).to_broadcast([128, 16, 4, 8]), ALU.add, ['lg', 'oh'], ['elm'])
        P.op('dve', lambda e: e.tensor_reduce(out=m1, in_=elm, axis=AX.X, op=ALU.max), ['elm'], ['m1'])
        TT('dve', e1, elm, b3(m1, 32), ALU.is_equal, ['elm', 'm1'], ['e1'])
        STT(elm2, e1, -1e30, elm, ALU.mult, ALU.add, ['e1', 'elm'], ['elm2'])
        P.op('dve', lambda e: e.tensor_reduce(out=m2, in_=elm2, axis=AX.X, op=ALU.max), ['elm2'], ['m2'])
        TT('dve', e2, elm2, b3(m2, 32), ALU.is_equal, ['elm2', 'm2'], ['e2'])
        TT('dve', dm, m2, m1, ALU.subtract, ['m1', 'm2'], ['dm'])
        ACT(dm, dm, AF.Exp, ['dm'], ['dm'])
        TS('dve', p1, dm, 1.0, None, ALU.add, ALU.bypass, ['dm'], ['p1'])
        P.op('dve', lambda e: e.reciprocal(out=p1, in_=p1), ['p1'], ['p1'])
        TT('dve', p2, dm, p1, ALU.mult, ['dm', 'p1'], ['p2'])
        TT('dve', p1, p1, gp, ALU.mult, ['p1', 'gp'], ['p1'])
        TT('dve', p2, p2, gp, ALU.mult, ['p2', 'gp'], ['p2'])
        TT('dve', comb, e1, b3(p1, 32), ALU.mult, ['e1', 'p1'], ['comb'])
        TT('dve', e2, e2, b3(p2, 32), ALU.mult, ['e2', 'p2'], ['e2'])
        TT('dve', comb, comb, e2, ALU.add, ['comb', 'e2'], ['comb'])
        dump('comb', comb[:, :, :].rearrange("p i c -> p (i c)"), [128, 512], F32, reads=['comb'])
        if stop == 'F':
            P.finish()
            return nc, dbg_out
        P.barrier()

        m0 = base0 + 8192
        sil = [A.f32(512, at=m0 + s * 512) for s in range(2)]
        hid = [A.bf(512, at=m0 + 1024 + s * 256) for s in range(2)]
        assert m0 + 1536 <= pers0
        hid4 = [[A.bf(512, at=m0 + 1024 + (bp * 2 + s) * 256) for s in range(2)] for bp in range(2)]
        assert m0 + 2048 <= pers0

        def moe_G(ex, tb):
            s2 = ex % 2
            bp = (ex * 4 + tb) % 2
            wgb = wb[s2][0].rearrange("p (k c) -> p k c", c=256)
            wub = wb[s2][1].rearrange("p (k c) -> p k c", c=256)
            tg, tu = 'wb%d_0' % s2, 'wb%d_1' % s2
            tsl = slice(tb * 512, (tb + 1) * 512)
            for dch in range(2):
                dc = slice(dch * 128, (dch + 1) * 128)
                bg, bu = 2 * dch, 2 * dch + 1
                for k in range(8):
                    MM(banks[bg][:, :], wgb[:, k, dc], u2T[:, k, tsl], [tg, 'u2T'], [BT(bg)], start=(k == 0), stop=(k == 7), inc=(k == 7))
                for k in range(8):
                    MM(banks[bu][:, :], wub[:, k, dc], u2T[:, k, tsl], [tu, 'u2T'], [BT(bu)], start=(k == 0), stop=(k == 7), inc=(k == 7))
                ACT(sil[dch], banks[bg][:, :], AF.Silu, [BT(bg)], ['sil%d' % dch])
                TT('dve', hid4[bp][dch], sil[dch], banks[bu][:, :], ALU.mult, ['sil%d' % dch, BT(bu)], ['hid%d_%d' % (bp, dch)])

        tmpE = [A.f32(512, at=work0 + q_ * 512) for q_ in range(4)]
        tctr = [0]
        OFFL = ((0, 1), (2, 0), (3, 0))

        def final_group(g):
            gs = slice(4 * g, 4 * g + 4)
            for i in range(4 * g, 4 * g + 4):
                P.op('act', lambda e, i=i: e.activation(out=junk, in_=h1[:, i, :], func=AF.Square, accum_out=ss[:, i:i + 1]),
                     reads=['h1_%d' % i], writes=['junk', 'ssg%d' % g])
            TS('dve', rstd[:, gs], ss[:, gs], 1.0 / D, 1e-6, ALU.mult, ALU.add, ['ssg%d' % g], ['rstdg%d' % g])
            ACT(rstd[:, gs], rstd[:, gs], AF.Ln, ['rstdg%d' % g], ['rstdg%d' % g])
            ACT(rstd[:, gs], rstd[:, gs], AF.Exp, ['rstdg%d' % g], ['rstdg%d' % g], scale=-0.5)
            for i in range(4 * g, 4 * g + 4):
                STT(h1[:, i, :], h1[:, i, :], rstd[:, i:i + 1], gbc, ALU.mult, ALU.mult, ['h1_%d' % i, 'rstdg%d' % g, 'gbc'], ['h1_%d' % i])
                P.dma(out_d[i * 128:(i + 1) * 128, :], h1[:, i, :], reads=['h1_%d' % i])

        def moe_D(ex, tb):
            s2 = ex % 2
            bp = (ex * 4 + tb) % 2
            wdb = wb[s2][2].rearrange("p (d c) -> p d c", c=D)
            td = 'wb%d_2' % s2
            for ti in range(4):
                i = tb * 4 + ti
                for half in range(2):
                    b = 4 + (ti % 2) * 2 + half
                    for dch in range(2):
                        MM(banks[b][:, :], hid4[bp][dch][:, ti * 128:(ti + 1) * 128], wdb[:, dch, half * 512:(half + 1) * 512],
                           ['hid%d_%d' % (bp, dch), td], [BT(b)], start=(dch == 0), stop=(dch == 1), inc=(dch == 1))
                    hsl = h1[:, i, half * 512:(half + 1) * 512]
                    if (ti, half) in OFFL:
                        q_ = tctr[0] % 4
                        tctr[0] += 1
                        ACT(tmpE[q_], banks[b][:, :], AF.Copy, [BT(b), 'comb'], ['tmpE%d' % q_], scale=comb[:, i, ex:ex + 1])
                        TT('pool', hsl, hsl, tmpE[q_], ALU.add, ['tmpE%d' % q_, 'h1_%d' % i], ['h1_%d' % i])
                    else:
                        STT(hsl, banks[b][:, :], comb[:, i, ex:ex + 1], hsl, ALU.mult, ALU.add, [BT(b), 'comb', 'h1_%d' % i], ['h1_%d' % i])

        blocks = [(ex, tb) for ex in range(NE) for tb in range(4)]
        P.dma(gbc, g3_d.partition_broadcast(128), writes=['gbc'])
        moe_G(0, 0)
        for n, (ex, tb) in enumerate(blocks):
            if tb == 0 and ex + 1 < NE:
                moe_dma(ex + 1)
            if tb == 2 and ex + 1 < NE:
                moe_cvt(ex + 1)
            if n + 1 < len(blocks):
                moe_G(*blocks[n + 1])
            moe_D(ex, tb)
            if ex == NE - 1:
                final_group(tb)
        P.finish()
    return nc, dbg_out


_CACHE = {}


def _consts():
    c = np.zeros((128, 1024), np.float32)
    c[:, 0:128] = np.eye(128, dtype=np.float32)
    p = np.arange(128)
    blk = (p[:, None] // 64 == p[None, :] // 64).astype(np.float32)
    c[:, 128:256] = blk / 64.0
    import ml_dtypes
    cb = np.zeros((128, 1536), ml_dtypes.bfloat16)
    cb[:, 0:128] = np.eye(128)
    cb[:, 128:256] = blk
    s = (p % 64)[:, None]
    t = np.arange(64)[None, :]
    cb[:, 256:320] = (s < t)
    cb[:, 320:384] = (s <= t)
    cb[:, 384:448] = (t < s)
    cb[:, 448:512] = (t == s)
    c[:, 256:1024] = np.ascontiguousarray(cb).view(np.float32)
    return c


def _prep_shared(inp):
    f = lambda a: np.ascontiguousarray(a, dtype=np.float32)
    w_in = inp["w_in"][0]
    wt = np.stack([w_in[:, c * 128:(c + 1) * 128].reshape(8, 128, 128).transpose(1, 0, 2).reshape(128, 1024)
                   for c in ORDER], 0)
    par = np.zeros((128, NPAR), np.float32)
    mu = inp["rwkv_mu"][0]
    for q in range(14):
        par[:, PC_MU + q] = mu[q * 128:(q + 1) * 128]
    cw = inp["conv_w"][0]
    for tap in range(3):
        for j in range(4):
            par[:, PC_CW + tap * 4 + j] = cw[tap, j * 128:(j + 1) * 128]
    for nm, col in (("decay_base", PC_DB), ("aaa_base", PC_AB), ("k_k", PC_KK), ("k_a", PC_KA),
                    ("ln_x_w", PC_LW), ("ln_x_b", PC_LB)):
        v = inp[nm][0]
        for hp in range(4):
            par[:, col + hp] = v[hp * 128:(hp + 1) * 128]
    rk = inp["r_k"][0].reshape(512)
    for hp in range(4):
        par[:, PC_RK + hp] = rk[hp * 128:(hp + 1) * 128]
    sh = {
        "w_in_t": f(wt),
        "params": par,
        "lora_up": f(np.concatenate([inp["decay_up"][0], inp["aaa_up"][0]], 0)),
        "gate_up": f(inp["gate_up"][0]),
        "g_mix": f(inp["norm_mix_g"][0]),
        "g_ffn": f(inp["norm_ffn_g"][0]),
        "g_fin": f(inp["norm_final_g"]),
        "w_out_t": f(inp["w_out"][0].reshape(8, 128, 1024).transpose(1, 0, 2).reshape(128, 8192)),
        "w_r_t": f(np.concatenate([inp["router_group_w"][0], inp["router_expert_w"][0]], 1)
                   .reshape(8, 128, 36).transpose(1, 0, 2).reshape(128, 288)),
        "b_r": f(np.concatenate([inp["router_group_b"][0], inp["router_expert_b"][0]], 0)),
        "wg_t": f(inp["expert_w_gate"][0].reshape(NE, 8, 128, 256).transpose(0, 2, 1, 3).reshape(NE, 128, 2048)),
        "wu_t": f(inp["expert_w_up"][0].reshape(NE, 8, 128, 256).transpose(0, 2, 1, 3).reshape(NE, 128, 2048)),
        "wd_t": f(inp["expert_w_down"][0].reshape(NE, 2, 128, 1024).transpose(0, 2, 1, 3).reshape(NE, 128, 2048)),
        "consts": _consts(),
    }
    return sh


def run(inp, n_cores=8, stop=None, dbg=(), trace=False):
    key = (stop, tuple(dbg))
    if key not in _CACHE:
        _CACHE[key] = build(stop=stop, dbg=dbg)
    nc, dbg_out = _CACHE[key]
    sh = _prep_shared(inp)
    x = np.asarray(inp["x"], dtype=np.float32)
    in_maps = []
    for c in range(n_cores):
        m = dict(sh)
        m["x"] = np.ascontiguousarray(x[c])
        in_maps.append(m)
    res = run_bass_kernel_spmd(nc, in_maps, core_ids=list(range(n_cores)), **({"trace": True} if trace else {}))
    return res


def kernel(**inputs):
    inp = {k: np.asarray(v) for k, v in inputs.items()}
    res = run(inp, n_cores=8)
    out = np.stack([np.asarray(res.results[c]["out"], dtype=np.float32) for c in range(8)], 0)
    return out
```

```python
import numpy as np
from contextlib import ExitStack
import concourse.bass as bass
import concourse.mybir as mybir
from concourse.bass_utils import run_bass_kernel_spmd

F32 = mybir.dt.float32
BF16 = mybir.dt.bfloat16
AF = mybir.ActivationFunctionType
ALU = mybir.AluOpType
AX = mybir.AxisListType

T = 2048
D = 1024
NTILE = 16
NE = 32
C0 = float(np.exp(-0.5))
ORDER = [24, 25, 12, 16, 20, 13, 17, 21, 14, 18, 22, 15, 19, 23,
         0, 4, 8, 1, 5, 9, 2, 6, 10, 3, 7, 11]
PC_MU = 0
PC_CW = 14
PC_DB = 26
PC_AB = 30
PC_KK = 34
PC_KA = 38
PC_RK = 42
PC_LW = 46
PC_LB = 50
NPAR = 54


class Prog:
    ENG = ('pe', 'act', 'dve', 'pool', 'sp')

    def __init__(self, nc, es, n_dma=24):
        self.nc = nc
        self.q = {e: [] for e in self.ENG}
        self.sem = {e: es.enter_context(nc.semaphore("s_" + e)) for e in ('pe', 'act', 'dve', 'pool')}
        self.dsem = [es.enter_context(nc.semaphore("d%d" % i)) for i in range(n_dma)]
        self.cnt = {e: 0 for e in ('pe', 'act', 'dve', 'pool')}
        self.dval = [0] * n_dma
        self.drr = 0
        self.waited = {}
        self.lastw = {}
        self.readers = {}
        self.ninst = 0
        self.cap = None

    def _semh(self, key):
        return self.sem[key] if isinstance(key, str) else self.dsem[key]

    def _need(self, eng, deps):
        for key, val in deps.items():
            if key == eng and eng == 'pe':
                continue
            if self.waited.get((eng, key), 0) >= val:
                continue
            self.waited[(eng, key)] = val
            h = self._semh(key)
            self.q[eng].append(lambda e, h=h, val=val: e.wait_ge(h, val))

    def _collect(self, eng, reads, writes):
        deps = {}
        for t in list(reads) + list(writes):
            w = self.lastw.get(t)
            if w and deps.get(w[0], 0) < w[1]:
                deps[w[0]] = w[1]
        for t in writes:
            for k, v in self.readers.get(t, {}).items():
                if k == eng and isinstance(k, str):
                    continue
                if deps.get(k, 0) < v:
                    deps[k] = v
        return deps

    def _record(self, key, val, reads, writes):
        for t in reads:
            r = self.readers.setdefault(t, {})
            if r.get(key, 0) < val:
                r[key] = val
        for t in writes:
            self.lastw[t] = (key, val)
            self.readers[t] = {}

    def op(self, eng, fn, reads=(), writes=(), inc=True):
        if self.cap is not None:
            self.cap.append(('op', eng, fn, list(reads), list(writes), inc))
            return
        self.ninst += 1
        deps = self._collect(eng, reads, writes)
        self._need(eng, deps)
        if inc or eng != 'pe':
            self.cnt[eng] += 1
            val = self.cnt[eng]
            h = self.sem[eng]
            self.q[eng].append(lambda e, fn=fn, h=h: fn(e).then_inc(h, 1))
        else:
            val = self.cnt[eng] + 1
            self.q[eng].append(lambda e, fn=fn: fn(e))
        self._record(eng, val, reads, writes)

    def dma(self, out, in_, reads=(), writes=()):
        if self.cap is not None:
            self.cap.append(('dma', out, in_, list(reads), list(writes)))
            return
        self.ninst += 1
        eng = 'sp'
        j = self.drr
        self.drr = (self.drr + 1) % len(self.dsem)
        deps = self._collect(eng, reads, writes)
        if self.dval[j] > 0:
            deps[j] = max(deps.get(j, 0), self.dval[j])
        self._need(eng, deps)
        self.dval[j] += 16
        val = self.dval[j]
        h = self.dsem[j]
        self.q[eng].append(lambda e, h=h, out=out, in_=in_: e.dma_start(out=out, in_=in_).then_inc(h, 16))
        self._record(j, val, reads, writes)

    def capture(self, f):
        assert self.cap is None
        self.cap = []
        f()
        ops, self.cap = self.cap, None
        return ops

    def replay(self, *lists):
        lists = [l for l in lists if l]
        idx = [0] * len(lists)
        tot = max(len(l) for l in lists) if lists else 0
        for step in range(tot):
            for j, l in enumerate(lists):
                hi = (step + 1) * len(l) // tot
                while idx[j] < hi:
                    o = l[idx[j]]
                    idx[j] += 1
                    if o[0] == 'op':
                        self.op(o[1], o[2], o[3], o[4], o[5])
                    else:
                        self.dma(o[1], o[2], o[3], o[4])

    def barrier(self):
        deps = {k: v for k, v in self.cnt.items() if v > 0}
        for j, v in enumerate(self.dval):
            if v > 0:
                deps[j] = v
        for eng in self.ENG:
            d = {k: v for k, v in deps.items() if k != eng}
            self._need(eng, d)

    def finish(self):
        deps = {j: v for j, v in enumerate(self.dval) if v > 0}
        self._need('sp', deps)
        with self.nc.Block() as block:
            @block.tensor
            def _(e):
                for f in self.q['pe']:
                    f(e)

            @block.scalar
            def _(e):
                for f in self.q['act']:
                    f(e)

            @block.vector
            def _(e):
                for f in self.q['dve']:
                    f(e)

            @block.gpsimd
            def _(e):
                for f in self.q['pool']:
                    f(e)

            @block.sync
            def _(e):
                for f in self.q['sp']:
                    f(e)


class Arena:
    def __init__(self, big, nwords):
        self.big = big
        self.n = nwords
        self.top = 0

    def f32(self, n, at=None):
        if at is None:
            at = self.top
            self.top += n
        assert at + n <= self.n, (at, n, self.n)
        return self.big[:, at:at + n]

    def bf(self, n, at=None):
        w = (n + 1) // 2
        if at is None:
            at = self.top
            self.top += w
        assert at + w <= self.n, (at, w, self.n)
        return self.big[:, at:at + w].bitcast(BF16)


def build(stop=None, dbg=()):
    nc = bass.Bass("TRN2", target_bir_lowering=False)
    dram = {}

    def din(name, shape, dt=F32):
        dram[name] = nc.dram_tensor(name, list(shape), dt, kind="ExternalInput").ap()
        return dram[name]

    x_d = din("x", [T, D])
    win_d = din("w_in_t", [26, 128, 8 * 128])
    par_d = din("params", [128, NPAR])
    lup_d = din("lora_up", [128, 512])
    gup_d = din("gate_up", [128, 512])
    g1_d = din("g_mix", [D])
    g2_d = din("g_ffn", [D])
    g3_d = din("g_fin", [D])
    wout_d = din("w_out_t", [128, 8 * 1024])
    wr_d = din("w_r_t", [128, 8 * 36])
    br_d = din("b_r", [36])
    wg_d = din("wg_t", [NE, 128, 8 * 256])
    wu_d = din("wu_t", [NE, 128, 8 * 256])
    wd_d = din("wd_t", [NE, 128, 2 * 1024])
    cst_d = din("consts", [128, 1024])
    out_d = nc.dram_tensor("out", [T, D], F32, kind="ExternalOutput").ap()
    dbg_out = {}

    es = ExitStack()
    with es:
        NW = 51200
        big = es.enter_context(nc.sbuf_tensor("big", [128, NW], F32))
        banks = [es.enter_context(nc.psum_tensor("ps%d" % i, [128, 512], F32)) for i in range(8)]
        P = Prog(nc, es)
        A = Arena(big, NW)

        def dump(name, ap, shape, dt=F32, reads=()):
            if name not in dbg:
                return
            t = nc.dram_tensor("dbg_" + name, list(shape), dt, kind="ExternalOutput").ap()
            dbg_out[name] = t
            P.dma(t, ap, reads=reads)

        cst = A.f32(1024)
        par = A.f32(64)
        gbc = A.f32(1024)
        P.dma(cst, cst_d[:, :], writes=['cst'])
        P.dma(par[:, 0:NPAR], par_d[:, :], writes=['par'])
        identF = cst[:, 0:128]
        bonesF = cst[:, 128:256]
        cstb = cst[:, 256:1024].bitcast(BF16)
        identB = cstb[:, 0:128]
        bonesB = cstb[:, 128:256]
        omka = A.f32(4)
        hpar = A.f32(8)
        omm_ = A.f32(16)
        P.op('dve', lambda e: e.tensor_scalar(out=omka, in0=par[:, PC_KA:PC_KA + 4], scalar1=-1.0, scalar2=1.0,
                                               op0=ALU.mult, op1=ALU.add), reads=['par'], writes=['omka'])

        base0 = A.top
        uT = A.bf(8 * T).rearrange("p (k t) -> p k t", t=T)
        tha = A.bf(T)
        sg = A.bf(T)
        pers0 = A.top
        AR = A.bf(4 * 2 * T).rearrange("p (h x t) -> p h x t", h=4, x=2)
        BK = A.bf(4 * 2 * T).rearrange("p (h x t) -> p h x t", h=4, x=2)
        VB = A.bf(4 * 2 * T).rearrange("p (h x t) -> p h x t", h=4, x=2)
        WC = A.f32(128).rearrange("p (h c) -> p h c", h=4)
        pers1 = A.top
        wst = [A.f32(1024) for _ in range(2)]
        wbf = [A.bf(1024).rearrange("p (k c) -> p k c", c=128) for _ in range(4)]
        lupb = A.bf(512)
        gupb = A.bf(512)
        work0 = A.top

        xall = A.f32(16 * D, at=pers0).rearrange("p (i d) -> p i d", d=D)
        xn = [A.f32(D, at=work0 + i * D) for i in range(2)]
        junk = A.bf(D, at=work0 + 2 * D)
        ss = A.f32(16, at=work0 + 2 * D + 512)
        rstd = A.f32(16, at=work0 + 2 * D + 528)
        lst = A.f32(512, at=work0 + 2 * D + 1024)
        gst = A.f32(512, at=work0 + 2 * D + 1536)

        P.dma(gbc, g1_d.partition_broadcast(128), writes=['gbc'])
        P.dma(lst, lup_d[:, :], writes=['lst'])
        P.dma(gst, gup_d[:, :], writes=['gst'])
        P.op('pool', lambda e: e.tensor_copy(out=lupb, in_=lst), reads=['lst'], writes=['lupb'])
        P.op('pool', lambda e: e.tensor_copy(out=gupb, in_=gst), reads=['gst'], writes=['gupb'])

        def rms_stats(src, tag):
            for i in range(NTILE):
                P.op('act', lambda e, i=i: e.activation(out=junk, in_=src(i), func=AF.Square,
                                                        accum_out=ss[:, i:i + 1]),
                     reads=[tag + str(i)], writes=['junk', 'ss'])
            P.op('dve', lambda e: e.tensor_scalar(out=rstd, in0=ss, scalar1=1.0 / D, scalar2=1e-6,
                                                   op0=ALU.mult, op1=ALU.add), reads=['ss'], writes=['rstd'])
            P.op('act', lambda e: e.activation(out=rstd, in_=rstd, func=AF.Ln), reads=['rstd'], writes=['rstd'])
            P.op('act', lambda e: e.activation(out=rstd, in_=rstd, func=AF.Exp, scale=-0.5), reads=['rstd'], writes=['rstd'])

        for i in range(NTILE):
            P.dma(xall[:, i, :], x_d[i * 128:(i + 1) * 128, :], writes=['xa%d' % i])
        rms_stats(lambda i: xall[:, i, :], 'xa')

        def norm_transpose(src, tag, dstT, dst_tag):
            for i in range(NTILE):
                xb = xn[i % 2]
                P.op('dve', lambda e, i=i, xb=xb: e.scalar_tensor_tensor(
                    out=xb, in0=src(i), scalar=rstd[:, i:i + 1], in1=gbc, op0=ALU.mult, op1=ALU.mult),
                    reads=[tag + str(i), 'rstd', 'gbc'], writes=['xn%d' % (i % 2)])
                for half in range(2):
                    bk = banks[(2 * i + half) % 4]
                    btag = 'bank%d' % ((2 * i + half) % 4)
                    for kk in range(4):
                        k = half * 4 + kk
                        P.op('pe', lambda e, bk=bk, kk=kk, k=k, xb=xb: e.transpose(
                            out=bk[:, kk * 128:(kk + 1) * 128], in_=xb[:, k * 128:(k + 1) * 128], identity=identF),
                            reads=['xn%d' % (i % 2), 'cst'], writes=[btag], inc=(kk == 3))
                    P.op('act', lambda e, bk=bk, half=half, i=i: e.activation(
                        out=dstT[:, half * 4:half * 4 + 4, i * 128:(i + 1) * 128],
                        in_=bk[:, :].rearrange("p (k t) -> p k t", t=128), func=AF.Copy),
                        reads=[btag], writes=[dst_tag])

        norm_transpose(lambda i: xall[:, i, :], 'xa', uT, 'uT')
        dump('uT', uT[:, 0, :], [128, T], BF16, reads=['uT'])
        if stop == 'A':
            P.finish()
            return nc, dbg_out
        P.barrier()

        def ACT(out, in_, func, reads, writes, **kw):
            P.op('act', lambda e: e.activation(out=out, in_=in_, func=func, **kw), reads, writes)

        def TT(eng, out, a, b, op, reads, writes):
            P.op(eng, lambda e: e.tensor_tensor(out=out, in0=a, in1=b, op=op), reads, writes)

        def TS(eng, out, a, s1, s2, op0, op1, reads, writes):
            P.op(eng, lambda e: e.tensor_scalar(out=out, in0=a, scalar1=s1, scalar2=s2, op0=op0, op1=op1), reads, writes)

        def STT(out, a, s, b, op0, op1, reads, writes):
            P.op('dve', lambda e: e.scalar_tensor_tensor(out=out, in0=a, scalar=s, in1=b, op0=op0, op1=op1), reads, writes)

        def MM(out, lhsT, rhs, reads, writes, start=True, stop=True, inc=True):
            P.op('pe', lambda e: e.matmul(out, lhsT=lhsT, rhs=rhs, start=start, stop=stop), reads, writes, inc=inc)

        def BT(b):
            return 'bank%d' % b

        wk = {}
        wtop = [work0]

        def walloc(name, n=512, dt=F32):
            if dt == F32:
                wk[name] = A.f32(n, at=wtop[0])
                wtop[0] += n
            else:
                wk[name] = A.bf(n, at=wtop[0])
                wtop[0] += (n + 1) // 2
            return wk[name]

        for nm in ('r', 'k', 't1'):
            walloc(nm)
        chb = walloc('chb', 514)
        conv_end = wtop[0]
        zc = {}
        for nm in ('r', 'k', 'v'):
            zc[nm] = walloc('zc_' + nm, 514)
        dtile = walloc('d')
        smask = walloc('smask')
        for nm in ('v', 'sig', 'a', 'cs', 'winc', 'winv'):
            walloc(nm)
        sqb = walloc('sqb', 512, BF16)
        rkb = walloc('rkb', 512, BF16)
        assert wtop[0] <= NW, wtop[0]
        W = wk
        ycT = A.bf(4 * T, at=work0 + 3968).rearrange("p (j t) -> p j t", t=T)
        assert conv_end <= work0 + 3968

        TS('dve', hpar, par[:, PC_DB:PC_DB + 8], 0.5, None, ALU.mult, ALU.bypass, ['par'], ['hpar'])
        TS('dve', omm_[:, 0:14], par[:, PC_MU:PC_MU + 14], -1.0, 1.0, ALU.mult, ALU.add, ['par'], ['omm'])

        wctr = [0]

        def load_w(ci):
            b = wctr[0] % 4
            sb = wctr[0] % 2
            wctr[0] += 1
            P.dma(wst[sb], win_d[ci, :, :], writes=['wst%d' % sb])
            P.op('pool', lambda e: e.tensor_copy(out=wbf[b][:, :, :].rearrange("p k c -> p (k c)"), in_=wst[sb]),
                 reads=['wst%d' % sb], writes=['wbf%d' % b])
            return wbf[b], 'wbf%d' % b

        def proj(wap, wtag, tb, b):
            for k in range(8):
                MM(banks[b][:, :], wap[:, k, :], uT[:, k, tb * 512:(tb + 1) * 512], [wtag, 'uT'], [BT(b)],
                   start=(k == 0), stop=(k == 7), inc=(k == 7))

        def shift_mix(nm, b, tb, mucol, out_ap, out_tag):
            z = zc[nm]
            zt = 'zc_' + nm
            if tb > 0:
                ACT(z[:, 0:1], z[:, 512:513], AF.Copy, [zt], [zt])
            else:
                P.op('pool', lambda e: e.memset(z[:, 0:1], 0.0), [], [zt])
            ACT(z[:, 1:513], banks[b][:, :], AF.Copy, [BT(b)], [zt])
            ACT(out_ap, banks[b][:, :], AF.Copy, [BT(b), 'omm'], [out_tag], scale=omm_[:, mucol - PC_MU:mucol - PC_MU + 1])
            STT(out_ap, z[:, 0:512], par[:, mucol:mucol + 1], out_ap, ALU.mult, ALU.add, [zt, 'par', out_tag], [out_tag])

        w24, t24 = load_w(0)
        w25, t25 = load_w(1)
        for tb in range(4):
            sl = slice(tb * 512, (tb + 1) * 512)
            proj(w24, t24, tb, 0)
            shift_mix('r', 0, tb, PC_MU + 12, W['t1'], 't1')
            ACT(tha[0:64, sl], W['t1'][0:64, :], AF.Tanh, ['t1'], ['tha'])
            ACT(tha[64:128, sl], W['t1'][64:128, :], AF.Copy, ['t1'], ['tha'])
            proj(w25, t25, tb, 1)
            shift_mix('k', 1, tb, PC_MU + 13, W['t1'], 't1')
            ACT(W['t1'], W['t1'], AF.Tanh, ['t1'], ['t1'], scale=0.5)
            TS('pool', sg[:, sl], W['t1'], 0.5, 0.5, ALU.mult, ALU.add, ['t1'], ['sg'])
        dump('tha', tha, [128, T], BF16, reads=['tha'])
        dump('sg', sg, [128, T], BF16, reads=['sg'])

        P.op('pool', lambda e: e.memset(smask, 1.0), [], ['smask'])
        P.op('pool', lambda e: e.memset(smask.rearrange("p (c t) -> p c t", t=64)[:, :, 0:1], 0.0), [], ['smask'])

        WBs = {}
        _ar3, _bk3, _vb3 = pers0 + 6144, pers0 + 8192 + 6144, pers0 + 16384 + 6144
        for j_, nm in enumerate(('r', 'k', 'v', 'sig')):
            WBs[nm] = A.f32(512, at=_ar3 + j_ * 512)
        for j_, nm in enumerate(('a', 'cs', 'winc', 'winv')):
            WBs[nm] = A.f32(512, at=_bk3 + j_ * 512)
        WBs['t1'] = A.f32(512, at=_vb3)
        WBs['sqb'] = A.bf(512, at=_vb3 + 512)
        WBs['rkb'] = A.bf(512, at=_vb3 + 768)
        WAs = dict(W)
        WAs['sqb'] = sqb
        WAs['rkb'] = rkb

        def wset(hp, tb):
            if hp < 3 and (hp * 4 + tb) % 2 == 1:
                return WBs, 'B'
            return WAs, 'A'

        def prep(hp, tb, part='AB'):
            Wx, sx = wset(hp, tb)
            T_ = lambda n: n + sx
            u_ = '%d%d' % (hp, tb)
            sl = slice(tb * 512, (tb + 1) * 512)
            hc = slice(hp * 128, (hp + 1) * 128)
            r_, k_, v_, sig, a_, cs, winc, winv, t1 = (Wx[n] for n in ('r', 'k', 'v', 'sig', 'a', 'cs', 'winc', 'winv', 't1'))
            sqb_, rkb_ = Wx['sqb'], Wx['rkb']
            if 'A' in part:
                prepA_(hp, tb, Wx, T_, u_, sl, hc)
            if 'B' in part:
                prepB_(hp, tb, Wx, T_, u_, sl, hc)

        def prepA_(hp, tb, Wx, T_, u_, sl, hc):
            r_, k_, v_, sig, a_, cs, winc, winv, t1 = (Wx[n] for n in ('r', 'k', 'v', 'sig', 'a', 'cs', 'winc', 'winv', 't1'))
            sqb_, rkb_ = Wx['sqb'], Wx['rkb']
            MM(banks[4][:, :], lupb[0:64, hc], tha[0:64, sl], ['lupb', 'tha'], [BT(4)])
            MM(banks[5][:, :], lupb[64:128, hc], tha[64:128, sl], ['lupb', 'tha'], [BT(5)])
            ACT(sig, banks[4][:, :], AF.Tanh, [BT(4), 'hpar'], [T_('sig')], scale=0.5, bias=hpar[:, hp:hp + 1])
            ACT(a_, banks[5][:, :], AF.Tanh, [BT(5), 'hpar'], [T_('a')], scale=0.5, bias=hpar[:, 4 + hp:5 + hp])
            TS('dve', sig, sig, 0.5, 0.5, ALU.mult, ALU.add, [T_('sig')], [T_('sig')])
            TS('dve', a_, a_, 0.5, 0.5, ALU.mult, ALU.add, [T_('a')], [T_('a')])
            P.op('dve', lambda e: e.tensor_tensor_scan(out=cs, data0=smask, data1=sig, initial=0.0,
                                                        op0=ALU.mult, op1=ALU.add), ['smask', T_('sig')], [T_('cs')])
            ACT(winc, cs, AF.Exp, [T_('cs')], [T_('winc')], scale=-C0)
            ACT(winv, cs, AF.Exp, [T_('cs')], [T_('winv')], scale=C0)
            TT('dve', sig, cs, sig, ALU.subtract, [T_('cs'), T_('sig')], [T_('sig')])
            ACT(sig, sig, AF.Exp, [T_('sig')], [T_('sig')], scale=-C0)
            ACT(WC[:, hp, tb * 8:(tb + 1) * 8], winc.rearrange("p (c t) -> p c t", t=64)[:, :, 63], AF.Copy,
                [T_('winc')], ['WC' + u_])
            TT('pool', AR[:, hp, 1, sl], r_, winc, ALU.mult, [T_('r'), T_('winc')], ['ARr' + u_])
            ACT(winc, k_, AF.Copy, [T_('k'), 'par'], [T_('winc')], scale=par[:, PC_KK + hp:PC_KK + hp + 1])
            TT('dve', sqb_, winc, winc, ALU.mult, [T_('winc')], [T_('sqb')])
            MM(banks[6][:, :], bonesB, sqb_, ['cst', T_('sqb')], [BT(6)])

        def prepB_(hp, tb, Wx, T_, u_, sl, hc):
            r_, k_, v_, sig, a_, cs, winc, winv, t1 = (Wx[n] for n in ('r', 'k', 'v', 'sig', 'a', 'cs', 'winc', 'winv', 't1'))
            sqb_, rkb_ = Wx['sqb'], Wx['rkb']
            TS('dve', t1, banks[6][:, :], 1e-24, None, ALU.max, ALU.bypass, [BT(6)], [T_('t1')])
            ACT(t1, t1, AF.Ln, [T_('t1')], [T_('t1')])
            ACT(t1, t1, AF.Exp, [T_('t1')], [T_('t1')], scale=-0.5)
            TT('dve', winc, winc, t1, ALU.mult, [T_('winc'), T_('t1')], [T_('winc')])
            STT(AR[:, hp, 0, sl], winc, -1.0, sig, ALU.mult, ALU.mult, [T_('winc'), T_('sig')], ['ARa' + u_])
            TS('dve', t1, a_, par[:, PC_KA + hp:PC_KA + hp + 1], omka[:, hp:hp + 1], ALU.mult, ALU.add,
               [T_('a'), 'par', 'omka'], [T_('t1')])
            TT('pool', k_, k_, t1, ALU.mult, [T_('k'), T_('t1')], [T_('k')])
            TT('dve', t1, a_, winc, ALU.mult, [T_('a'), T_('winc')], [T_('t1')])
            TT('pool', BK[:, hp, 0, sl], t1, winv, ALU.mult, [T_('t1'), T_('winv')], ['BKb' + u_])
            TT('pool', BK[:, hp, 1, sl], k_, winv, ALU.mult, [T_('k'), T_('winv')], ['BKk' + u_])
            STT(rkb_, r_, par[:, PC_RK + hp:PC_RK + hp + 1], k_, ALU.mult, ALU.mult, [T_('r'), T_('k'), 'par'], [T_('rkb')])
            MM(banks[7][:, :], bonesB, rkb_, ['cst', T_('rkb')], [BT(7)])
            TT('dve', VB[:, hp, 1, sl], banks[7][:, :], v_, ALU.mult, [BT(7), T_('v')], ['VBb' + u_])
            ACT(VB[:, hp, 0, sl], v_, AF.Copy, [T_('v')], ['VBv' + u_])

        wsets = {}

        def part1(hp, tb):
            if tb == 0:
                wsets[hp] = [load_w(2 + hp * 3 + ti) for ti in range(3)]
            ws = wsets[hp]
            Wx, sx = wset(hp, tb)
            for ti, nm in enumerate(('r', 'k', 'v')):
                proj(ws[ti][0], ws[ti][1], tb, ti)
                shift_mix(nm, ti, tb, PC_MU + ti * 4 + hp, Wx[nm], nm + sx)

        its = [(hp, tb) for hp in range(3) for tb in range(4)]
        part1(*its[0])
        prep(its[0][0], its[0][1], 'A')
        for n, (hp, tb) in enumerate(its):
            la = P.capture(lambda: prep(hp, tb, 'B'))
            lb = []
            if n + 1 < len(its):
                hp2, tb2 = its[n + 1]
                lb = P.capture(lambda: (part1(hp2, tb2), prep(hp2, tb2, 'A')))
            P.replay(la, lb)
        P.barrier()
        for tb in range(4):
            part1(3, tb)
            prep(3, tb)
        for hp in range(4):
            if hp in (0, 3):
                dump('AR%d' % hp, AR[:, hp, :, :].rearrange("p x t -> p (x t)"), [128, 2 * T], BF16, reads=['AR'])
                dump('BK%d' % hp, BK[:, hp, :, :].rearrange("p x t -> p (x t)"), [128, 2 * T], BF16, reads=['BK'])
                dump('VB%d' % hp, VB[:, hp, :, :].rearrange("p x t -> p (x t)"), [128, 2 * T], BF16, reads=['VB'])
        for nm_ in ('sig', 'a', 'cs', 'winc', 'winv', 't1'):
            dump('w_' + nm_, W[nm_], [128, 512], F32, reads=[nm_])
        dump('hpar', hpar, [128, 8], F32, reads=['hpar'])
        dump('par', par[:, 0:NPAR], [128, NPAR], F32, reads=['par'])
        dump('WC', WC[:, :, :].rearrange("p h c -> p (h c)"), [128, 128], F32, reads=['WC'])
        if stop == 'B':
            P.finish()
            return nc, dbg_out
        P.barrier()

        cs0, cs1 = work0, work0 + 2050
        csets = []
        for base_ in (cs0, cs1):
            csets.append(dict(b=A.bf(512, at=base_), c=A.bf(512, at=base_ + 256), t1=A.f32(512, at=base_ + 512),
                              chb=A.f32(514, at=base_ + 1024)))
        assert cs1 + 1538 <= work0 + 3968
        cits = [(j, tb) for j in range(4) for tb in range(4)]
        cws = {}

        def conv_it(n):
            j, tb = cits[n]
            if tb == 0:
                cws[j] = [load_w(14 + j * 3 + ti) for ti in range(3)]
            ws = cws[j]
            S_ = csets[n % 2]
            Sp = csets[(n + 1) % 2]
            sx = 'c%d' % (n % 2)
            px = 'c%d' % ((n + 1) % 2)
            sl = slice(tb * 512, (tb + 1) * 512)
            bo = 4 * (n % 2)
            for ti in range(3):
                proj(ws[ti][0], ws[ti][1], tb, bo + ti)
            ACT(S_['b'], banks[bo][:, :], AF.Copy, [BT(bo)], ['b' + sx])
            ACT(S_['c'], banks[bo + 1][:, :], AF.Copy, [BT(bo + 1)], ['c' + sx])
            chb_ = S_['chb']
            if tb == 0:
                P.op('pool', lambda e: e.memset(chb_[:, 0:2], 0.0), [], ['cy' + sx])
            TT('dve', chb_[:, 2:514], banks[bo + 2][:, :], S_['c'], ALU.mult, [BT(bo + 2), 'c' + sx], ['chb' + sx])
            def carry():
                if tb < 3:
                    ACT(Sp['chb'][:, 0:2], chb_[:, 512:514], AF.Copy, ['chb' + sx], ['cy' + px])
            if n % 2 == 0:
                carry()
            cw = lambda tap: par[:, PC_CW + tap * 4 + j:PC_CW + tap * 4 + j + 1]
            t1_ = S_['t1']
            ACT(t1_, chb_[:, 2:514], AF.Copy, ['chb' + sx, 'par'], ['t1' + sx], scale=cw(2))
            STT(t1_, chb_[:, 1:513], cw(1), t1_, ALU.mult, ALU.add, ['chb' + sx, 'cy' + sx, 't1' + sx, 'par'], ['t1' + sx])
            STT(t1_, chb_[:, 0:512], cw(0), t1_, ALU.mult, ALU.add, ['chb' + sx, 'cy' + sx, 't1' + sx, 'par'], ['t1' + sx])
            TT('pool', ycT[:, j, sl], t1_, S_['b'], ALU.mult, ['t1' + sx, 'b' + sx], ['ycT%d%d' % (j, tb)])
            if n % 2 == 1:
                carry()

        for n in range(0, len(cits), 2):
            la = P.capture(lambda: conv_it(n))
            lb = P.capture(lambda: conv_it(n + 1))
            P.replay(la, lb)
        P.barrier()
        dump('ycT', ycT[:, 0, :], [128, T], BF16, reads=['ycT'])
        if stop == 'C':
            P.finish()
            return nc, dbg_out
        P.barrier()

        maskS = cstb[:, 256:384]
        maskL = cstb[:, 384:448]
        ident2 = cstb[:, 448:512]
        r1, r2, r3 = [base0], [pers1], [work0]

        def cbf(reg, n):
            ap = A.bf(n, at=reg[0])
            reg[0] += (n + 1) // 2
            return ap

        def cf(reg, n):
            ap = A.f32(n, at=reg[0])
            reg[0] += n
            return ap

        h128 = lambda ap: ap.rearrange("p (h x) -> p h x", x=128)
        h64 = lambda ap: ap.rearrange("p (h x) -> p h x", x=64)
        hcx = lambda ap: ap.rearrange("p (h c x) -> p h c x", h=4, c=2)
        yrT = cbf(r1, 4 * T).rearrange("p (j t) -> p j t", t=T)
        AL = [h128(cbf(r1, 1024)) for _ in range(2)]
        RB = [h128(cbf(r1, 1024)) for _ in range(2)]
        Ktm = [h64(cbf(r1, 512)) for _ in range(2)]
        Vtm = [h64(cbf(r1, 512)) for _ in range(2)]
        S2m = [h128(cbf(r1, 1024)) for _ in range(2)]
        AUs = [h128(cbf(r1, 1024)) for _ in range(2)]
        assert r1[0] <= base0 + 9216, r1[0]
        NY = [h128(cbf(r2, 1024)) for _ in range(2)]
        NTs = [h64(cbf(r2, 512)) for _ in range(2)]
        Yf = h64(cbf(r2, 512))
        RhT = [hcx(cbf(r2, 512)) for _ in range(2)]
        GT = [hcx(cbf(r2, 512)) for _ in range(2)]
        O0s = [hcx(cf(r2, 512)) for _ in range(2)]
        Hb = [h64(cbf(r2, 256)) for _ in range(2)]
        assert r2[0] <= pers1 + 4096 + 256, r2[0]
        Qs = [hcx(cf(r3, 512)) for _ in range(2)]
        ot = cf(r3, 512)
        osq = cf(r3, 512)
        dd = cf(r3, 512)
        rs = cf(r3, 512)
        htmp = cf(r3, 256)
        ob = cbf(r3, 512)
        osqb = cbf(r3, 512)
        assert r3[0] <= work0 + 3968, r3[0]
        WC4 = A.f32(128, at=pers1 - 128).rearrange("p (h c o) -> p h c o", h=4, o=1)
        lnw4 = par[:, PC_LW:PC_LW + 4].rearrange("p (h o) -> p h o", o=1)
        lnb4 = par[:, PC_LB:PC_LB + 4].rearrange("p (h o) -> p h o", o=1)

        def pbf(b):
            return banks[b][:, :].bitcast(BF16)

        hv = lambda ap, hd: ap.rearrange("p (a b) x -> p a b x", b=2)[:, :, hd, :]
        crs = (slice(0, 64), slice(64, 128))
        hrs = crs
        NTB = (4, 7)
        QB = (4, 7)
        OB = (5, 6)
        G4 = [(cp, hd) for cp in range(2) for hd in range(2)]
        gt_ = lambda name, cp, hd: '%s_%d%d' % (name, cp, hd)
        both = lambda name, hd: [gt_(name, 0, hd), gt_(name, 1, hd)]
        allg = lambda name: [gt_(name, cp, hd) for cp, hd in G4]
        bP = lambda cp, hd: 'bP%d' % (cp * 2 + hd)
        bQ = lambda cp, hd: 'bQ%d_%d' % (cp, hd)
        bO = lambda cp, hd: 'bO%d_%d' % (cp, hd)
        m64 = lambda m: m.rearrange("p (o x) -> p o x", o=1).to_broadcast([128, 4, 64])
        mS1, mS2, mL4, id4 = m64(maskS[:, 0:64]), m64(maskS[:, 64:128]), m64(maskL), m64(ident2)
        mSf = maskS.rearrange("p (o x) -> p o x", o=1).to_broadcast([128, 4, 128])
        P.op('pool', lambda e: e.memset(Hb[0][:, :, :], 0.0), [], ['Hb0_0', 'Hb0_1'])

        S1B, S2B, STB = (0, 3), (1, 2), (0, 3)
        PBK = lambda cp, hd: cp * 2 + hd
        QBK = lambda cp, hd: 4 + cp * 2 + hd
        bk = lambda n: ['bk%d' % n]

        def st_T(i):
            par = i % 2
            tk = slice(i * 128, (i + 1) * 128)
            wb5, wb6 = bk(5), bk(6)
            for hp in range(4):
                P.op('pe', lambda e, hp=hp: e.transpose(out=pbf(5)[:, hp * 128:(hp + 1) * 128], in_=AR[:, hp, 0, tk],
                                                        identity=identB), ['AR', 'cst'], wb5, inc=False)
                P.op('pe', lambda e, hp=hp: e.transpose(out=pbf(5)[:, 512 + hp * 128:512 + (hp + 1) * 128],
                                                        in_=BK[:, hp, 0, tk], identity=identB), ['BK', 'cst'], wb5, inc=(hp == 3))
            for hp in range(4):
                P.op('pe', lambda e, hp=hp: e.transpose(out=pbf(6)[:, hp * 128:(hp + 1) * 128], in_=BK[:, hp, 1, tk],
                                                        identity=identB), ['BK', 'cst'], wb6, inc=False)
                P.op('pe', lambda e, hp=hp: e.transpose(out=pbf(6)[:, 512 + hp * 128:512 + (hp + 1) * 128],
                                                        in_=VB[:, hp, 0, tk], identity=identB), ['VB', 'cst'], wb6, inc=(hp == 3))
            ACT(AL[par][:, :, 0:64], h64(pbf(5)[:, 0:512]), AF.Copy, wb5, allg('ALa%d' % par))
            ACT(RB[par][:, :, 64:128], h64(pbf(5)[:, 512:1024]), AF.Copy, wb5, allg('RBb%d' % par))
            ACT(Ktm[par][:, :, :], h64(pbf(6)[:, 0:512]), AF.Copy, wb6, allg('Ktm%d' % par))
            ACT(Vtm[par][:, :, :], h64(pbf(6)[:, 512:1024]), AF.Copy, wb6, allg('Vtm%d' % par))

        def st_S(i, hd):
            par = i % 2
            hr = hrs[hd]
            b1, b2, b3 = S1B[hd], S2B[hd], NTB[hd]
            t1, t2, t3 = bk(b1), bk(b2), bk(b3)
            for hp in range(4):
                for cp in range(2):
                    cr = crs[cp]
                    t64 = slice(i * 128 + cp * 64, i * 128 + cp * 64 + 64)
                    last = (hp == 3 and cp == 1)
                    cs_ = slice(hp * 128, hp * 128 + 128)
                    MM(banks[b1][cr, cs_], BK[hr, hp, 0, t64], AR[hr, hp, :, t64], ['BK', 'AR'], t1, inc=last)
                    MM(banks[b2][cr, cs_], BK[hr, hp, 1, t64], AR[hr, hp, :, t64], ['BK', 'AR'], t2, inc=last)
                    MM(banks[b3][cr, hp * 64:hp * 64 + 64], AR[hr, hp, 0, t64], BK[hr, hp, 0, t64], ['BK', 'AR'], t3, inc=last)
            TT('dve', hv(NY[0], hd)[:, :, 0:64], h128(banks[b1][:, :])[:, :, 0:64], mS1, ALU.mult, t1 + ['cst'], both('NY0n', hd))
            TT('dve', hv(NTs[0], hd), h64(banks[b3][:, 0:256]), mL4, ALU.mult, t3 + ['cst'], both('NT0', hd))
            TT('pool', hv(NY[1], hd)[:, :, 64:128], hv(NY[0], hd)[:, :, 0:64], id4, ALU.add, both('NY0n', hd) + ['cst'], both('NY1y', hd))
            TT('dve', hv(RB[par], hd)[:, :, 0:64], h128(banks[b1][:, :])[:, :, 64:128], mS2, ALU.mult, t1 + ['cst'], both('RBr%d' % par, hd))
            TT('dve', hv(S2m[par], hd), h128(banks[b2][:, :]), mSf, ALU.mult, t2 + ['cst'], both('S2m%d' % par, hd))

        def st_N(i, lv, cp, hd):
            cr = crs[cp]
            pb, qb = PBK(cp, hd), QBK(cp, hd)
            tP, tQ = bk(pb), bk(qb)
            qc = lambda hp: slice(hp * 64, hp * 64 + 64)
            if lv == 0:
                rn, rt = gt_('NY0n', cp, hd), gt_('NT0', cp, hd)
                for hp in range(4):
                    h = 2 * hp + hd
                    MM(banks[pb][cr, hp * 128:hp * 128 + 64], NTs[0][cr, h, :], NY[0][cr, h, 0:64], [rn, rt], tP, inc=(hp == 3))
                    MM(banks[qb][cr, qc(hp)], NY[0][cr, h, 0:64], NTs[0][cr, h, :], [rn, rt], tQ, inc=(hp == 3))
                ACT(hv(NY[1], hd)[cr, :, 0:64], h128(banks[pb][cr, :])[:, :, 0:64], AF.Copy, tP, [gt_('NY1n', cp, hd)])
                ACT(hv(NTs[1], hd)[cr, :, :], h64(banks[qb][cr, 0:256]), AF.Copy, tQ, [gt_('NT1', cp, hd)])
                return
            a_, b_ = lv % 2, (lv + 1) % 2
            NYa, NYb, NTa, NTb = NY[a_], NY[b_], NTs[a_], NTs[b_]
            rn, ry, rt = gt_('NY%dn' % a_, cp, hd), gt_('NY%dy' % a_, cp, hd), gt_('NT%d' % a_, cp, hd)
            wn, wy, wt = gt_('NY%dn' % b_, cp, hd), gt_('NY%dy' % b_, cp, hd), gt_('NT%d' % b_, cp, hd)
            for hp in range(4):
                h = 2 * hp + hd
                if lv < 5:
                    MM(banks[pb][cr, hp * 128:hp * 128 + 128], NTa[cr, h, :], NYa[cr, h, :], [rn, ry, rt], tP, inc=(hp == 3))
                    MM(banks[qb][cr, qc(hp)], NYa[cr, h, 0:64], NTa[cr, h, :], [rn, rt], tQ, inc=(hp == 3))
                else:
                    MM(banks[pb][cr, hp * 128 + 64:hp * 128 + 128], NTa[cr, h, :], NYa[cr, h, 64:128], [ry, rt], tP, inc=(hp == 3))
            bv = h128(banks[pb][cr, :])
            if lv < 5:
                ACT(hv(NYb, hd)[cr, :, 0:64], bv[:, :, 0:64], AF.Copy, tP, [wn])
                TT('dve', hv(NYb, hd)[cr, :, 64:128], bv[:, :, 64:128], hv(NYa, hd)[cr, :, 64:128], ALU.add, tP + [ry], [wy])
                ACT(hv(NTb, hd)[cr, :, :], h64(banks[qb][cr, 0:256]), AF.Copy, tQ, [wt])
            else:
                TT('dve', hv(Yf, hd)[cr, :, :], bv[:, :, 64:128], hv(NYa, hd)[cr, :, 64:128], ALU.add, tP + [ry], [gt_('Yf', cp, hd)])

        def st_L(i, cp, hd):
            par = i % 2
            cr = crs[cp]
            qb = QBK(cp, hd)
            tQ = bk(qb)
            for hp in range(4):
                h = 2 * hp + hd
                MM(banks[qb][cr, hp * 64:hp * 64 + 64], S2m[par][cr, h, 0:64], Vtm[par][cr, h, :],
                   [gt_('S2m%d' % par, cp, hd), gt_('Vtm%d' % par, cp, hd)], tQ, inc=(hp == 3))
            ACT(hv(AL[par], hd)[cr, :, 64:128], h64(banks[qb][cr, 0:256]), AF.Copy, tQ, [gt_('ALl%d' % par, cp, hd)])

        def st_AU(i, cp, hd):
            par = i % 2
            cr = crs[cp]
            pb = PBK(cp, hd)
            tP = bk(pb)
            for hp in range(4):
                h = 2 * hp + hd
                MM(banks[pb][cr, hp * 128:hp * 128 + 128], Yf[cr, h, :], AL[par][cr, h, :],
                   [gt_('Yf', cp, hd), gt_('ALa%d' % par, cp, hd), gt_('ALl%d' % par, cp, hd)], tP, inc=(hp == 3))
            ACT(hv(AUs[par], hd)[cr, :, :], h128(banks[pb][cr, :]), AF.Copy, tP, [gt_('AUs%d' % par, cp, hd)])

        def st_E(i, cp, hd):
            par = i % 2
            cr, hr = crs[cp], hrs[hd]
            pb = PBK(cp, hd)
            tP = bk(pb)
            t64 = slice(i * 128 + cp * 64, i * 128 + cp * 64 + 64)
            for hp in range(4):
                h = 2 * hp + hd
                MM(banks[pb][hr, hp * 128:hp * 128 + 128], AUs[par][cr, h, 0:64], RB[par][cr, h, :],
                   [gt_('AUs%d' % par, cp, hd), gt_('RBr%d' % par, cp, hd), gt_('RBb%d' % par, cp, hd)], tP, inc=(hp == 3))
            ev = h128(banks[pb][hr, :])
            TT('dve', RhT[par][hr, :, cp, :], ev[:, :, 0:64], AR[hr, :, 1, t64], ALU.add, tP + ['AR'], [gt_('RhT%d' % par, cp, hd)])
            idb = ident2[hr, :].rearrange("p (o x) -> p o x", o=1).to_broadcast([64, 4, 64])
            TT('dve', GT[par][hr, :, cp, :], ev[:, :, 64:128], idb, ALU.add, tP + ['cst'], [gt_('GT%d' % par, cp, hd)])

        def st_OQ(i, cp, hd):
            par = i % 2
            cr, hr = crs[cp], hrs[hd]
            c = 2 * i + cp
            ob_, qb = PBK(cp, hd), QBK(cp, hd)
            tO, tQ = bk(ob_), bk(qb)
            rA = [gt_('AUs%d' % par, cp, hd), gt_('RBr%d' % par, cp, hd), gt_('RBb%d' % par, cp, hd)]
            rV = [gt_('Vtm%d' % par, cp, hd), gt_('S2m%d' % par, cp, hd), gt_('Ktm%d' % par, cp, hd)]
            for hp in range(4):
                h = 2 * hp + hd
                col = slice(hp * 64, hp * 64 + 64)
                MM(banks[ob_][hr, col], AUs[par][cr, h, 64:128], RB[par][cr, h, 0:64], rA, tO, start=True, stop=False, inc=False)
                MM(banks[ob_][hr, col], Vtm[par][cr, h, :], S2m[par][cr, h, 64:128], rV, tO, start=False, stop=True, inc=(hp == 3))
                MM(banks[qb][hr, col], RB[par][cr, h, 64:128], AUs[par][cr, h, 64:128], rA, tQ, start=True, stop=False, inc=False)
                MM(banks[qb][hr, col], Ktm[par][cr, h, :], Vtm[par][cr, h, :], rV, tQ, start=False, stop=True, inc=(hp == 3))
            ACT(O0s[par][hr, :, cp, :], h64(banks[ob_][hr, 0:256]), AF.Copy, tO, [gt_('O0s%d' % par, cp, hd)])
            wc1 = WC4[hr, :, c, :].to_broadcast([64, 4, 64])
            TT('dve', Qs[par][hr, :, cp, :], h64(banks[qb][hr, 0:256]), wc1, ALU.mult, tQ + ['WC'], [gt_('Qs%d' % par, cp, hd)])

        o4 = hcx(ot)
        h3 = h64(htmp)

        def st_ST(i, cp, hd):
            par = i % 2
            hr = hrs[hd]
            c = 2 * i + cp
            Hc, Hn = Hb[c % 2], Hb[(c + 1) % 2]
            tc_, tn = 'Hb%d_%d' % (c % 2, hd), 'Hb%d_%d' % ((c + 1) % 2, hd)
            sb = STB[hd]
            tB = bk(sb)
            for hp in range(4):
                MM(banks[sb][hr, hp * 64:hp * 64 + 64], Hc[hr, hp, :], RhT[par][hr, hp, cp, :],
                   [tc_, gt_('RhT%d' % par, cp, hd)], tB, inc=False)
                MM(banks[sb][hr, 256 + hp * 64:256 + hp * 64 + 64], GT[par][hr, hp, cp, :], Hc[hr, hp, :],
                   [tc_, gt_('GT%d' % par, cp, hd)], tB, inc=(hp == 3))
            b7 = banks[sb][hr, :].rearrange("p (z h x) -> p z h x", z=2, h=4)
            TT('dve', o4[hr, :, cp, :], b7[:, 0, :, :], O0s[par][hr, :, cp, :], ALU.add, tB + [gt_('O0s%d' % par, cp, hd)], ['ot_%d' % hd])
            wc1 = WC4[hr, :, c, :].to_broadcast([64, 4, 64])
            TT('dve', h3[hr, :, :], b7[:, 1, :, :], wc1, ALU.mult, tB + ['WC'], ['htmp_%d' % hd])
            TT('pool', Hn[hr, :, :], h3[hr, :, :], Qs[par][hr, :, cp, :], ALU.add, ['htmp_%d' % hd, gt_('Qs%d' % par, cp, hd)], [tn])

        def st_GN(i):
            tk = slice(i * 128, (i + 1) * 128)
            rot = ['ot_0', 'ot_1']
            ACT(ob, ot, AF.Copy, rot, ['ob'])
            TT('pool', osqb, ot, ot, ALU.mult, rot, ['osqb'])
            MM(banks[1][:, :], bonesB, ob, ['cst', 'ob'], bk(1))
            MM(banks[2][:, :], bonesB, osqb, ['cst', 'osqb'], bk(2))
            TS('dve', rs, banks[1][:, :], 1.0 / 64, None, ALU.mult, ALU.bypass, bk(1), ['rs'])
            TT('dve', dd, ot, rs, ALU.subtract, rot + ['rs'], ['dd'])
            TT('pool', rs, rs, rs, ALU.mult, ['rs'], ['rs'])
            TS('dve', osq, banks[2][:, :], 1.0 / 64, 64e-5, ALU.mult, ALU.add, bk(2), ['osq'])
            TT('pool', rs, osq, rs, ALU.subtract, ['osq', 'rs'], ['rs'])
            ACT(rs, rs, AF.Ln, ['rs'], ['rs'])
            ACT(rs, rs, AF.Exp, ['rs'], ['rs'], scale=-0.5)
            for hp in range(4):
                MM(banks[1][:, hp * 128:(hp + 1) * 128], gupb[:, hp * 128:(hp + 1) * 128], sg[:, tk], ['gupb', 'sg'], bk(1),
                   inc=(hp == 3))
            TT('pool', dd, dd, rs, ALU.mult, ['dd', 'rs'], ['dd'])
            d3 = dd.rearrange("p (h x) -> p h x", x=128)
            TT('pool', d3, d3, lnw4.to_broadcast([128, 4, 128]), ALU.mult, ['dd', 'par'], ['dd'])
            TT('pool', d3, d3, lnb4.to_broadcast([128, 4, 128]), ALU.add, ['dd', 'par'], ['dd'])
            TT('pool', d3, d3, VB[:, :, 1, tk], ALU.add, ['dd', 'VB'], ['dd'])
            TT('dve', yrT[:, :, tk], d3, h128(banks[1][:, :]), ALU.mult, ['dd'] + bk(1), ['yrT'])

        def P_stages(i):
            st = [lambda: st_T(i), lambda: (st_S(i, 0), st_S(i, 1))]
            for lv in range(6):
                st.append(lambda lv=lv: [st_N(i, lv, cp, hd) for cp, hd in G4])
            st.append(lambda: [st_L(i, cp, hd) for cp, hd in G4])
            st.append(lambda: [st_AU(i, cp, hd) for cp, hd in G4])
            st.append(lambda: [st_E(i, cp, hd) for cp, hd in G4])
            st.append(lambda: [st_OQ(i, cp, hd) for cp, hd in G4])
            return st

        def Q_stages(i):
            return [lambda: (st_ST(i, 0, 0), st_ST(i, 0, 1)), lambda: (st_ST(i, 1, 0), st_ST(i, 1, 1)), lambda: st_GN(i)]

        import os as _os
        if _os.environ.get('NOPIPE'):
            _ds = int(_os.environ.get('DSTOP', '999'))
            for i in range(NTILE):
                for k_, f in enumerate(P_stages(i) + Q_stages(i)):
                    f()
                    if i == 0 and k_ == _ds:
                        P.finish()
                        return nc, dbg_out
        else:
            for f in P_stages(0):
                f()
            for i in range(NTILE):
                ps = P_stages(i + 1) if i + 1 < NTILE else []
                qs = Q_stages(i)
                _k = int(_os.environ.get('PIPEK', '1'))
                slots = None
                for k, f in enumerate(ps):
                    f()
                    if slots is None:
                        if k == _k:
                            for q_ in qs:
                                q_()
                    elif k in slots:
                        qs[slots[k]]()
                if not ps:
                    for f in qs:
                        f()
        dump('yrT', yrT[:, 0, :], [128, T], BF16, reads=['yrT'])
        if stop == 'D':
            P.finish()
            return nc, dbg_out
        P.barrier()
        h1 = A.f32(16 * D, at=pers0).rearrange("p (i d) -> p i d", d=D)
        o1 = pers0 + 16 * D
        wob = A.bf(8 * D, at=o1).rearrange("p (k c) -> p k c", c=D)
        wos = [A.f32(D, at=o1 + 4096 + s * D) for s in range(2)]
        xs = [A.f32(D, at=o1 + 4096 + 2 * D + s * D) for s in range(2)]
        assert o1 + 4096 + 4 * D <= pers0 + 24576
        def xload(i):
            P.dma(h1[:, i, :], x_d[i * 128:(i + 1) * 128, :], writes=['h1_%d' % i])
        for k in range(8):
            P.dma(wos[k % 2], wout_d[:, k * D:(k + 1) * D], writes=['wos%d' % (k % 2)])
            P.op('pool', lambda e, k=k: e.tensor_copy(out=wob[:, k, :], in_=wos[k % 2]), ['wos%d' % (k % 2)], ['wob'])
            if k >= 1:
                xload(2 * (k - 1))
                xload(2 * (k - 1) + 1)
        xload(14)
        xload(15)
        for i in range(NTILE):
            tk = slice(i * 128, (i + 1) * 128)
            for half in range(2):
                b = (2 * i + half) % 4
                for k in range(8):
                    lhs = ycT[:, k, tk] if k < 4 else yrT[:, k - 4, tk]
                    MM(banks[b][:, :], lhs, wob[:, k, half * 512:(half + 1) * 512], ['ycT', 'yrT', 'wob'], [BT(b)],
                       start=(k == 0), stop=(k == 7), inc=(k == 7))
                TT('dve', h1[:, i, half * 512:(half + 1) * 512], banks[b][:, :], h1[:, i, half * 512:(half + 1) * 512],
                   ALU.add, [BT(b), 'h1_%d' % i], ['h1_%d' % i])
        dump('h1', h1[:, 0, :], [128, D], F32, reads=['h1_0'])
        if stop == 'E':
            P.finish()
            return nc, dbg_out
        P.barrier()

        stg = [A.f32(2048, at=o1 + s * 2048) for s in range(3)]
        wb = [[A.bf(2048, at=o1 + 6144 + (s2 * 3 + s) * 1024) for s in range(3)] for s2 in range(2)]
        assert o1 + 6144 + 6144 <= work0 - 512
        srcs = (wg_d, wu_d, wd_d)

        def moe_dma(ex):
            for s in range(3):
                P.dma(stg[s], srcs[s][ex, :, :], writes=['stg%d' % s])

        def moe_cvt(ex):
            s2 = ex % 2
            for s in range(3):
                if s != 1:
                    P.op('pool', lambda e, s=s, s2=s2: e.tensor_copy(out=wb[s2][s], in_=stg[s]), ['stg%d' % s], ['wb%d_%d' % (s2, s)])
                else:
                    ACT(wb[s2][s], stg[s], AF.Copy, ['stg%d' % s], ['wb%d_%d' % (s2, s)])

        moe_dma(0)
        moe_cvt(0)
        u2T = A.bf(8 * T, at=base0).rearrange("p (k t) -> p k t", t=T)
        P.dma(gbc, g2_d.partition_broadcast(128), writes=['gbc'])
        rms_stats(lambda i: h1[:, i, :], 'h1_')
        norm_transpose(lambda i: h1[:, i, :], 'h1_', u2T, 'u2T')
        rtop = [work0 + 2700]

        def ralloc(n):
            ap = A.f32(n, at=rtop[0])
            rtop[0] += n
            return ap

        wrs = ralloc(288)
        wrb = A.bf(288, at=rtop[0]).rearrange("p (k c) -> p k c", c=36)
        rtop[0] += 144
        brbc = ralloc(36)
        lg = ralloc(16 * 36).rearrange("p (i c) -> p i c", c=36)
        gmax = ralloc(16)
        gsh = ralloc(64).rearrange("p (i c) -> p i c", c=4)
        gsum = ralloc(16)
        gp = ralloc(16)
        oh = ralloc(64).rearrange("p (i c) -> p i c", c=4)
        elm = ralloc(512).rearrange("p (i c) -> p i c", c=32)
        elm2 = ralloc(512).rearrange("p (i c) -> p i c", c=32)
        e1 = ralloc(512).rearrange("p (i c) -> p i c", c=32)
        e2 = ralloc(512).rearrange("p (i c) -> p i c", c=32)
        comb = ralloc(512).rearrange("p (i c) -> p i c", c=32)
        m1 = ralloc(16)
        m2 = ralloc(16)
        dm = ralloc(16)
        p1 = ralloc(16)
        p2 = ralloc(16)
        assert rtop[0] <= NW
        P.dma(wrs, wr_d[:, :], writes=['wrs'])
        P.dma(brbc, br_d.partition_broadcast(128), writes=['brbc'])
        P.op('pool', lambda e: e.tensor_copy(out=wrb[:, :, :].rearrange("p k c -> p (k c)"), in_=wrs), ['wrs'], ['wrb'])
        for i in range(NTILE):
            tk = slice(i * 128, (i + 1) * 128)
            b = 4 + i // 8
            for k in range(8):
                MM(banks[b][:, (i % 8) * 36:(i % 8) * 36 + 36], u2T[:, k, tk], wrb[:, k, :], ['u2T', 'wrb'], [BT(b)],
                   start=(k == 0), stop=(k == 7), inc=(k == 7))
        bb3 = brbc.rearrange("p (o c) -> p o c", o=1).to_broadcast([128, 8, 36])
        for hb in range(2):
            TT('dve', lg[:, hb * 8:(hb + 1) * 8, :], banks[4 + hb][:, 0:288].rearrange("p (i c) -> p i c", c=36), bb3,
               ALU.add, [BT(4 + hb), 'brbc'], ['lg'])
        dump('lg', lg[:, :, :].rearrange("p i c -> p (i c)"), [128, 576], F32, reads=['lg'])
        gl = lg[:, :, 0:4]
        el = lg[:, :, 4:36]
        b3 = lambda ap, n: ap.rearrange("p (i o) -> p i o", o=1).to_broadcast([128, 16, n])
        P.op('dve', lambda e: e.tensor_reduce(out=gmax, in_=gl, axis=AX.X, op=ALU.max), ['lg'], ['gmax'])
        TT('dve', gsh, gl, b3(gmax, 4), ALU.subtract, ['lg', 'gmax'], ['gsh'])
        TT('dve', oh, gl, b3(gmax, 4), ALU.is_equal, ['lg', 'gmax'], ['oh'])
        ACT(gsh, gsh, AF.Exp, ['gsh'], ['gsh'])
        P.op('dve', lambda e: e.tensor_reduce(out=gsum, in_=gsh, axis=AX.X, op=ALU.add), ['gsh'], ['gsum'])
        P.op('dve', lambda e: e.reciprocal(out=gp, in_=gsum), ['gsum'], ['gp'])
        TS('dve', oh, oh, 1e30, -1e30, ALU.mult, ALU.add, ['oh'], ['oh'])
        TT('dve', elm.rearrange("p i (g x) -> p i g x", g=4), el.rearrange("p i (g x) -> p i g x", g=4),
           oh.rearrange("p i (g o) -> p i g o", o=1).to_broadcast([128, 16, 4, 8]), ALU.add, ['lg', 'oh'], ['elm'])
        P.op('dve', lambda e: e.tensor_reduce(out=m1, in_=elm, axis=AX.X, op=ALU.max), ['elm'], ['m1'])
        TT('dve', e1, elm, b3(m1, 32), ALU.is_equal, ['elm', 'm1'], ['e1'])
        STT(elm2, e1, -1e30, elm, ALU.mult, ALU.add, ['e1', 'elm'], ['elm2'])
        P.op('dve', lambda e: e.tensor_reduce(out=m2, in_=elm2, axis=AX.X, op=ALU.max), ['elm2'], ['m2'])
        TT('dve', e2, elm2, b3(m2, 32), ALU.is_equal, ['elm2', 'm2'], ['e2'])
        TT('dve', dm, m2, m1, ALU.subtract, ['m1', 'm2'], ['dm'])
        ACT(dm, dm, AF.Exp, ['dm'], ['dm'])
        TS('dve', p1, dm, 1.0, None, ALU.add, ALU.bypass, ['dm'], ['p1'])
        P.op('dve', lambda e: e.reciprocal(out=p1, in_=p1), ['p1'], ['p1'])
        TT('dve', p2, dm, p1, ALU.mult, ['dm', 'p1'], ['p2'])
        TT('dve', p1, p1, gp, ALU.mult, ['p1', 'gp'], ['p1'])
        TT('dve', p2, p2, gp, ALU.mult, ['p2', 'gp'], ['p2'])
        TT('dve', comb, e1, b3(p1, 32), ALU.mult, ['e1', 'p1'], ['comb'])
        TT('dve', e2, e2, b3(p2, 32), ALU.mult, ['e2', 'p2'], ['e2'])
        TT('dve', comb, comb, e2, ALU.add, ['comb', 'e2'], ['comb'])
        dump('comb', comb[:, :, :].rearrange("p i c -> p (i c)"), [128, 512], F32, reads=['comb'])
        if stop == 'F':
            P.finish()
            return nc, dbg_out
        P.barrier()

        m0 = base0 + 8192
        sil = [A.f32(512, at=m0 + s * 512) for s in range(2)]
        hid = [A.bf(512, at=m0 + 1024 + s * 256) for s in range(2)]
        assert m0 + 1536 <= pers0
        hid4 = [[A.bf(512, at=m0 + 1024 + (bp * 2 + s) * 256) for s in range(2)] for bp in range(2)]
        assert m0 + 2048 <= pers0

        def moe_G(ex, tb):
            s2 = ex % 2
            bp = (ex * 4 + tb) % 2
            wgb = wb[s2][0].rearrange("p (k c) -> p k c", c=256)
            wub = wb[s2][1].rearrange("p (k c) -> p k c", c=256)
            tg, tu = 'wb%d_0' % s2, 'wb%d_1' % s2
            tsl = slice(tb * 512, (tb + 1) * 512)
            for dch in range(2):
                dc = slice(dch * 128, (dch + 1) * 128)
                bg, bu = 2 * dch, 2 * dch + 1
                for k in range(8):
                    MM(banks[bg][:, :], wgb[:, k, dc], u2T[:, k, tsl], [tg, 'u2T'], [BT(bg)], start=(k == 0), stop=(k == 7), inc=(k == 7))
                for k in range(8):
                    MM(banks[bu][:, :], wub[:, k, dc], u2T[:, k, tsl], [tu, 'u2T'], [BT(bu)], start=(k == 0), stop=(k == 7), inc=(k == 7))
                ACT(sil[dch], banks[bg][:, :], AF.Silu, [BT(bg)], ['sil%d' % dch])
                TT('dve', hid4[bp][dch], sil[dch], banks[bu][:, :], ALU.mult, ['sil%d' % dch, BT(bu)], ['hid%d_%d' % (bp, dch)])

        tmpE = [A.f32(512, at=work0 + q_ * 512) for q_ in range(4)]
        tctr = [0]
        OFFL = ((0, 1), (2, 0), (3, 0))

        def final_group(g):
            gs = slice(4 * g, 4 * g + 4)
            for i in range(4 * g, 4 * g + 4):
                P.op('act', lambda e, i=i: e.activation(out=junk, in_=h1[:, i, :], func=AF.Square, accum_out=ss[:, i:i + 1]),
                     reads=['h1_%d' % i], writes=['junk', 'ssg%d' % g])
            TS('dve', rstd[:, gs], ss[:, gs], 1.0 / D, 1e-6, ALU.mult, ALU.add, ['ssg%d' % g], ['rstdg%d' % g])
            ACT(rstd[:, gs], rstd[:, gs], AF.Ln, ['rstdg%d' % g], ['rstdg%d' % g])
            ACT(rstd[:, gs], rstd[:, gs], AF.Exp, ['rstdg%d' % g], ['rstdg%d' % g], scale=-0.5)
            for i in range(4 * g, 4 * g + 4):
                STT(h1[:, i, :], h1[:, i, :], rstd[:, i:i + 1], gbc, ALU.mult, ALU.mult, ['h1_%d' % i, 'rstdg%d' % g, 'gbc'], ['h1_%d' % i])
                P.dma(out_d[i * 128:(i + 1) * 128, :], h1[:, i, :], reads=['h1_%d' % i])

        def moe_D(ex, tb):
            s2 = ex % 2
            bp = (ex * 4 + tb) % 2
            wdb = wb[s2][2].rearrange("p (d c) -> p d c", c=D)
            td = 'wb%d_2' % s2
            for ti in range(4):
                i = tb * 4 + ti
                for half in range(2):
                    b = 4 + (ti % 2) * 2 + half
                    for dch in range(2):
                        MM(banks[b][:, :], hid4[bp][dch][:, ti * 128:(ti + 1) * 128], wdb[:, dch, half * 512:(half + 1) * 512],
                           ['hid%d_%d' % (bp, dch), td], [BT(b)], start=(dch == 0), stop=(dch == 1), inc=(dch == 1))
                    hsl = h1[:, i, half * 512:(half + 1) * 512]
                    if (ti, half) in OFFL:
                        q_ = tctr[0] % 4
                        tctr[0] += 1
                        ACT(tmpE[q_], banks[b][:, :], AF.Copy, [BT(b), 'comb'], ['tmpE%d' % q_], scale=comb[:, i, ex:ex + 1])
                        TT('pool', hsl, hsl, tmpE[q_], ALU.add, ['tmpE%d' % q_, 'h1_%d' % i], ['h1_%d' % i])
                    else:
                        STT(hsl, banks[b][:, :], comb[:, i, ex:ex + 1], hsl, ALU.mult, ALU.add, [BT(b), 'comb', 'h1_%d' % i], ['h1_%d' % i])

        blocks = [(ex, tb) for ex in range(NE) for tb in range(4)]
        P.dma(gbc, g3_d.partition_broadcast(128), writes=['gbc'])
        moe_G(0, 0)
        for n, (ex, tb) in enumerate(blocks):
            if tb == 0 and ex + 1 < NE:
                moe_dma(ex + 1)
            if tb == 2 and ex + 1 < NE:
                moe_cvt(ex + 1)
            if n + 1 < len(blocks):
                moe_G(*blocks[n + 1])
            moe_D(ex, tb)
            if ex == NE - 1:
                final_group(tb)
        P.finish()
    return nc, dbg_out


_CACHE = {}


def _consts():
    c = np.zeros((128, 1024), np.float32)
    c[:, 0:128] = np.eye(128, dtype=np.float32)
    p = np.arange(128)
    blk = (p[:, None] // 64 == p[None, :] // 64).astype(np.float32)
    c[:, 128:256] = blk / 64.0
    import ml_dtypes
    cb = np.zeros((128, 1536), ml_dtypes.bfloat16)
    cb[:, 0:128] = np.eye(128)
    cb[:, 128:256] = blk
    s = (p % 64)[:, None]
    t = np.arange(64)[None, :]
    cb[:, 256:320] = (s < t)
    cb[:, 320:384] = (s <= t)
    cb[:, 384:448] = (t < s)
    cb[:, 448:512] = (t == s)
    c[:, 256:1024] = np.ascontiguousarray(cb).view(np.float32)
    return c


def _prep_shared(inp):
    f = lambda a: np.ascontiguousarray(a, dtype=np.float32)
    w_in = inp["w_in"][0]
    wt = np.stack([w_in[:, c * 128:(c + 1) * 128].reshape(8, 128, 128).transpose(1, 0, 2).reshape(128, 1024)
                   for c in ORDER], 0)
    par = np.zeros((128, NPAR), np.float32)
    mu = inp["rwkv_mu"][0]
    for q in range(14):
        par[:, PC_MU + q] = mu[q * 128:(q + 1) * 128]
    cw = inp["conv_w"][0]
    for tap in range(3):
        for j in range(4):
            par[:, PC_CW + tap * 4 + j] = cw[tap, j * 128:(j + 1) * 128]
    for nm, col in (("decay_base", PC_DB), ("aaa_base", PC_AB), ("k_k", PC_KK), ("k_a", PC_KA),
                    ("ln_x_w", PC_LW), ("ln_x_b", PC_LB)):
        v = inp[nm][0]
        for hp in range(4):
            par[:, col + hp] = v[hp * 128:(hp + 1) * 128]
    rk = inp["r_k"][0].reshape(512)
    for hp in range(4):
        par[:, PC_RK + hp] = rk[hp * 128:(hp + 1) * 128]
    sh = {
        "w_in_t": f(wt),
        "params": par,
        "lora_up": f(np.concatenate([inp["decay_up"][0], inp["aaa_up"][0]], 0)),
        "gate_up": f(inp["gate_up"][0]),
        "g_mix": f(inp["norm_mix_g"][0]),
        "g_ffn": f(inp["norm_ffn_g"][0]),
        "g_fin": f(inp["norm_final_g"]),
        "w_out_t": f(inp["w_out"][0].reshape(8, 128, 1024).transpose(1, 0, 2).reshape(128, 8192)),
        "w_r_t": f(np.concatenate([inp["router_group_w"][0], inp["router_expert_w"][0]], 1)
                   .reshape(8, 128, 36).transpose(1, 0, 2).reshape(128, 288)),
        "b_r": f(np.concatenate([inp["router_group_b"][0], inp["router_expert_b"][0]], 0)),
        "wg_t": f(inp["expert_w_gate"][0].reshape(NE, 8, 128, 256).transpose(0, 2, 1, 3).reshape(NE, 128, 2048)),
        "wu_t": f(inp["expert_w_up"][0].reshape(NE, 8, 128, 256).transpose(0, 2, 1, 3).reshape(NE, 128, 2048)),
        "wd_t": f(inp["expert_w_down"][0].reshape(NE, 2, 128, 1024).transpose(0, 2, 1, 3).reshape(NE, 128, 2048)),
        "consts": _consts(),
    }
    return sh


def run(inp, n_cores=8, stop=None, dbg=(), trace=False):
    key = (stop, tuple(dbg))
    if key not in _CACHE:
        _CACHE[key] = build(stop=stop, dbg=dbg)
    nc, dbg_out = _CACHE[key]
    sh = _prep_shared(inp)
    x = np.asarray(inp["x"], dtype=np.float32)
    in_maps = []
    for c in range(n_cores):
        m = dict(sh)
        m["x"] = np.ascontiguousarray(x[c])
        in_maps.append(m)
    res = run_bass_kernel_spmd(nc, in_maps, core_ids=list(range(n_cores)), **({"trace": True} if trace else {}))
    return res


def kernel(**inputs):
    inp = {k: np.asarray(v) for k, v in inputs.items()}
    res = run(inp, n_cores=8)
    out = np.stack([np.asarray(res.results[c]["out"], dtype=np.float32) for c in range(8)], 0)
    return out
```

```python
import numpy as np
from contextlib import ExitStack
import concourse.bass as bass
import concourse.mybir as mybir
from concourse.bass_utils import run_bass_kernel_spmd

F32 = mybir.dt.float32
BF16 = mybir.dt.bfloat16
AF = mybir.ActivationFunctionType
ALU = mybir.AluOpType
AX = mybir.AxisListType

T = 2048
D = 1024
NTILE = 16
NE = 32
C0 = float(np.exp(-0.5))
ORDER = [24, 25, 12, 16, 20, 13, 17, 21, 14, 18, 22, 15, 19, 23,
         0, 4, 8, 1, 5, 9, 2, 6, 10, 3, 7, 11]
PC_MU = 0
PC_CW = 14
PC_DB = 26
PC_AB = 30
PC_KK = 34
PC_KA = 38
PC_RK = 42
PC_LW = 46
PC_LB = 50
NPAR = 54


class Prog:
    ENG = ('pe', 'act', 'dve', 'pool', 'sp')

    def __init__(self, nc, es, n_dma=24):
        self.nc = nc
        self.q = {e: [] for e in self.ENG}
        self.sem = {e: es.enter_context(nc.semaphore("s_" + e)) for e in ('pe', 'act', 'dve', 'pool')}
        self.dsem = [es.enter_context(nc.semaphore("d%d" % i)) for i in range(n_dma)]
        self.cnt = {e: 0 for e in ('pe', 'act', 'dve', 'pool')}
        self.dval = [0] * n_dma
        self.drr = 0
        self.waited = {}
        self.lastw = {}
        self.readers = {}
        self.ninst = 0
        self.cap = None

    def _semh(self, key):
        return self.sem[key] if isinstance(key, str) else self.dsem[key]

    def _need(self, eng, deps):
        for key, val in deps.items():
            if key == eng and eng == 'pe':
                continue
            if self.waited.get((eng, key), 0) >= val:
                continue
            self.waited[(eng, key)] = val
            h = self._semh(key)
            self.q[eng].append(lambda e, h=h, val=val: e.wait_ge(h, val))

    def _collect(self, eng, reads, writes):
        deps = {}
        for t in list(reads) + list(writes):
            w = self.lastw.get(t)
            if w and deps.get(w[0], 0) < w[1]:
                deps[w[0]] = w[1]
        for t in writes:
            for k, v in self.readers.get(t, {}).items():
                if k == eng and isinstance(k, str):
                    continue
                if deps.get(k, 0) < v:
                    deps[k] = v
        return deps

    def _record(self, key, val, reads, writes):
        for t in reads:
            r = self.readers.setdefault(t, {})
            if r.get(key, 0) < val:
                r[key] = val
        for t in writes:
            self.lastw[t] = (key, val)
            self.readers[t] = {}

    def op(self, eng, fn, reads=(), writes=(), inc=True):
        if self.cap is not None:
            self.cap.append(('op', eng, fn, list(reads), list(writes), inc))
            return
        self.ninst += 1
        deps = self._collect(eng, reads, writes)
        self._need(eng, deps)
        if inc or eng != 'pe':
            self.cnt[eng] += 1
            val = self.cnt[eng]
            h = self.sem[eng]
            self.q[eng].append(lambda e, fn=fn, h=h: fn(e).then_inc(h, 1))
        else:
            val = self.cnt[eng] + 1
            self.q[eng].append(lambda e, fn=fn: fn(e))
        self._record(eng, val, reads, writes)

    def dma(self, out, in_, reads=(), writes=()):
        if self.cap is not None:
            self.cap.append(('dma', out, in_, list(reads), list(writes)))
            return
        self.ninst += 1
        eng = 'sp'
        j = self.drr
        self.drr = (self.drr + 1) % len(self.dsem)
        deps = self._collect(eng, reads, writes)
        if self.dval[j] > 0:
            deps[j] = max(deps.get(j, 0), self.dval[j])
        self._need(eng, deps)
        self.dval[j] += 16
        val = self.dval[j]
        h = self.dsem[j]
        self.q[eng].append(lambda e, h=h, out=out, in_=in_: e.dma_start(out=out, in_=in_).then_inc(h, 16))
        self._record(j, val, reads, writes)

    def capture(self, f):
        assert self.cap is None
        self.cap = []
        f()
        ops, self.cap = self.cap, None
        return ops

    def replay(self, *lists):
        lists = [l for l in lists if l]
        idx = [0] * len(lists)
        tot = max(len(l) for l in lists) if lists else 0
        for step in range(tot):
            for j, l in enumerate(lists):
                hi = (step + 1) * len(l) // tot
                while idx[j] < hi:
                    o = l[idx[j]]
                    idx[j] += 1
                    if o[0] == 'op':
                        self.op(o[1], o[2], o[3], o[4], o[5])
                    else:
                        self.dma(o[1], o[2], o[3], o[4])

    def barrier(self):
        deps = {k: v for k, v in self.cnt.items() if v > 0}
        for j, v in enumerate(self.dval):
            if v > 0:
                deps[j] = v
        for eng in self.ENG:
            d = {k: v for k, v in deps.items() if k != eng}
            self._need(eng, d)

    def finish(self):
        deps = {j: v for j, v in enumerate(self.dval) if v > 0}
        self._need('sp', deps)
        with self.nc.Block() as block:
            @block.tensor
            def _(e):
                for f in self.q['pe']:
                    f(e)

            @block.scalar
            def _(e):
                for f in self.q['act']:
                    f(e)

            @block.vector
            def _(e):
                for f in self.q['dve']:
                    f(e)

            @block.gpsimd
            def _(e):
                for f in self.q['pool']:
                    f(e)

            @block.sync
            def _(e):
                for f in self.q['sp']:
                    f(e)


class Arena:
    def __init__(self, big, nwords):
        self.big = big
        self.n = nwords
        self.top = 0

    def f32(self, n, at=None):
        if at is None:
            at = self.top
            self.top += n
        assert at + n <= self.n, (at, n, self.n)
        return self.big[:, at:at + n]

    def bf(self, n, at=None):
        w = (n + 1) // 2
        if at is None:
            at = self.top
            self.top += w
        assert at + w <= self.n, (at, w, self.n)
        return self.big[:, at:at + w].bitcast(BF16)


def build(stop=None, dbg=()):
    nc = bass.Bass("TRN2", target_bir_lowering=False)
    dram = {}

    def din(name, shape, dt=F32):
        dram[name] = nc.dram_tensor(name, list(shape), dt, kind="ExternalInput").ap()
        return dram[name]

    x_d = din("x", [T, D])
    win_d = din("w_in_t", [26, 128, 8 * 128])
    par_d = din("params", [128, NPAR])
    lup_d = din("lora_up", [128, 512])
    gup_d = din("gate_up", [128, 512])
    g1_d = din("g_mix", [D])
    g2_d = din("g_ffn", [D])
    g3_d = din("g_fin", [D])
    wout_d = din("w_out_t", [128, 8 * 1024])
    wr_d = din("w_r_t", [128, 8 * 36])
    br_d = din("b_r", [36])
    wg_d = din("wg_t", [NE, 128, 8 * 256])
    wu_d = din("wu_t", [NE, 128, 8 * 256])
    wd_d = din("wd_t", [NE, 128, 2 * 1024])
    cst_d = din("consts", [128, 1024])
    out_d = nc.dram_tensor("out", [T, D], F32, kind="ExternalOutput").ap()
    dbg_out = {}

    es = ExitStack()
    with es:
        NW = 51200
        big = es.enter_context(nc.sbuf_tensor("big", [128, NW], F32))
        banks = [es.enter_context(nc.psum_tensor("ps%d" % i, [128, 512], F32)) for i in range(8)]
        P = Prog(nc, es)
        A = Arena(big, NW)

        def dump(name, ap, shape, dt=F32, reads=()):
            if name not in dbg:
                return
            t = nc.dram_tensor("dbg_" + name, list(shape), dt, kind="ExternalOutput").ap()
            dbg_out[name] = t
            P.dma(t, ap, reads=reads)

        cst = A.f32(1024)
        par = A.f32(64)
        gbc = A.f32(1024)
        P.dma(cst, cst_d[:, :], writes=['cst'])
        P.dma(par[:, 0:NPAR], par_d[:, :], writes=['par'])
        identF = cst[:, 0:128]
        bonesF = cst[:, 128:256]
        cstb = cst[:, 256:1024].bitcast(BF16)
        identB = cstb[:, 0:128]
        bonesB = cstb[:, 128:256]
        omka = A.f32(4)
        hpar = A.f32(8)
        omm_ = A.f32(16)
        P.op('dve', lambda e: e.tensor_scalar(out=omka, in0=par[:, PC_KA:PC_KA + 4], scalar1=-1.0, scalar2=1.0,
                                               op0=ALU.mult, op1=ALU.add), reads=['par'], writes=['omka'])

        base0 = A.top
        uT = A.bf(8 * T).rearrange("p (k t) -> p k t", t=T)
        tha = A.bf(T)
        sg = A.bf(T)
        pers0 = A.top
        AR = A.bf(4 * 2 * T).rearrange("p (h x t) -> p h x t", h=4, x=2)
        BK = A.bf(4 * 2 * T).rearrange("p (h x t) -> p h x t", h=4, x=2)
        VB = A.bf(4 * 2 * T).rearrange("p (h x t) -> p h x t", h=4, x=2)
        WC = A.f32(128).rearrange("p (h c) -> p h c", h=4)
        pers1 = A.top
        wst = [A.f32(1024) for _ in range(2)]
        wbf = [A.bf(1024).rearrange("p (k c) -> p k c", c=128) for _ in range(4)]
        lupb = A.bf(512)
        gupb = A.bf(512)
        work0 = A.top

        xall = A.f32(16 * D, at=pers0).rearrange("p (i d) -> p i d", d=D)
        xn = [A.f32(D, at=work0 + i * D) for i in range(2)]
        junk = A.bf(D, at=work0 + 2 * D)
        ss = A.f32(16, at=work0 + 2 * D + 512)
        rstd = A.f32(16, at=work0 + 2 * D + 528)
        lst = A.f32(512, at=work0 + 2 * D + 1024)
        gst = A.f32(512, at=work0 + 2 * D + 1536)

        P.dma(gbc, g1_d.partition_broadcast(128), writes=['gbc'])
        P.dma(lst, lup_d[:, :], writes=['lst'])
        P.dma(gst, gup_d[:, :], writes=['gst'])
        P.op('pool', lambda e: e.tensor_copy(out=lupb, in_=lst), reads=['lst'], writes=['lupb'])
        P.op('pool', lambda e: e.tensor_copy(out=gupb, in_=gst), reads=['gst'], writes=['gupb'])

        def rms_stats(src, tag):
            for i in range(NTILE):
                P.op('act', lambda e, i=i: e.activation(out=junk, in_=src(i), func=AF.Square,
                                                        accum_out=ss[:, i:i + 1]),
                     reads=[tag + str(i)], writes=['junk', 'ss'])
            P.op('dve', lambda e: e.tensor_scalar(out=rstd, in0=ss, scalar1=1.0 / D, scalar2=1e-6,
                                                   op0=ALU.mult, op1=ALU.add), reads=['ss'], writes=['rstd'])
            P.op('act', lambda e: e.activation(out=rstd, in_=rstd, func=AF.Ln), reads=['rstd'], writes=['rstd'])
            P.op('act', lambda e: e.activation(out=rstd, in_=rstd, func=AF.Exp, scale=-0.5), reads=['rstd'], writes=['rstd'])

        for i in range(NTILE):
            P.dma(xall[:, i, :], x_d[i * 128:(i + 1) * 128, :], writes=['xa%d' % i])
        rms_stats(lambda i: xall[:, i, :], 'xa')

        def norm_transpose(src, tag, dstT, dst_tag):
            for i in range(NTILE):
                xb = xn[i % 2]
                P.op('dve', lambda e, i=i, xb=xb: e.scalar_tensor_tensor(
                    out=xb, in0=src(i), scalar=rstd[:, i:i + 1], in1=gbc, op0=ALU.mult, op1=ALU.mult),
                    reads=[tag + str(i), 'rstd', 'gbc'], writes=['xn%d' % (i % 2)])
                for half in range(2):
                    bk = banks[(2 * i + half) % 4]
                    btag = 'bank%d' % ((2 * i + half) % 4)
                    for kk in range(4):
                        k = half * 4 + kk
                        P.op('pe', lambda e, bk=bk, kk=kk, k=k, xb=xb: e.transpose(
                            out=bk[:, kk * 128:(kk + 1) * 128], in_=xb[:, k * 128:(k + 1) * 128], identity=identF),
                            reads=['xn%d' % (i % 2), 'cst'], writes=[btag], inc=(kk == 3))
                    P.op('act', lambda e, bk=bk, half=half, i=i: e.activation(
                        out=dstT[:, half * 4:half * 4 + 4, i * 128:(i + 1) * 128],
                        in_=bk[:, :].rearrange("p (k t) -> p k t", t=128), func=AF.Copy),
                        reads=[btag], writes=[dst_tag])

        norm_transpose(lambda i: xall[:, i, :], 'xa', uT, 'uT')
        dump('uT', uT[:, 0, :], [128, T], BF16, reads=['uT'])
        if stop == 'A':
            P.finish()
            return nc, dbg_out
        P.barrier()

        def ACT(out, in_, func, reads, writes, **kw):
            P.op('act', lambda e: e.activation(out=out, in_=in_, func=func, **kw), reads, writes)

        def TT(eng, out, a, b, op, reads, writes):
            P.op(eng, lambda e: e.tensor_tensor(out=out, in0=a, in1=b, op=op), reads, writes)

        def TS(eng, out, a, s1, s2, op0, op1, reads, writes):
            P.op(eng, lambda e: e.tensor_scalar(out=out, in0=a, scalar1=s1, scalar2=s2, op0=op0, op1=op1), reads, writes)

        def STT(out, a, s, b, op0, op1, reads, writes):
            P.op('dve', lambda e: e.scalar_tensor_tensor(out=out, in0=a, scalar=s, in1=b, op0=op0, op1=op1), reads, writes)

        def MM(out, lhsT, rhs, reads, writes, start=True, stop=True, inc=True):
            P.op('pe', lambda e: e.matmul(out, lhsT=lhsT, rhs=rhs, start=start, stop=stop), reads, writes, inc=inc)

        def BT(b):
            return 'bank%d' % b

        wk = {}
        wtop = [work0]

        def walloc(name, n=512, dt=F32):
            if dt == F32:
                wk[name] = A.f32(n, at=wtop[0])
                wtop[0] += n
            else:
                wk[name] = A.bf(n, at=wtop[0])
                wtop[0] += (n + 1) // 2
            return wk[name]

        for nm in ('r', 'k', 't1'):
            walloc(nm)
        chb = walloc('chb', 514)
        conv_end = wtop[0]
        zc = {}
        for nm in ('r', 'k', 'v'):
            zc[nm] = walloc('zc_' + nm, 514)
        dtile = walloc('d')
        smask = walloc('smask')
        for nm in ('v', 'sig', 'a', 'cs', 'winc', 'winv'):
            walloc(nm)
        sqb = walloc('sqb', 512, BF16)
        rkb = walloc('rkb', 512, BF16)
        assert wtop[0] <= NW, wtop[0]
        W = wk
        ycT = A.bf(4 * T, at=work0 + 3968).rearrange("p (j t) -> p j t", t=T)
        assert conv_end <= work0 + 3968

        TS('dve', hpar, par[:, PC_DB:PC_DB + 8], 0.5, None, ALU.mult, ALU.bypass, ['par'], ['hpar'])
        TS('dve', omm_[:, 0:14], par[:, PC_MU:PC_MU + 14], -1.0, 1.0, ALU.mult, ALU.add, ['par'], ['omm'])

        wctr = [0]

        def load_w(ci):
            b = wctr[0] % 4
            sb = wctr[0] % 2
            wctr[0] += 1
            P.dma(wst[sb], win_d[ci, :, :], writes=['wst%d' % sb])
            P.op('pool', lambda e: e.tensor_copy(out=wbf[b][:, :, :].rearrange("p k c -> p (k c)"), in_=wst[sb]),
                 reads=['wst%d' % sb], writes=['wbf%d' % b])
            return wbf[b], 'wbf%d' % b

        def proj(wap, wtag, tb, b):
            for k in range(8):
                MM(banks[b][:, :], wap[:, k, :], uT[:, k, tb * 512:(tb + 1) * 512], [wtag, 'uT'], [BT(b)],
                   start=(k == 0), stop=(k == 7), inc=(k == 7))

        def shift_mix(nm, b, tb, mucol, out_ap, out_tag):
            z = zc[nm]
            zt = 'zc_' + nm
            if tb > 0:
                ACT(z[:, 0:1], z[:, 512:513], AF.Copy, [zt], [zt])
            else:
                P.op('pool', lambda e: e.memset(z[:, 0:1], 0.0), [], [zt])
            ACT(z[:, 1:513], banks[b][:, :], AF.Copy, [BT(b)], [zt])
            ACT(out_ap, banks[b][:, :], AF.Copy, [BT(b), 'omm'], [out_tag], scale=omm_[:, mucol - PC_MU:mucol - PC_MU + 1])
            STT(out_ap, z[:, 0:512], par[:, mucol:mucol + 1], out_ap, ALU.mult, ALU.add, [zt, 'par', out_tag], [out_tag])

        w24, t24 = load_w(0)
        w25, t25 = load_w(1)
        for tb in range(4):
            sl = slice(tb * 512, (tb + 1) * 512)
            proj(w24, t24, tb, 0)
            shift_mix('r', 0, tb, PC_MU + 12, W['t1'], 't1')
            ACT(tha[0:64, sl], W['t1'][0:64, :], AF.Tanh, ['t1'], ['tha'])
            ACT(tha[64:128, sl], W['t1'][64:128, :], AF.Copy, ['t1'], ['tha'])
            proj(w25, t25, tb, 1)
            shift_mix('k', 1, tb, PC_MU + 13, W['t1'], 't1')
            ACT(W['t1'], W['t1'], AF.Tanh, ['t1'], ['t1'], scale=0.5)
            TS('pool', sg[:, sl], W['t1'], 0.5, 0.5, ALU.mult, ALU.add, ['t1'], ['sg'])
        dump('tha', tha, [128, T], BF16, reads=['tha'])
        dump('sg', sg, [128, T], BF16, reads=['sg'])

        P.op('pool', lambda e: e.memset(smask, 1.0), [], ['smask'])
        P.op('pool', lambda e: e.memset(smask.rearrange("p (c t) -> p c t", t=64)[:, :, 0:1], 0.0), [], ['smask'])

        WBs = {}
        _ar3, _bk3, _vb3 = pers0 + 6144, pers0 + 8192 + 6144, pers0 + 16384 + 6144
        for j_, nm in enumerate(('r', 'k', 'v', 'sig')):
            WBs[nm] = A.f32(512, at=_ar3 + j_ * 512)
        for j_, nm in enumerate(('a', 'cs', 'winc', 'winv')):
            WBs[nm] = A.f32(512, at=_bk3 + j_ * 512)
        WBs['t1'] = A.f32(512, at=_vb3)
        WBs['sqb'] = A.bf(512, at=_vb3 + 512)
        WBs['rkb'] = A.bf(512, at=_vb3 + 768)
        WAs = dict(W)
        WAs['sqb'] = sqb
        WAs['rkb'] = rkb

        def wset(hp, tb):
            if hp < 3 and (hp * 4 + tb) % 2 == 1:
                return WBs, 'B'
            return WAs, 'A'

        def prep(hp, tb, part='AB'):
            Wx, sx = wset(hp, tb)
            T_ = lambda n: n + sx
            u_ = '%d%d' % (hp, tb)
            sl = slice(tb * 512, (tb + 1) * 512)
            hc = slice(hp * 128, (hp + 1) * 128)
            r_, k_, v_, sig, a_, cs, winc, winv, t1 = (Wx[n] for n in ('r', 'k', 'v', 'sig', 'a', 'cs', 'winc', 'winv', 't1'))
            sqb_, rkb_ = Wx['sqb'], Wx['rkb']
            if 'A' in part:
                prepA_(hp, tb, Wx, T_, u_, sl, hc)
            if 'B' in part:
                prepB_(hp, tb, Wx, T_, u_, sl, hc)

        def prepA_(hp, tb, Wx, T_, u_, sl, hc):
            r_, k_, v_, sig, a_, cs, winc, winv, t1 = (Wx[n] for n in ('r', 'k', 'v', 'sig', 'a', 'cs', 'winc', 'winv', 't1'))
            sqb_, rkb_ = Wx['sqb'], Wx['rkb']
            MM(banks[4][:, :], lupb[0:64, hc], tha[0:64, sl], ['lupb', 'tha'], [BT(4)])
            MM(banks[5][:, :], lupb[64:128, hc], tha[64:128, sl], ['lupb', 'tha'], [BT(5)])
            ACT(sig, banks[4][:, :], AF.Tanh, [BT(4), 'hpar'], [T_('sig')], scale=0.5, bias=hpar[:, hp:hp + 1])
            ACT(a_, banks[5][:, :], AF.Tanh, [BT(5), 'hpar'], [T_('a')], scale=0.5, bias=hpar[:, 4 + hp:5 + hp])
            TS('dve', sig, sig, 0.5, 0.5, ALU.mult, ALU.add, [T_('sig')], [T_('sig')])
            TS('dve', a_, a_, 0.5, 0.5, ALU.mult, ALU.add, [T_('a')], [T_('a')])
            P.op('dve', lambda e: e.tensor_tensor_scan(out=cs, data0=smask, data1=sig, initial=0.0,
                                                        op0=ALU.mult, op1=ALU.add), ['smask', T_('sig')], [T_('cs')])
            ACT(winc, cs, AF.Exp, [T_('cs')], [T_('winc')], scale=-C0)
            ACT(winv, cs, AF.Exp, [T_('cs')], [T_('winv')], scale=C0)
            TT('dve', sig, cs, sig, ALU.subtract, [T_('cs'), T_('sig')], [T_('sig')])
            ACT(sig, sig, AF.Exp, [T_('sig')], [T_('sig')], scale=-C0)
            ACT(WC[:, hp, tb * 8:(tb + 1) * 8], winc.rearrange("p (c t) -> p c t", t=64)[:, :, 63], AF.Copy,
                [T_('winc')], ['WC' + u_])
            TT('pool', AR[:, hp, 1, sl], r_, winc, ALU.mult, [T_('r'), T_('winc')], ['ARr' + u_])
            ACT(winc, k_, AF.Copy, [T_('k'), 'par'], [T_('winc')], scale=par[:, PC_KK + hp:PC_KK + hp + 1])
            TT('dve', sqb_, winc, winc, ALU.mult, [T_('winc')], [T_('sqb')])
            MM(banks[6][:, :], bonesB, sqb_, ['cst', T_('sqb')], [BT(6)])

        def prepB_(hp, tb, Wx, T_, u_, sl, hc):
            r_, k_, v_, sig, a_, cs, winc, winv, t1 = (Wx[n] for n in ('r', 'k', 'v', 'sig', 'a', 'cs', 'winc', 'winv', 't1'))
            sqb_, rkb_ = Wx['sqb'], Wx['rkb']
            TS('dve', t1, banks[6][:, :], 1e-24, None, ALU.max, ALU.bypass, [BT(6)], [T_('t1')])
            ACT(t1, t1, AF.Ln, [T_('t1')], [T_('t1')])
            ACT(t1, t1, AF.Exp, [T_('t1')], [T_('t1')], scale=-0.5)
            TT('dve', winc, winc, t1, ALU.mult, [T_('winc'), T_('t1')], [T_('winc')])
            STT(AR[:, hp, 0, sl], winc, -1.0, sig, ALU.mult, ALU.mult, [T_('winc'), T_('sig')], ['ARa' + u_])
            TS('dve', t1, a_, par[:, PC_KA + hp:PC_KA + hp + 1], omka[:, hp:hp + 1], ALU.mult, ALU.add,
               [T_('a'), 'par', 'omka'], [T_('t1')])
            TT('pool', k_, k_, t1, ALU.mult, [T_('k'), T_('t1')], [T_('k')])
            TT('dve', t1, a_, winc, ALU.mult, [T_('a'), T_('winc')], [T_('t1')])
            TT('pool', BK[:, hp, 0, sl], t1, winv, ALU.mult, [T_('t1'), T_('winv')], ['BKb' + u_])
            TT('pool', BK[:, hp, 1, sl], k_, winv, ALU.mult, [T_('k'), T_('winv')], ['BKk' + u_])
            STT(rkb_, r_, par[:, PC_RK + hp:PC_RK + hp + 1], k_, ALU.mult, ALU.mult, [T_('r'), T_('k'), 'par'], [T_('rkb')])
            MM(banks[7][:, :], bonesB, rkb_, ['cst', T_('rkb')], [BT(7)])
            TT('dve', VB[:, hp, 1, sl], banks[7][:, :], v_, ALU.mult, [BT(7), T_('v')], ['VBb' + u_])
            ACT(VB[:, hp, 0, sl], v_, AF.Copy, [T_('v')], ['VBv' + u_])

        wsets = {}

        def part1(hp, tb):
            if tb == 0:
                wsets[hp] = [load_w(2 + hp * 3 + ti) for ti in range(3)]
            ws = wsets[hp]
            Wx, sx = wset(hp, tb)
            for ti, nm in enumerate(('r', 'k', 'v')):
                proj(ws[ti][0], ws[ti][1], tb, ti)
                shift_mix(nm, ti, tb, PC_MU + ti * 4 + hp, Wx[nm], nm + sx)

        its = [(hp, tb) for hp in range(3) for tb in range(4)]
        part1(*its[0])
        prep(its[0][0], its[0][1], 'A')
        for n, (hp, tb) in enumerate(its):
            la = P.capture(lambda: prep(hp, tb, 'B'))
            lb = []
            if n + 1 < len(its):
                hp2, tb2 = its[n + 1]
                lb = P.capture(lambda: (part1(hp2, tb2), prep(hp2, tb2, 'A')))
            P.replay(la, lb)
        P.barrier()
        for tb in range(4):
            part1(3, tb)
            prep(3, tb)
        for hp in range(4):
            if hp in (0, 3):
                dump('AR%d' % hp, AR[:, hp, :, :].rearrange("p x t -> p (x t)"), [128, 2 * T], BF16, reads=['AR'])
                dump('BK%d' % hp, BK[:, hp, :, :].rearrange("p x t -> p (x t)"), [128, 2 * T], BF16, reads=['BK'])
                dump('VB%d' % hp, VB[:, hp, :, :].rearrange("p x t -> p (x t)"), [128, 2 * T], BF16, reads=['VB'])
        for nm_ in ('sig', 'a', 'cs', 'winc', 'winv', 't1'):
            dump('w_' + nm_, W[nm_], [128, 512], F32, reads=[nm_])
        dump('hpar', hpar, [128, 8], F32, reads=['hpar'])
        dump('par', par[:, 0:NPAR], [128, NPAR], F32, reads=['par'])
        dump('WC', WC[:, :, :].rearrange("p h c -> p (h c)"), [128, 128], F32, reads=['WC'])
        if stop == 'B':
            P.finish()
            return nc, dbg_out
        P.barrier()

        cs0, cs1 = work0, work0 + 2050
        csets = []
        for base_ in (cs0, cs1):
            csets.append(dict(b=A.bf(512, at=base_), c=A.bf(512, at=base_ + 256), t1=A.f32(512, at=base_ + 512),
                              chb=A.f32(514, at=base_ + 1024)))
        assert cs1 + 1538 <= work0 + 3968
        cits = [(j, tb) for j in range(4) for tb in range(4)]
        cws = {}

        def conv_it(n):
            j, tb = cits[n]
            if tb == 0:
                cws[j] = [load_w(14 + j * 3 + ti) for ti in range(3)]
            ws = cws[j]
            S_ = csets[n % 2]
            Sp = csets[(n + 1) % 2]
            sx = 'c%d' % (n % 2)
            px = 'c%d' % ((n + 1) % 2)
            sl = slice(tb * 512, (tb + 1) * 512)
            bo = 4 * (n % 2)
            for ti in range(3):
                proj(ws[ti][0], ws[ti][1], tb, bo + ti)
            ACT(S_['b'], banks[bo][:, :], AF.Copy, [BT(bo)], ['b' + sx])
            ACT(S_['c'], banks[bo + 1][:, :], AF.Copy, [BT(bo + 1)], ['c' + sx])
            chb_ = S_['chb']
            if tb == 0:
                P.op('pool', lambda e: e.memset(chb_[:, 0:2], 0.0), [], ['cy' + sx])
            TT('dve', chb_[:, 2:514], banks[bo + 2][:, :], S_['c'], ALU.mult, [BT(bo + 2), 'c' + sx], ['chb' + sx])
            def carry():
                if tb < 3:
                    ACT(Sp['chb'][:, 0:2], chb_[:, 512:514], AF.Copy, ['chb' + sx], ['cy' + px])
            if n % 2 == 0:
                carry()
            cw = lambda tap: par[:, PC_CW + tap * 4 + j:PC_CW + tap * 4 + j + 1]
            t1_ = S_['t1']
            ACT(t1_, chb_[:, 2:514], AF.Copy, ['chb' + sx, 'par'], ['t1' + sx], scale=cw(2))
            STT(t1_, chb_[:, 1:513], cw(1), t1_, ALU.mult, ALU.add, ['chb' + sx, 'cy' + sx, 't1' + sx, 'par'], ['t1' + sx])
            STT(t1_, chb_[:, 0:512], cw(0), t1_, ALU.mult, ALU.add, ['chb' + sx, 'cy' + sx, 't1' + sx, 'par'], ['t1' + sx])
            TT('pool', ycT[:, j, sl], t1_, S_['b'], ALU.mult, ['t1' + sx, 'b' + sx], ['ycT%d%d' % (j, tb)])
            if n % 2 == 1:
                carry()

        for n in range(0, len(cits), 2):
            la = P.capture(lambda: conv_it(n))
            lb = P.capture(lambda: conv_it(n + 1))
            P.replay(la, lb)
        P.barrier()
        dump('ycT', ycT[:, 0, :], [128, T], BF16, reads=['ycT'])
        if stop == 'C':
            P.finish()
            return nc, dbg_out
        P.barrier()

        maskS = cstb[:, 256:384]
        maskL = cstb[:, 384:448]
        ident2 = cstb[:, 448:512]
        r1, r2, r3 = [base0], [pers1], [work0]

        def cbf(reg, n):
            ap = A.bf(n, at=reg[0])
            reg[0] += (n + 1) // 2
            return ap

        def cf(reg, n):
            ap = A.f32(n, at=reg[0])
            reg[0] += n
            return ap

        h128 = lambda ap: ap.rearrange("p (h x) -> p h x", x=128)
        h64 = lambda ap: ap.rearrange("p (h x) -> p h x", x=64)
        hcx = lambda ap: ap.rearrange("p (h c x) -> p h c x", h=4, c=2)
        yrT = cbf(r1, 4 * T).rearrange("p (j t) -> p j t", t=T)
        AL = [h128(cbf(r1, 1024)) for _ in range(2)]
        RB = [h128(cbf(r1, 1024)) for _ in range(2)]
        Ktm = [h64(cbf(r1, 512)) for _ in range(2)]
        Vtm = [h64(cbf(r1, 512)) for _ in range(2)]
        S2m = [h128(cbf(r1, 1024)) for _ in range(2)]
        AUs = [h128(cbf(r1, 1024)) for _ in range(2)]
        assert r1[0] <= base0 + 9216, r1[0]
        NY = [h128(cbf(r2, 1024)) for _ in range(2)]
        NTs = [h64(cbf(r2, 512)) for _ in range(2)]
        Yf = h64(cbf(r2, 512))
        RhT = [hcx(cbf(r2, 512)) for _ in range(2)]
        GT = [hcx(cbf(r2, 512)) for _ in range(2)]
        O0s = [hcx(cf(r2, 512)) for _ in range(2)]
        Hb = [h64(cbf(r2, 256)) for _ in range(2)]
        assert r2[0] <= pers1 + 4096 + 256, r2[0]
        Qs = [hcx(cf(r3, 512)) for _ in range(2)]
        ot = cf(r3, 512)
        osq = cf(r3, 512)
        dd = cf(r3, 512)
        rs = cf(r3, 512)
        htmp = cf(r3, 256)
        ob = cbf(r3, 512)
        osqb = cbf(r3, 512)
        assert r3[0] <= work0 + 3968, r3[0]
        WC4 = A.f32(128, at=pers1 - 128).rearrange("p (h c o) -> p h c o", h=4, o=1)
        lnw4 = par[:, PC_LW:PC_LW + 4].rearrange("p (h o) -> p h o", o=1)
        lnb4 = par[:, PC_LB:PC_LB + 4].rearrange("p (h o) -> p h o", o=1)

        def pbf(b):
            return banks[b][:, :].bitcast(BF16)

        hv = lambda ap, hd: ap.rearrange("p (a b) x -> p a b x", b=2)[:, :, hd, :]
        crs = (slice(0, 64), slice(64, 128))
        hrs = crs
        NTB = (4, 7)
        QB = (4, 7)
        OB = (5, 6)
        G4 = [(cp, hd) for cp in range(2) for hd in range(2)]
        gt_ = lambda name, cp, hd: '%s_%d%d' % (name, cp, hd)
        both = lambda name, hd: [gt_(name, 0, hd), gt_(name, 1, hd)]
        allg = lambda name: [gt_(name, cp, hd) for cp, hd in G4]
        bP = lambda cp, hd: 'bP%d' % (cp * 2 + hd)
        bQ = lambda cp, hd: 'bQ%d_%d' % (cp, hd)
        bO = lambda cp, hd: 'bO%d_%d' % (cp, hd)
        m64 = lambda m: m.rearrange("p (o x) -> p o x", o=1).to_broadcast([128, 4, 64])
        mS1, mS2, mL4, id4 = m64(maskS[:, 0:64]), m64(maskS[:, 64:128]), m64(maskL), m64(ident2)
        mSf = maskS.rearrange("p (o x) -> p o x", o=1).to_broadcast([128, 4, 128])
        P.op('pool', lambda e: e.memset(Hb[0][:, :, :], 0.0), [], ['Hb0_0', 'Hb0_1'])

        S1B, S2B, STB = (0, 3), (1, 2), (0, 3)
        PBK = lambda cp, hd: cp * 2 + hd
        QBK = lambda cp, hd: 4 + cp * 2 + hd
        bk = lambda n: ['bk%d' % n]

        def st_T(i):
            par = i % 2
            tk = slice(i * 128, (i + 1) * 128)
            wb5, wb6 = bk(5), bk(6)
            for hp in range(4):
                P.op('pe', lambda e, hp=hp: e.transpose(out=pbf(5)[:, hp * 128:(hp + 1) * 128], in_=AR[:, hp, 0, tk],
                                                        identity=identB), ['AR', 'cst'], wb5, inc=False)
                P.op('pe', lambda e, hp=hp: e.transpose(out=pbf(5)[:, 512 + hp * 128:512 + (hp + 1) * 128],
                                                        in_=BK[:, hp, 0, tk], identity=identB), ['BK', 'cst'], wb5, inc=(hp == 3))
            for hp in range(4):
                P.op('pe', lambda e, hp=hp: e.transpose(out=pbf(6)[:, hp * 128:(hp + 1) * 128], in_=BK[:, hp, 1, tk],
                                                        identity=identB), ['BK', 'cst'], wb6, inc=False)
                P.op('pe', lambda e, hp=hp: e.transpose(out=pbf(6)[:, 512 + hp * 128:512 + (hp + 1) * 128],
                                                        in_=VB[:, hp, 0, tk], identity=identB), ['VB', 'cst'], wb6, inc=(hp == 3))
            ACT(AL[par][:, :, 0:64], h64(pbf(5)[:, 0:512]), AF.Copy, wb5, allg('ALa%d' % par))
            ACT(RB[par][:, :, 64:128], h64(pbf(5)[:, 512:1024]), AF.Copy, wb5, allg('RBb%d' % par))
            ACT(Ktm[par][:, :, :], h64(pbf(6)[:, 0:512]), AF.Copy, wb6, allg('Ktm%d' % par))
            ACT(Vtm[par][:, :, :], h64(pbf(6)[:, 512:1024]), AF.Copy, wb6, allg('Vtm%d' % par))

        def st_S(i, hd):
            par = i % 2
            hr = hrs[hd]
            b1, b2, b3 = S1B[hd], S2B[hd], NTB[hd]
            t1, t2, t3 = bk(b1), bk(b2), bk(b3)
            for hp in range(4):
                for cp in range(2):
                    cr = crs[cp]
                    t64 = slice(i * 128 + cp * 64, i * 128 + cp * 64 + 64)
                    last = (hp == 3 and cp == 1)
                    cs_ = slice(hp * 128, hp * 128 + 128)
                    MM(banks[b1][cr, cs_], BK[hr, hp, 0, t64], AR[hr, hp, :, t64], ['BK', 'AR'], t1, inc=last)
                    MM(banks[b2][cr, cs_], BK[hr, hp, 1, t64], AR[hr, hp, :, t64], ['BK', 'AR'], t2, inc=last)
                    MM(banks[b3][cr, hp * 64:hp * 64 + 64], AR[hr, hp, 0, t64], BK[hr, hp, 0, t64], ['BK', 'AR'], t3, inc=last)
            TT('dve', hv(NY[0], hd)[:, :, 0:64], h128(banks[b1][:, :])[:, :, 0:64], mS1, ALU.mult, t1 + ['cst'], both('NY0n', hd))
            TT('dve', hv(NTs[0], hd), h64(banks[b3][:, 0:256]), mL4, ALU.mult, t3 + ['cst'], both('NT0', hd))
            TT('pool', hv(NY[1], hd)[:, :, 64:128], hv(NY[0], hd)[:, :, 0:64], id4, ALU.add, both('NY0n', hd) + ['cst'], both('NY1y', hd))
            TT('dve', hv(RB[par], hd)[:, :, 0:64], h128(banks[b1][:, :])[:, :, 64:128], mS2, ALU.mult, t1 + ['cst'], both('RBr%d' % par, hd))
            TT('dve', hv(S2m[par], hd), h128(banks[b2][:, :]), mSf, ALU.mult, t2 + ['cst'], both('S2m%d' % par, hd))

        def st_N(i, lv, cp, hd):
            cr = crs[cp]
            pb, qb = PBK(cp, hd), QBK(cp, hd)
            tP, tQ = bk(pb), bk(qb)
            qc = lambda hp: slice(hp * 64, hp * 64 + 64)
            if lv == 0:
                rn, rt = gt_('NY0n', cp, hd), gt_('NT0', cp, hd)
                for hp in range(4):
                    h = 2 * hp + hd
                    MM(banks[pb][cr, hp * 128:hp * 128 + 64], NTs[0][cr, h, :], NY[0][cr, h, 0:64], [rn, rt], tP, inc=(hp == 3))
                    MM(banks[qb][cr, qc(hp)], NY[0][cr, h, 0:64], NTs[0][cr, h, :], [rn, rt], tQ, inc=(hp == 3))
                ACT(hv(NY[1], hd)[cr, :, 0:64], h128(banks[pb][cr, :])[:, :, 0:64], AF.Copy, tP, [gt_('NY1n', cp, hd)])
                ACT(hv(NTs[1], hd)[cr, :, :], h64(banks[qb][cr, 0:256]), AF.Copy, tQ, [gt_('NT1', cp, hd)])
                return
            a_, b_ = lv % 2, (lv + 1) % 2
            NYa, NYb, NTa, NTb = NY[a_], NY[b_], NTs[a_], NTs[b_]
            rn, ry, rt = gt_('NY%dn' % a_, cp, hd), gt_('NY%dy' % a_, cp, hd), gt_('NT%d' % a_, cp, hd)
            wn, wy, wt = gt_('NY%dn' % b_, cp, hd), gt_('NY%dy' % b_, cp, hd), gt_('NT%d' % b_, cp, hd)
            for hp in range(4):
                h = 2 * hp + hd
                if lv < 5:
                    MM(banks[pb][cr, hp * 128:hp * 128 + 128], NTa[cr, h, :], NYa[cr, h, :], [rn, ry, rt], tP, inc=(hp == 3))
                    MM(banks[qb][cr, qc(hp)], NYa[cr, h, 0:64], NTa[cr, h, :], [rn, rt], tQ, inc=(hp == 3))
                else:
                    MM(banks[pb][cr, hp * 128 + 64:hp * 128 + 128], NTa[cr, h, :], NYa[cr, h, 64:128], [ry, rt], tP, inc=(hp == 3))
            bv = h128(banks[pb][cr, :])
            if lv < 5:
                ACT(hv(NYb, hd)[cr, :, 0:64], bv[:, :, 0:64], AF.Copy, tP, [wn])
                TT('dve', hv(NYb, hd)[cr, :, 64:128], bv[:, :, 64:128], hv(NYa, hd)[cr, :, 64:128], ALU.add, tP + [ry], [wy])
                ACT(hv(NTb, hd)[cr, :, :], h64(banks[qb][cr, 0:256]), AF.Copy, tQ, [wt])
            else:
                TT('dve', hv(Yf, hd)[cr, :, :], bv[:, :, 64:128], hv(NYa, hd)[cr, :, 64:128], ALU.add, tP + [ry], [gt_('Yf', cp, hd)])

        def st_L(i, cp, hd):
            par = i % 2
            cr = crs[cp]
            qb = QBK(cp, hd)
            tQ = bk(qb)
            for hp in range(4):
                h = 2 * hp + hd
                MM(banks[qb][cr, hp * 64:hp * 64 + 64], S2m[par][cr, h, 0:64], Vtm[par][cr, h, :],
                   [gt_('S2m%d' % par, cp, hd), gt_('Vtm%d' % par, cp, hd)], tQ, inc=(hp == 3))
            ACT(hv(AL[par], hd)[cr, :, 64:128], h64(banks[qb][cr, 0:256]), AF.Copy, tQ, [gt_('ALl%d' % par, cp, hd)])

        def st_AU(i, cp, hd):
            par = i % 2
            cr = crs[cp]
            pb = PBK(cp, hd)
            tP = bk(pb)
            for hp in range(4):
                h = 2 * hp + hd
                MM(banks[pb][cr, hp * 128:hp * 128 + 128], Yf[cr, h, :], AL[par][cr, h, :],
                   [gt_('Yf', cp, hd), gt_('ALa%d' % par, cp, hd), gt_('ALl%d' % par, cp, hd)], tP, inc=(hp == 3))
            ACT(hv(AUs[par], hd)[cr, :, :], h128(banks[pb][cr, :]), AF.Copy, tP, [gt_('AUs%d' % par, cp, hd)])

        def st_E(i, cp, hd):
            par = i % 2
            cr, hr = crs[cp], hrs[hd]
            pb = PBK(cp, hd)
            tP = bk(pb)
            t64 = slice(i * 128 + cp * 64, i * 128 + cp * 64 + 64)
            for hp in range(4):
                h = 2 * hp + hd
                MM(banks[pb][hr, hp * 128:hp * 128 + 128], AUs[par][cr, h, 0:64], RB[par][cr, h, :],
                   [gt_('AUs%d' % par, cp, hd), gt_('RBr%d' % par, cp, hd), gt_('RBb%d' % par, cp, hd)], tP, inc=(hp == 3))
            ev = h128(banks[pb][hr, :])
            TT('dve', RhT[par][hr, :, cp, :], ev[:, :, 0:64], AR[hr, :, 1, t64], ALU.add, tP + ['AR'], [gt_('RhT%d' % par, cp, hd)])
            idb = ident2[hr, :].rearrange("p (o x) -> p o x", o=1).to_broadcast([64, 4, 64])
            TT('dve', GT[par][hr, :, cp, :], ev[:, :, 64:128], idb, ALU.add, tP + ['cst'], [gt_('GT%d' % par, cp, hd)])

        def st_OQ(i, cp, hd):
            par = i % 2
            cr, hr = crs[cp], hrs[hd]
            c = 2 * i + cp
            ob_, qb = PBK(cp, hd), QBK(cp, hd)
            tO, tQ = bk(ob_), bk(qb)
            rA = [gt_('AUs%d' % par, cp, hd), gt_('RBr%d' % par, cp, hd), gt_('RBb%d' % par, cp, hd)]
            rV = [gt_('Vtm%d' % par, cp, hd), gt_('S2m%d' % par, cp, hd), gt_('Ktm%d' % par, cp, hd)]
            for hp in range(4):
                h = 2 * hp + hd
                col = slice(hp * 64, hp * 64 + 64)
                MM(banks[ob_][hr, col], AUs[par][cr, h, 64:128], RB[par][cr, h, 0:64], rA, tO, start=True, stop=False, inc=False)
                MM(banks[ob_][hr, col], Vtm[par][cr, h, :], S2m[par][cr, h, 64:128], rV, tO, start=False, stop=True, inc=(hp == 3))
                MM(banks[qb][hr, col], RB[par][cr, h, 64:128], AUs[par][cr, h, 64:128], rA, tQ, start=True, stop=False, inc=False)
                MM(banks[qb][hr, col], Ktm[par][cr, h, :], Vtm[par][cr, h, :], rV, tQ, start=False, stop=True, inc=(hp == 3))
            ACT(O0s[par][hr, :, cp, :], h64(banks[ob_][hr, 0:256]), AF.Copy, tO, [gt_('O0s%d' % par, cp, hd)])
            wc1 = WC4[hr, :, c, :].to_broadcast([64, 4, 64])
            TT('dve', Qs[par][hr, :, cp, :], h64(banks[qb][hr, 0:256]), wc1, ALU.mult, tQ + ['WC'], [gt_('Qs%d' % par, cp, hd)])

        o4 = hcx(ot)
        h3 = h64(htmp)

        def st_ST(i, cp, hd):
            par = i % 2
            hr = hrs[hd]
            c = 2 * i + cp
            Hc, Hn = Hb[c % 2], Hb[(c + 1) % 2]
            tc_, tn = 'Hb%d_%d' % (c % 2, hd), 'Hb%d_%d' % ((c + 1) % 2, hd)
            sb = STB[hd]
            tB = bk(sb)
            for hp in range(4):
                MM(banks[sb][hr, hp * 64:hp * 64 + 64], Hc[hr, hp, :], RhT[par][hr, hp, cp, :],
                   [tc_, gt_('RhT%d' % par, cp, hd)], tB, inc=False)
                MM(banks[sb][hr, 256 + hp * 64:256 + hp * 64 + 64], GT[par][hr, hp, cp, :], Hc[hr, hp, :],
                   [tc_, gt_('GT%d' % par, cp, hd)], tB, inc=(hp == 3))
            b7 = banks[sb][hr, :].rearrange("p (z h x) -> p z h x", z=2, h=4)
            TT('dve', o4[hr, :, cp, :], b7[:, 0, :, :], O0s[par][hr, :, cp, :], ALU.add, tB + [gt_('O0s%d' % par, cp, hd)], ['ot_%d' % hd])
            wc1 = WC4[hr, :, c, :].to_broadcast([64, 4, 64])
            TT('dve', h3[hr, :, :], b7[:, 1, :, :], wc1, ALU.mult, tB + ['WC'], ['htmp_%d' % hd])
            TT('pool', Hn[hr, :, :], h3[hr, :, :], Qs[par][hr, :, cp, :], ALU.add, ['htmp_%d' % hd, gt_('Qs%d' % par, cp, hd)], [tn])

        def st_GN(i):
            tk = slice(i * 128, (i + 1) * 128)
            rot = ['ot_0', 'ot_1']
            ACT(ob, ot, AF.Copy, rot, ['ob'])
            TT('pool', osqb, ot, ot, ALU.mult, rot, ['osqb'])
            MM(banks[1][:, :], bonesB, ob, ['cst', 'ob'], bk(1))
            MM(banks[2][:, :], bonesB, osqb, ['cst', 'osqb'], bk(2))
            TS('dve', rs, banks[1][:, :], 1.0 / 64, None, ALU.mult, ALU.bypass, bk(1), ['rs'])
            TT('dve', dd, ot, rs, ALU.subtract, rot + ['rs'], ['dd'])
            TT('pool', rs, rs, rs, ALU.mult, ['rs'], ['rs'])
            TS('dve', osq, banks[2][:, :], 1.0 / 64, 64e-5, ALU.mult, ALU.add, bk(2), ['osq'])
            TT('pool', rs, osq, rs, ALU.subtract, ['osq', 'rs'], ['rs'])
            ACT(rs, rs, AF.Ln, ['rs'], ['rs'])
            ACT(rs, rs, AF.Exp, ['rs'], ['rs'], scale=-0.5)
            for hp in range(4):
                MM(banks[1][:, hp * 128:(hp + 1) * 128], gupb[:, hp * 128:(hp + 1) * 128], sg[:, tk], ['gupb', 'sg'], bk(1),
                   inc=(hp == 3))
            TT('pool', dd, dd, rs, ALU.mult, ['dd', 'rs'], ['dd'])
            d3 = dd.rearrange("p (h x) -> p h x", x=128)
            TT('pool', d3, d3, lnw4.to_broadcast([128, 4, 128]), ALU.mult, ['dd', 'par'], ['dd'])
            TT('pool', d3, d3, lnb4.to_broadcast([128, 4, 128]), ALU.add, ['dd', 'par'], ['dd'])
            TT('pool', d3, d3, VB[:, :, 1, tk], ALU.add, ['dd', 'VB'], ['dd'])
            TT('dve', yrT[:, :, tk], d3, h128(banks[1][:, :]), ALU.mult, ['dd'] + bk(1), ['yrT'])

        def P_stages(i):
            st = [lambda: st_T(i), lambda: (st_S(i, 0), st_S(i, 1))]
            for lv in range(6):
                st.append(lambda lv=lv: [st_N(i, lv, cp, hd) for cp, hd in G4])
            st.append(lambda: [st_L(i, cp, hd) for cp, hd in G4])
            st.append(lambda: [st_AU(i, cp, hd) for cp, hd in G4])
            st.append(lambda: [st_E(i, cp, hd) for cp, hd in G4])
            st.append(lambda: [st_OQ(i, cp, hd) for cp, hd in G4])
            return st

        def Q_stages(i):
            return [lambda: (st_ST(i, 0, 0), st_ST(i, 0, 1)), lambda: (st_ST(i, 1, 0), st_ST(i, 1, 1)), lambda: st_GN(i)]

        import os as _os
        if _os.environ.get('NOPIPE'):
            _ds = int(_os.environ.get('DSTOP', '999'))
            for i in range(NTILE):
                for k_, f in enumerate(P_stages(i) + Q_stages(i)):
                    f()
                    if i == 0 and k_ == _ds:
                        P.finish()
                        return nc, dbg_out
        else:
            for f in P_stages(0):
                f()
            for i in range(NTILE):
                ps = P_stages(i + 1) if i + 1 < NTILE else []
                qs = Q_stages(i)
                _k = int(_os.environ.get('PIPEK', '1'))
                slots = None
                for k, f in enumerate(ps):
                    f()
                    if slots is None:
                        if k == _k:
                            for q_ in qs:
                                q_()
                    elif k in slots:
                        qs[slots[k]]()
                if not ps:
                    for f in qs:
                        f()
        dump('yrT', yrT[:, 0, :], [128, T], BF16, reads=['yrT'])
        if stop == 'D':
            P.finish()
            return nc, dbg_out
        P.barrier()
        h1 = A.f32(16 * D, at=pers0).rearrange("p (i d) -> p i d", d=D)
        o1 = pers0 + 16 * D
        wob = A.bf(8 * D, at=o1 + 8192).rearrange("p (k c) -> p k c", c=D)
        wos = [A.f32(D, at=o1 + s * D) for s in range(4)]
        assert o1 + 8192 + 4096 <= work0 - 512
        u2T = A.bf(8 * T, at=base0).rearrange("p (k t) -> p k t", t=T)
        P.dma(gbc, g2_d.partition_broadcast(128), writes=['gbc'])

        stg = [A.f32(2048, at=o1 + s * 2048) for s in range(3)]
        wb = [[A.bf(2048, at=o1 + 6144 + (s2 * 3 + s) * 1024) for s in range(3)] for s2 in range(2)]
        assert o1 + 6144 + 6144 <= work0 - 512
        srcs = (wg_d, wu_d, wd_d)

        def moe_dma(ex, extra=()):
            for s in range(3):
                P.dma(stg[s], srcs[s][ex, :, :], writes=['stg%d' % s] + list(extra))

        def moe_cvt(ex):
            s2 = ex % 2
            for s in range(3):
                if s != 1:
                    P.op('pool', lambda e, s=s, s2=s2: e.tensor_copy(out=wb[s2][s], in_=stg[s]), ['stg%d' % s], ['wb%d_%d' % (s2, s)])
                else:
                    ACT(wb[s2][s], stg[s], AF.Copy, ['stg%d' % s], ['wb%d_%d' % (s2, s)])

        def xload(i):
            P.dma(h1[:, i, :], x_d[i * 128:(i + 1) * 128, :], writes=['h1_%d' % i])
        for k in range(8):
            P.dma(wos[k % 4], wout_d[:, k * D:(k + 1) * D], writes=['wos%d' % (k % 4)])
            if k % 2 == 0:
                P.op('dve', lambda e, k=k: e.tensor_copy(out=wob[:, k, :], in_=wos[k % 4]), ['wos%d' % (k % 4)], ['wob%d' % k])
            else:
                ACT(wob[:, k, :], wos[k % 4], AF.Copy, ['wos%d' % (k % 4)], ['wob%d' % k])
        for i in range(NTILE):
            xload(i)
        moe_dma(0, extra=['wos%d' % q_ for q_ in range(4)])

        def eproj(i):
            tk = slice(i * 128, (i + 1) * 128)
            for half in range(2):
                b = (2 * i + half) % 4
                hs = slice(half * 512, (half + 1) * 512)
                for k in range(8):
                    lhs = ycT[:, k, tk] if k < 4 else yrT[:, k - 4, tk]
                    MM(banks[b][:, :], lhs, wob[:, k, hs], ['ycT', 'yrT%d' % i, 'wob%d' % k], [BT(b)],
                       start=(k == 0), stop=(k == 7), inc=(k == 7))
                TT('dve', h1[:, i, hs], banks[b][:, :], h1[:, i, hs], ALU.add, [BT(b), 'h1_%d' % i], ['h1_%d' % i])
            P.op('act', lambda e: e.activation(out=junk, in_=h1[:, i, :], func=AF.Square, accum_out=ss[:, i:i + 1]),
                 reads=['h1_%d' % i], writes=['junk', 'ssg%d' % (i // 4)])

        def fnorm(g):
            gs = slice(4 * g, 4 * g + 4)
            TS('dve', rstd[:, gs], ss[:, gs], 1.0 / D, 1e-6, ALU.mult, ALU.add, ['ssg%d' % g], ['rstdg%d' % g])
            ACT(rstd[:, gs], rstd[:, gs], AF.Ln, ['rstdg%d' % g], ['rstdg%d' % g])
            ACT(rstd[:, gs], rstd[:, gs], AF.Exp, ['rstdg%d' % g], ['rstdg%d' % g], scale=-0.5)
            for i in range(4 * g, 4 * g + 4):
                xb = xn[i % 2]
                STT(xb, h1[:, i, :], rstd[:, i:i + 1], gbc, ALU.mult, ALU.mult, ['h1_%d' % i, 'rstdg%d' % g, 'gbc'], ['xn%d' % (i % 2)])
                for half in range(2):
                    bn = 4 + (2 * i + half) % 4
                    for kk in range(4):
                        k = half * 4 + kk
                        P.op('pe', lambda e, bn=bn, kk=kk, k=k, xb=xb: e.transpose(
                            out=banks[bn][:, kk * 128:(kk + 1) * 128], in_=xb[:, k * 128:(k + 1) * 128], identity=identF),
                            reads=['xn%d' % (i % 2), 'cst'], writes=[BT(bn)], inc=(kk == 3))
                    P.op('act', lambda e, bn=bn, half=half, i=i: e.activation(
                        out=u2T[:, half * 4:half * 4 + 4, i * 128:(i + 1) * 128],
                        in_=banks[bn][:, :].rearrange("p (k t) -> p k t", t=128), func=AF.Copy),
                        reads=[BT(bn)], writes=['u2T', 'yrT%d' % i])

        for i in range(4):
            eproj(i)
        for g in range(4):
            if g < 3:
                for i in range(4 * g + 4, 4 * g + 8):
                    eproj(i)
            fnorm(g)
        dump('h1', h1[:, 0, :], [128, D], F32, reads=['h1_0'])
        if stop == 'E':
            P.finish()
            return nc, dbg_out
        P.barrier()

        moe_cvt(0)
        rtop = [work0 + 2700]

        def ralloc(n):
            ap = A.f32(n, at=rtop[0])
            rtop[0] += n
            return ap

        wrs = ralloc(288)
        wrb = A.bf(288, at=rtop[0]).rearrange("p (k c) -> p k c", c=36)
        rtop[0] += 144
        brbc = ralloc(36)
        lg = ralloc(16 * 36).rearrange("p (i c) -> p i c", c=36)
        gmax = ralloc(16)
        gsh = ralloc(64).rearrange("p (i c) -> p i c", c=4)
        gsum = ralloc(16)
        gp = ralloc(16)
        oh = ralloc(64).rearrange("p (i c) -> p i c", c=4)
        elm = ralloc(512).rearrange("p (i c) -> p i c", c=32)
        elm2 = ralloc(512).rearrange("p (i c) -> p i c", c=32)
        e1 = ralloc(512).rearrange("p (i c) -> p i c", c=32)
        e2 = ralloc(512).rearrange("p (i c) -> p i c", c=32)
        comb = ralloc(512).rearrange("p (i c) -> p i c", c=32)
        m1 = ralloc(16)
        m2 = ralloc(16)
        dm = ralloc(16)
        p1 = ralloc(16)
        p2 = ralloc(16)
        assert rtop[0] <= NW
        P.dma(wrs, wr_d[:, :], writes=['wrs'])
        P.dma(brbc, br_d.partition_broadcast(128), writes=['brbc'])
        P.op('pool', lambda e: e.tensor_copy(out=wrb[:, :, :].rearrange("p k c -> p (k c)"), in_=wrs), ['wrs'], ['wrb'])
        for i in range(NTILE):
            tk = slice(i * 128, (i + 1) * 128)
            b = 4 + i // 8
            for k in range(8):
                MM(banks[b][:, (i % 8) * 36:(i % 8) * 36 + 36], u2T[:, k, tk], wrb[:, k, :], ['u2T', 'wrb'], [BT(b)],
                   start=(k == 0), stop=(k == 7), inc=(k == 7))
        bb3 = brbc.rearrange("p (o c) -> p o c", o=1).to_broadcast([128, 8, 36])
        for hb in range(2):
            TT('dve', lg[:, hb * 8:(hb + 1) * 8, :], banks[4 + hb][:, 0:288].rearrange("p (i c) -> p i c", c=36), bb3,
               ALU.add, [BT(4 + hb), 'brbc'], ['lg'])
        dump('lg', lg[:, :, :].rearrange("p i c -> p (i c)"), [128, 576], F32, reads=['lg'])
        gl = lg[:, :, 0:4]
        el = lg[:, :, 4:36]
        b3 = lambda ap, n: ap.rearrange("p (i o) -> p i o", o=1).to_broadcast([128, 16, n])
        P.op('dve', lambda e: e.tensor_reduce(out=gmax, in_=gl, axis=AX.X, op=ALU.max), ['lg'], ['gmax'])
        TT('dve', gsh, gl, b3(gmax, 4), ALU.subtract, ['lg', 'gmax'], ['gsh'])
        TT('dve', oh, gl, b3(gmax, 4), ALU.is_equal, ['lg', 'gmax'], ['oh'])
        ACT(gsh, gsh, AF.Exp, ['gsh'], ['gsh'])
        P.op('dve', lambda e: e.tensor_reduce(out=gsum, in_=gsh, axis=AX.X, op=ALU.add), ['gsh'], ['gsum'])
        P.op('dve', lambda e: e.reciprocal(out=gp, in_=gsum), ['gsum'], ['gp'])
        TS('dve', oh, oh, 1e30, -1e30, ALU.mult, ALU.add, ['oh'], ['oh'])
        TT('dve', elm.rearrange("p i (g x) -> p i g x", g=4), el.rearrange("p i (g x) -> p i g x", g=4),
           oh.rearrange("p i (g o) -> p i g o", o=1).to_broadcast([128, 16, 4, 8]), ALU.add, ['lg', 'oh'], ['elm'])
        P.op('dve', lambda e: e.tensor_reduce(out=m1, in_=elm, axis=AX.X, op=ALU.max), ['elm'], ['m1'])
        TT('dve', e1, elm, b3(m1, 32), ALU.is_equal, ['elm', 'm1'], ['e1'])
        STT(elm2, e1, -1e30, elm, ALU.mult, ALU.add, ['e1', 'elm'], ['elm2'])
        P.op('dve', lambda e: e.tensor_reduce(out=m2, in_=elm2, axis=AX.X, op=ALU.max), ['elm2'], ['m2'])
        TT('dve', e2, elm2, b3(m2, 32), ALU.is_equal, ['elm2', 'm2'], ['e2'])
        TT('dve', dm, m2, m1, ALU.subtract, ['m1', 'm2'], ['dm'])
        ACT(dm, dm, AF.Exp, ['dm'], ['dm'])
        TS('dve', p1, dm, 1.0, None, ALU.add, ALU.bypass, ['dm'], ['p1'])
        P.op('dve', lambda e: e.reciprocal(out=p1, in_=p1), ['p1'], ['p1'])
        TT('dve', p2, dm, p1, ALU.mult, ['dm', 'p1'], ['p2'])
        TT('dve', p1, p1, gp, ALU.mult, ['p1', 'gp'], ['p1'])
        TT('dve', p2, p2, gp, ALU.mult, ['p2', 'gp'], ['p2'])
        TT('dve', comb, e1, b3(p1, 32), ALU.mult, ['e1', 'p1'], ['comb'])
        TT('dve', e2, e2, b3(p2, 32), ALU.mult, ['e2', 'p2'], ['e2'])
        TT('dve', comb, comb, e2, ALU.add, ['comb', 'e2'], ['comb'])
        dump('comb', comb[:, :, :].rearrange("p i c -> p (i c)"), [128, 512], F32, reads=['comb'])
        if stop == 'F':
            P.finish()
            return nc, dbg_out
        P.barrier()

        m0 = base0 + 8192
        sil = [A.f32(512, at=m0 + s * 512) for s in range(2)]
        hid = [A.bf(512, at=m0 + 1024 + s * 256) for s in range(2)]
        assert m0 + 1536 <= pers0
        hid4 = [[A.bf(512, at=m0 + 1024 + (bp * 2 + s) * 256) for s in range(2)] for bp in range(2)]
        assert m0 + 2048 <= pers0

        def moe_G(ex, tb):
            s2 = ex % 2
            bp = (ex * 4 + tb) % 2
            wgb = wb[s2][0].rearrange("p (k c) -> p k c", c=256)
            wub = wb[s2][1].rearrange("p (k c) -> p k c", c=256)
            tg, tu = 'wb%d_0' % s2, 'wb%d_1' % s2
            tsl = slice(tb * 512, (tb + 1) * 512)
            for dch in range(2):
                dc = slice(dch * 128, (dch + 1) * 128)
                bg, bu = 2 * dch, 2 * dch + 1
                for k in range(8):
                    MM(banks[bg][:, :], wgb[:, k, dc], u2T[:, k, tsl], [tg, 'u2T'], [BT(bg)], start=(k == 0), stop=(k == 7), inc=(k == 7))
                for k in range(8):
                    MM(banks[bu][:, :], wub[:, k, dc], u2T[:, k, tsl], [tu, 'u2T'], [BT(bu)], start=(k == 0), stop=(k == 7), inc=(k == 7))
                ACT(sil[dch], banks[bg][:, :], AF.Silu, [BT(bg)], ['sil%d' % dch])
                TT('dve', hid4[bp][dch], sil[dch], banks[bu][:, :], ALU.mult, ['sil%d' % dch, BT(bu)], ['hid%d_%d' % (bp, dch)])

        tmpE = [A.f32(512, at=work0 + q_ * 512) for q_ in range(4)]
        tctr = [0]
        OFFL = ((0, 1), (2, 0), (3, 0))

        def final_group(g):
            gs = slice(4 * g, 4 * g + 4)
            for i in range(4 * g, 4 * g + 4):
                P.op('act', lambda e, i=i: e.activation(out=junk, in_=h1[:, i, :], func=AF.Square, accum_out=ss[:, i:i + 1]),
                     reads=['h1_%d' % i], writes=['junk', 'ssg%d' % g])
            TS('dve', rstd[:, gs], ss[:, gs], 1.0 / D, 1e-6, ALU.mult, ALU.add, ['ssg%d' % g], ['rstdg%d' % g])
            ACT(rstd[:, gs], rstd[:, gs], AF.Ln, ['rstdg%d' % g], ['rstdg%d' % g])
            ACT(rstd[:, gs], rstd[:, gs], AF.Exp, ['rstdg%d' % g], ['rstdg%d' % g], scale=-0.5)
            for i in range(4 * g, 4 * g + 4):
                STT(h1[:, i, :], h1[:, i, :], rstd[:, i:i + 1], gbc, ALU.mult, ALU.mult, ['h1_%d' % i, 'rstdg%d' % g, 'gbc'], ['h1_%d' % i])
                P.dma(out_d[i * 128:(i + 1) * 128, :], h1[:, i, :], reads=['h1_%d' % i])

        def moe_D(ex, tb):
            s2 = ex % 2
            bp = (ex * 4 + tb) % 2
            wdb = wb[s2][2].rearrange("p (d c) -> p d c", c=D)
            td = 'wb%d_2' % s2
            for ti in range(4):
                i = tb * 4 + ti
                for half in range(2):
                    b = 4 + (ti % 2) * 2 + half
                    for dch in range(2):
                        MM(banks[b][:, :], hid4[bp][dch][:, ti * 128:(ti + 1) * 128], wdb[:, dch, half * 512:(half + 1) * 512],
                           ['hid%d_%d' % (bp, dch), td], [BT(b)], start=(dch == 0), stop=(dch == 1), inc=(dch == 1))
                    hsl = h1[:, i, half * 512:(half + 1) * 512]
                    if (ti, half) in OFFL:
                        q_ = tctr[0] % 4
                        tctr[0] += 1
                        ACT(tmpE[q_], banks[b][:, :], AF.Copy, [BT(b), 'comb'], ['tmpE%d' % q_], scale=comb[:, i, ex:ex + 1])
                        TT('pool', hsl, hsl, tmpE[q_], ALU.add, ['tmpE%d' % q_, 'h1_%d' % i], ['h1_%d' % i])
                    else:
                        STT(hsl, banks[b][:, :], comb[:, i, ex:ex + 1], hsl, ALU.mult, ALU.add, [BT(b), 'comb', 'h1_%d' % i], ['h1_%d' % i])

        blocks = [(ex, tb) for ex in range(NE) for tb in range(4)]
        P.dma(gbc, g3_d.partition_broadcast(128), writes=['gbc'])
        moe_G(0, 0)
        for n, (ex, tb) in enumerate(blocks):
            if tb == 0 and ex + 1 < NE:
                moe_dma(ex + 1)
            if tb == 2 and ex + 1 < NE:
                moe_cvt(ex + 1)
            if n + 1 < len(blocks):
                moe_G(*blocks[n + 1])
            moe_D(ex, tb)
            if ex == NE - 1:
                final_group(tb)
        P.finish()
    return nc, dbg_out


_CACHE = {}


def _consts():
    c = np.zeros((128, 1024), np.float32)
    c[:, 0:128] = np.eye(128, dtype=np.float32)
    p = np.arange(128)
    blk = (p[:, None] // 64 == p[None, :] // 64).astype(np.float32)
    c[:, 128:256] = blk / 64.0
    import ml_dtypes
    cb = np.zeros((128, 1536), ml_dtypes.bfloat16)
    cb[:, 0:128] = np.eye(128)
    cb[:, 128:256] = blk
    s = (p % 64)[:, None]
    t = np.arange(64)[None, :]
    cb[:, 256:320] = (s < t)
    cb[:, 320:384] = (s <= t)
    cb[:, 384:448] = (t < s)
    cb[:, 448:512] = (t == s)
    c[:, 256:1024] = np.ascontiguousarray(cb).view(np.float32)
    return c


def _prep_shared(inp):
    f = lambda a: np.ascontiguousarray(a, dtype=np.float32)
    w_in = inp["w_in"][0]
    wt = np.stack([w_in[:, c * 128:(c + 1) * 128].reshape(8, 128, 128).transpose(1, 0, 2).reshape(128, 1024)
                   for c in ORDER], 0)
    par = np.zeros((128, NPAR), np.float32)
    mu = inp["rwkv_mu"][0]
    for q in range(14):
        par[:, PC_MU + q] = mu[q * 128:(q + 1) * 128]
    cw = inp["conv_w"][0]
    for tap in range(3):
        for j in range(4):
            par[:, PC_CW + tap * 4 + j] = cw[tap, j * 128:(j + 1) * 128]
    for nm, col in (("decay_base", PC_DB), ("aaa_base", PC_AB), ("k_k", PC_KK), ("k_a", PC_KA),
                    ("ln_x_w", PC_LW), ("ln_x_b", PC_LB)):
        v = inp[nm][0]
        for hp in range(4):
            par[:, col + hp] = v[hp * 128:(hp + 1) * 128]
    rk = inp["r_k"][0].reshape(512)
    for hp in range(4):
        par[:, PC_RK + hp] = rk[hp * 128:(hp + 1) * 128]
    sh = {
        "w_in_t": f(wt),
        "params": par,
        "lora_up": f(np.concatenate([inp["decay_up"][0], inp["aaa_up"][0]], 0)),
        "gate_up": f(inp["gate_up"][0]),
        "g_mix": f(inp["norm_mix_g"][0]),
        "g_ffn": f(inp["norm_ffn_g"][0]),
        "g_fin": f(inp["norm_final_g"]),
        "w_out_t": f(inp["w_out"][0].reshape(8, 128, 1024).transpose(1, 0, 2).reshape(128, 8192)),
        "w_r_t": f(np.concatenate([inp["router_group_w"][0], inp["router_expert_w"][0]], 1)
                   .reshape(8, 128, 36).transpose(1, 0, 2).reshape(128, 288)),
        "b_r": f(np.concatenate([inp["router_group_b"][0], inp["router_expert_b"][0]], 0)),
        "wg_t": f(inp["expert_w_gate"][0].reshape(NE, 8, 128, 256).transpose(0, 2, 1, 3).reshape(NE, 128, 2048)),
        "wu_t": f(inp["expert_w_up"][0].reshape(NE, 8, 128, 256).transpose(0, 2, 1, 3).reshape(NE, 128, 2048)),
        "wd_t": f(inp["expert_w_down"][0].reshape(NE, 2, 128, 1024).transpose(0, 2, 1, 3).reshape(NE, 128, 2048)),
        "consts": _consts(),
    }
    return sh


def run(inp, n_cores=8, stop=None, dbg=(), trace=False):
    key = (stop, tuple(dbg))
    if key not in _CACHE:
        _CACHE[key] = build(stop=stop, dbg=dbg)
    nc, dbg_out = _CACHE[key]
    sh = _prep_shared(inp)
    x = np.asarray(inp["x"], dtype=np.float32)
    in_maps = []
    for c in range(n_cores):
        m = dict(sh)
        m["x"] = np.ascontiguousarray(x[c])
        in_maps.append(m)
    res = run_bass_kernel_spmd(nc, in_maps, core_ids=list(range(n_cores)), **({"trace": True} if trace else {}))
    return res


def kernel(**inputs):
    inp = {k: np.asarray(v) for k, v in inputs.items()}
    res = run(inp, n_cores=8)
    out = np.stack([np.asarray(res.results[c]["out"], dtype=np.float32) for c in range(8)], 0)
    return out
```
